# Optimizing a Trainium2 kernel written in Bass

```python
import jax, jax.numpy as jnp
from jax import lax
import numpy as np

D_MODEL = 2048
BATCH = 8
SEQ = 2048
DEPTH = 2

GRID_W = 64
CTX_LEN = 256
N_MIXERS = 2
N_HGRN_LAYERS = (DEPTH + 1) // 2
N_CONV_LAYERS = DEPTH // 2
HGRN_EXPAND = 128
HGRN_HEADS = D_MODEL // HGRN_EXPAND
HGRN_HEAD_V = D_MODEL // HGRN_HEADS
HGRN_CHUNK = 64
CONV_WIDTH = 31
CONV_HALF = D_MODEL // 2
MOE_GROUPS = 8
MOE_PER_GROUP = 8
MOE_EXPERTS = MOE_GROUPS * MOE_PER_GROUP
MOE_TOPK = 2
MOE_HIDDEN = D_MODEL // 4
MOE_BLOCK = 256
EPS = 1e-6

kernel_name = 'hybrid_hgrn2_conformer_hmoe_dit'


def rmsnorm(x, g):
    xf = x.astype(jnp.float32)
    y = xf * lax.rsqrt(jnp.mean(xf * xf, axis=-1, keepdims=True) + EPS)
    return (y * g.astype(jnp.float32)).astype(x.dtype)


def layernorm(x, g, b):
    xf = x.astype(jnp.float32)
    mu = jnp.mean(xf, axis=-1, keepdims=True)
    xc = xf - mu
    y = xc * lax.rsqrt(jnp.mean(xc * xc, axis=-1, keepdims=True) + EPS)
    return (y * g.astype(jnp.float32) + b.astype(jnp.float32)).astype(x.dtype)


def modulate(h, shift, scale):
    return h * (1.0 + scale) + shift


def _forget(z, lb):
    f = lb + (1.0 - lb) * jax.nn.sigmoid(z.astype(jnp.float32))
    return jnp.log(f), 1.0 - f


def _chunked_gated_scan(q, k, v, log_f, s0):
    b_, t_, h_, _ = q.shape
    vd = v.shape[-1]
    nc = t_ // HGRN_CHUNK

    def to_chunks(a):
        a = a.astype(jnp.float32).reshape(b_, nc, HGRN_CHUNK, h_, a.shape[-1])
        return a.transpose(1, 0, 3, 2, 4)

    tril = jnp.tril(jnp.ones((HGRN_CHUNK, HGRN_CHUNK), dtype=bool))[:, :, None]

    def step(s, inp):
        qc, kc, vc, gc = inp
        bcum = jnp.cumsum(gc, axis=2)
        o_inter = jnp.einsum('bhck,bhkv->bhcv', qc * jnp.exp(bcum), s)
        diff = bcum[:, :, :, None, :] - bcum[:, :, None, :, :]
        decay = jnp.where(tril, jnp.exp(jnp.where(tril, diff, 0.0)), 0.0)
        scores = jnp.einsum('bhtk,bhtsk,bhsk->bhts', qc, decay, kc)
        o_intra = jnp.einsum('bhts,bhsv->bhtv', scores, vc)
        blast = bcum[:, :, -1:, :]
        s_new = jnp.exp(blast[:, :, 0, :])[..., None] * s + jnp.einsum(
            'bhck,bhcv->bhkv', kc * jnp.exp(blast - bcum), vc)
        return s_new, o_inter + o_intra

    s_fin, o = lax.scan(step, s0, (to_chunks(q), to_chunks(k), to_chunks(v), to_chunks(log_f)))
    o = o.transpose(1, 0, 3, 2, 4).reshape(b_, t_, h_, vd)
    return o, s_fin


def hgrn2_mixer(a_lat, a_ctx, w_in, lb, onorm_g, w_out, ctx_out):
    lb_fw = lb[0].reshape(HGRN_HEADS, HGRN_EXPAND)
    lb_bw = lb[1].reshape(HGRN_HEADS, HGRN_EXPAND)

    def project(a):
        b_, t_, _ = a.shape
        q, i_in, z_fw, z_bw, g = jnp.split(a @ w_in, 5, axis=-1)
        hk = lambda t: t.reshape(b_, t_, HGRN_HEADS, HGRN_EXPAND)
        lf_fw, k_fw = _forget(hk(z_fw), lb_fw)
        lf_bw, k_bw = _forget(hk(z_bw), lb_bw)
        v = i_in.reshape(b_, t_, HGRN_HEADS, HGRN_HEAD_V)
        return hk(jax.nn.silu(q)), v, lf_fw, k_fw, lf_bw, k_bw, g

    def bidir(p, s_fw, s_bw):
        q, v, lf_fw, k_fw, lf_bw, k_bw, _ = p
        flip = lambda t: t[:, ::-1]
        o_fw, s_fw_new = _chunked_gated_scan(q, k_fw, v, lf_fw, s_fw)
        o_bw, s_bw_new = _chunked_gated_scan(flip(q), flip(k_bw), flip(v), flip(lf_bw), s_bw)
        return o_fw + flip(o_bw), s_fw_new, s_bw_new

    def readout(o, g):
        b_, t_, _ = g.shape
        o = rmsnorm(o, onorm_g).astype(g.dtype)
        o = o * jax.nn.silu(g).reshape(b_, t_, HGRN_HEADS, HGRN_HEAD_V)
        return o.reshape(b_, t_, D_MODEL) @ w_out

    p_ctx = project(a_ctx)
    p_lat = project(a_lat)
    s0 = jnp.zeros((a_lat.shape[0], HGRN_HEADS, HGRN_EXPAND, HGRN_HEAD_V), jnp.float32)
    o_ctx, s_fw, s_bw = bidir(p_ctx, s0, s0)
    o_lat, _, _ = bidir(p_lat, s_fw, s_bw)
    y_lat = readout(o_lat, p_lat[-1])
    y_ctx = readout(o_ctx, p_ctx[-1]) if ctx_out else None
    return y_lat, y_ctx


def _dwconv_1d(u, w):
    pad = CONV_WIDTH // 2
    return lax.conv_general_dilated(
        u, w[:, None, :].astype(u.dtype), window_strides=(1,), padding=[(pad, pad)],
        dimension_numbers=('NWC', 'WIO', 'NWC'), feature_group_count=u.shape[-1])


def conformer_conv(a, w_pw1, w_dw, b_dw, ln_g, ln_b, w_pw2, on_grid):
    val, gate = jnp.split(a @ w_pw1, 2, axis=-1)
    u = val * jax.nn.sigmoid(gate)
    if on_grid:
        b_, t_, d = u.shape
        rows = t_ // GRID_W
        ug = u.reshape(b_, rows, GRID_W, d)
        uh = ug[..., :CONV_HALF].reshape(b_ * rows, GRID_W, CONV_HALF)
        uh = _dwconv_1d(uh, w_dw[:, :CONV_HALF]).reshape(b_, rows, GRID_W, CONV_HALF)
        uv = ug[..., CONV_HALF:].transpose(0, 2, 1, 3).reshape(b_ * GRID_W, rows, d - CONV_HALF)
        uv = _dwconv_1d(uv, w_dw[:, CONV_HALF:]).reshape(b_, GRID_W, rows, d - CONV_HALF).transpose(0, 2, 1, 3)
        u = jnp.concatenate([uh, uv], axis=-1).reshape(b_, t_, d)
    else:
        u = _dwconv_1d(u, w_dw)
    u = jax.nn.silu(layernorm(u + b_dw, ln_g, ln_b))
    return u @ w_pw2


def hier_moe(h, w_r1, b_r1, w_r2, b_r2, w_gate, w_up, w_down):
    n, d = h.shape
    f32 = jnp.float32
    lg1 = (h @ w_r1).astype(f32) + b_r1.astype(f32)
    p1 = jax.nn.softmax(lg1, axis=-1)
    grp = jnp.argmax(lg1, axis=-1)
    pg = jnp.take_along_axis(p1, grp[:, None], axis=1)
    lg2 = jnp.einsum('nd,gde->nge', h, w_r2).astype(f32) + b_r2.astype(f32)
    lg2 = jnp.take_along_axis(lg2, grp[:, None, None], axis=1)[:, 0]
    top_p, top_i = lax.top_k(jax.nn.softmax(lg2, axis=-1), MOE_TOPK)
    gate = pg * top_p / jnp.sum(top_p, axis=-1, keepdims=True)
    expert = grp[:, None].astype(jnp.int32) * MOE_PER_GROUP + top_i.astype(jnp.int32)

    e_flat = expert.reshape(-1)
    w_flat = gate.reshape(-1)
    tok_flat = jnp.repeat(jnp.arange(n, dtype=jnp.int32), MOE_TOPK)
    order = jnp.argsort(e_flat)
    e_s, tok_s, w_s = e_flat[order], tok_flat[order], w_flat[order]
    counts = jnp.zeros((MOE_EXPERTS,), jnp.int32).at[e_flat].add(1)
    padded = (counts + MOE_BLOCK - 1) // MOE_BLOCK * MOE_BLOCK
    pad_end = jnp.cumsum(padded)
    pad_start = pad_end - padded
    raw_start = jnp.cumsum(counts) - counts
    pos = pad_start[e_s] + jnp.arange(n * MOE_TOPK, dtype=jnp.int32) - raw_start[e_s]
    n_blocks = -(-(n * MOE_TOPK) // MOE_BLOCK) + MOE_EXPERTS
    slot_tok = jnp.full((n_blocks * MOE_BLOCK,), n, jnp.int32).at[pos].set(tok_s)
    slot_w = jnp.zeros((n_blocks * MOE_BLOCK,), f32).at[pos].set(w_s)
    blk_start = jnp.arange(n_blocks, dtype=jnp.int32) * MOE_BLOCK
    blk_e = jnp.minimum(jnp.searchsorted(pad_end, blk_start, side='right'), MOE_EXPERTS - 1)
    h_pad = jnp.concatenate([h, jnp.zeros((1, d), h.dtype)], axis=0)

    def run_block(args):
        e, toks = args
        xb = h_pad[toks]
        return (jax.nn.silu(xb @ w_gate[e]) * (xb @ w_up[e])) @ w_down[e]

    out = lax.map(run_block, (blk_e, slot_tok.reshape(n_blocks, MOE_BLOCK)))
    out = out.reshape(-1, d) * slot_w[:, None].astype(h.dtype)
    y = jnp.zeros((n + 1, d), h.dtype).at[slot_tok].add(out)
    return y[:n]


def setup_inputs(seed: int = 0) -> dict:
    key = jax.random.key(seed)
    ks = jax.random.split(key, 28)
    f32 = jnp.float32
    D = D_MODEL
    nrm = lambda k, shape, s: jax.random.normal(k, shape, f32) * s
    return {
        'x': nrm(ks[0], (BATCH, SEQ, D), 1.0),
        'c': nrm(ks[1], (BATCH, D), 1.0),
        'ctx': nrm(ks[2], (BATCH, CTX_LEN, D), 1.0),
        'c_ctx': nrm(ks[3], (D,), 1.0),
        'ada_w': nrm(ks[4], (DEPTH, D, 6 * D), 0.5 * D ** -0.5),
        'ada_b': nrm(ks[5], (DEPTH, 6 * D), 0.02),
        'norm1_g': 1.0 + nrm(ks[6], (DEPTH, D), 0.02),
        'norm2_g': 1.0 + nrm(ks[7], (DEPTH, D), 0.02),
        'hgrn_w_in': nrm(ks[8], (N_HGRN_LAYERS, D, 5 * D), D ** -0.5),
        'hgrn_lb': nrm(ks[9], (2, DEPTH + 1, D), 0.1),
        'hgrn_onorm_g': 1.0 + nrm(ks[10], (N_HGRN_LAYERS, HGRN_HEAD_V), 0.02),
        'hgrn_w_out': nrm(ks[11], (N_HGRN_LAYERS, D, D), D ** -0.5),
        'conv_w_pw1': nrm(ks[12], (N_CONV_LAYERS, D, 2 * D), D ** -0.5),
        'conv_w_dw': nrm(ks[13], (N_CONV_LAYERS, CONV_WIDTH, D), CONV_WIDTH ** -0.5),
        'conv_b_dw': nrm(ks[14], (N_CONV_LAYERS, D), 0.02),
        'conv_ln_g': 1.0 + nrm(ks[15], (N_CONV_LAYERS, D), 0.02),
        'conv_ln_b': nrm(ks[16], (N_CONV_LAYERS, D), 0.02),
        'conv_w_pw2': nrm(ks[17], (N_CONV_LAYERS, D, D), D ** -0.5),
        'moe_w_r1': nrm(ks[18], (DEPTH, D, MOE_GROUPS), D ** -0.5),
        'moe_b_r1': nrm(ks[19], (DEPTH, MOE_GROUPS), 0.01),
        'moe_w_r2': nrm(ks[20], (DEPTH, MOE_GROUPS, D, MOE_PER_GROUP), D ** -0.5),
        'moe_b_r2': nrm(ks[21], (DEPTH, MOE_GROUPS, MOE_PER_GROUP), 0.01),
        'moe_w_gate': nrm(ks[22], (DEPTH, MOE_EXPERTS, D, MOE_HIDDEN), D ** -0.5),
        'moe_w_up': nrm(ks[23], (DEPTH, MOE_EXPERTS, D, MOE_HIDDEN), D ** -0.5),
        'moe_w_down': nrm(ks[24], (DEPTH, MOE_EXPERTS, MOE_HIDDEN, D), MOE_HIDDEN ** -0.5),
        'final_g': 1.0 + nrm(ks[25], (D,), 0.02),
    }


def reference(x, c, ctx, c_ctx, ada_w, ada_b, norm1_g, norm2_g, hgrn_w_in, hgrn_lb, hgrn_onorm_g,
              hgrn_w_out, conv_w_pw1, conv_w_dw, conv_b_dw, conv_ln_g, conv_ln_b, conv_w_pw2,
              moe_w_r1, moe_b_r1, moe_w_r2, moe_b_r2, moe_w_gate, moe_w_up, moe_w_down, final_g):
    bsz, seq, d = x.shape
    lb_all = jnp.cumsum(jax.nn.softmax(hgrn_lb.astype(jnp.float32), axis=1), axis=1)
    s_c = jax.nn.silu(c)
    s_cc = jax.nn.silu(c_ctx)
    reads_ctx = [i % N_MIXERS == 0 for i in range(DEPTH)]
    h, hc = x, ctx
    for i in range(DEPTH):
        j = i // N_MIXERS
        ctx_later = any(reads_ctx[i + 1:])
        mod = jnp.split((s_c @ ada_w[i] + ada_b[i])[:, None, :], 6, axis=-1)
        mod_c = jnp.split((s_cc @ ada_w[i] + ada_b[i])[None, None, :], 6, axis=-1)
        a = modulate(rmsnorm(h, norm1_g[i]), mod[0], mod[1])
        ac = None
        if reads_ctx[i] or ctx_later:
            ac = modulate(rmsnorm(hc, norm1_g[i]), mod_c[0], mod_c[1])
        if i % N_MIXERS == 0:
            y, yc = hgrn2_mixer(a, ac, hgrn_w_in[j], lb_all[:, i], hgrn_onorm_g[j], hgrn_w_out[j], ctx_later)
        else:
            y = conformer_conv(a, conv_w_pw1[j], conv_w_dw[j], conv_b_dw[j], conv_ln_g[j], conv_ln_b[j],
                               conv_w_pw2[j], True)
            yc = None
            if ctx_later:
                yc = conformer_conv(ac, conv_w_pw1[j], conv_w_dw[j], conv_b_dw[j], conv_ln_g[j],
                                    conv_ln_b[j], conv_w_pw2[j], False)
        h = h + mod[2] * y
        b = modulate(rmsnorm(h, norm2_g[i]), mod[3], mod[4])
        moe_args = (moe_w_r1[i], moe_b_r1[i], moe_w_r2[i], moe_b_r2[i], moe_w_gate[i], moe_w_up[i], moe_w_down[i])
        if ctx_later:
            hc = hc + mod_c[2] * yc
            bc = modulate(rmsnorm(hc, norm2_g[i]), mod_c[3], mod_c[4])
            n_lat = bsz * seq
            z = hier_moe(jnp.concatenate([b.reshape(-1, d), bc.reshape(-1, d)], axis=0), *moe_args)
            h = h + mod[5] * z[:n_lat].reshape(bsz, seq, d)
            hc = hc + mod_c[5] * z[n_lat:].reshape(hc.shape)
        else:
            z = hier_moe(b.reshape(-1, d), *moe_args)
            h = h + mod[5] * z.reshape(bsz, seq, d)
    return rmsnorm(h, final_g)
```

```python
import numpy as np
import concourse.bass as bass
import concourse.mybir as mybir

F32 = mybir.dt.float32
BF16 = mybir.dt.bfloat16
I32 = mybir.dt.int32
AF = mybir.ActivationFunctionType
ALU = mybir.AluOpType
AX = mybir.AxisListType

SAME_ENG_SYNC = False


class Reg:
    __slots__ = ("name", "w", "r", "excl")

    def __init__(self, name):
        self.name = name
        self.excl = False
        self.w = None
        self.r = {}


def _tok_key(t):
    return t[0:2]


class Prog:
    ENGS = ["pe", "act", "dve", "pool", "sp"]

    def __init__(self, nc, esems, dsems):
        self.nc = nc
        self.esems = esems
        self.dsems = dsems
        self.dcount = {q: [0] * len(v) for q, v in dsems.items()}
        self.drr = {q: 0 for q in dsems}
        self.ecount = {e: 0 for e in self.ENGS}
        self.regs = {}
        self.reset_phase()

    def reg(self, *key):
        r = self.regs.get(key)
        if r is None:
            r = self.regs[key] = Reg(key)
        return r

    def reset_phase(self):
        self.ops = {e: [] for e in self.ENGS}
        for r in self.regs.values():
            r.w = None
            r.r = {}

    def _deps(self, reads, writes):
        deps = {}

        def add(t):
            k = _tok_key(t)
            if k not in deps or deps[k][2] < t[2]:
                deps[k] = t
        for r in reads:
            if r.w is not None:
                add(r.w)
        for w in writes:
            if w.w is not None:
                add(w.w)
            for t in w.r.values():
                add(t)
        return deps

    def _commit(self, tok, reads, writes):
        k = _tok_key(tok)
        for r in reads:
            r.r[k] = tok
        for w in writes:
            w.w = tok
            w.r = {}

    def op(self, eng, fn, reads=(), writes=()):
        writes = list(writes) + [r for r in reads if r.excl]
        reads = [r for r in reads if not r.excl]
        deps = self._deps(reads, writes)
        idx = len(self.ops[eng])
        tok = ("E", eng, idx)
        t = deps.get(("E", eng))
        if t is not None and (eng == "pe" or idx - t[2] > 3):
            deps.pop(("E", eng), None)
        self.ops[eng].append(dict(fn=fn, deps=list(deps.values()), dma=None, inc=False))
        self._commit(tok, reads, writes)

    def dma(self, q, fn, reads=(), writes=()):
        deps = self._deps(reads, writes)
        i = self.drr[q]
        self.drr[q] = (i + 1) % len(self.dsems[q])
        prev = self.dcount[q][i]
        if prev > 0:
            t = ("D", (q, i), prev)
            k = _tok_key(t)
            if k not in deps or deps[k][2] < prev:
                deps[k] = t
        self.dcount[q][i] = prev + 16
        tok = ("D", (q, i), prev + 16)
        self.ops[q].append(dict(fn=fn, deps=list(deps.values()), dma=(q, i), inc=False))
        self._commit(tok, reads, writes)

    def emit_phase(self, name=None):
        nc = self.nc
        finals = {e: [] for e in self.ENGS}
        for q in self.dsems:
            for i, c in enumerate(self.dcount[q]):
                if c > 0:
                    finals[q].append((self.dsems[q][i], c))
        for e in self.ENGS:
            for o in self.ops[e]:
                for t in o["deps"]:
                    if t[0] == "E":
                        self.ops[t[1]][t[2]]["inc"] = True
        cum = {}
        for e in self.ENGS:
            c = self.ecount[e]
            arr = []
            for o in self.ops[e]:
                if o["inc"] and o["dma"] is None:
                    c += 1
                arr.append(c)
            cum[e] = arr
        ops = self.ops
        esems, dsems = self.esems, self.dsems

        def run(eng_name, eng):
            waited = {}
            for o in ops[eng_name]:
                for t in o["deps"]:
                    if t[0] == "E":
                        sem = esems[t[1]]
                        val = cum[t[1]][t[2]]
                        key = ("E", t[1])
                    else:
                        sem = dsems[t[1][0]][t[1][1]]
                        val = t[2]
                        key = t[1]
                    if waited.get(key, -1) >= val:
                        continue
                    waited[key] = val
                    eng.wait_ge(sem, val)
                ins = o["fn"](eng)
                if o["dma"] is not None:
                    q, i = o["dma"]
                    ins.then_inc(dsems[q][i], 16)
                elif o["inc"]:
                    ins.then_inc(esems[eng_name], 1)
            for sem, c in finals[eng_name]:
                eng.wait_ge(sem, c)

        with nc.Block() as block:
            @block.tensor
            def _(e):
                run("pe", e)

            @block.scalar
            def _(e):
                run("act", e)

            @block.vector
            def _(e):
                run("dve", e)

            @block.gpsimd
            def _(e):
                run("pool", e)

            @block.sync
            def _(e):
                run("sp", e)
        for e in self.ENGS:
            if cum[e]:
                self.ecount[e] = cum[e][-1]
        n = {e: len(self.ops[e]) for e in self.ENGS}
        self.reset_phase()
        return n


from contextlib import ExitStack
import ml_dtypes
from concourse.bass_utils import run_bass_kernel_spmd

T = 2048; D = 2048; CT = 256; NT = 16; NTC = 2; CAP = 256; NE = 64
EPS = 1e-6
XS_ROWS = NE * CAP

_cache = {}
CSTOP = 99
LATSTOP = 99
COPYSEL = 0


class Tn:
    def __init__(self, P, t, name):
        self.t = t
        self.r = P.reg(name)


class _Stop(Exception):
    pass


def build_program(debug=False, stop=None, nheads=16):
    nc = bass.Bass("TRN2", target_bir_lowering=False)
    dr = lambda n, s, d=F32: nc.dram_tensor(n, s, d, kind="ExternalInput").ap()
    x = dr("x", [T, D]); ctx = dr("ctx", [CT, D]); ccol = dr("ccol", [128, 32])
    ada_w = dr("ada_w", [2, D, 6 * D]); ada_b = dr("ada_b", [2, 6 * D])
    n1g = dr("n1g", [2, D]); n2g = dr("n2g", [2, D]); fing = dr("fing", [1, D])
    w_in = dr("w_in", [D, 5 * D]); hlb = dr("hlb", [2, 3, D]); ong = dr("ong", [1, 128]); w_out = dr("w_out", [D, D])
    w_pw1 = dr("w_pw1", [D, 2 * D]); wdw = dr("wdw", [128, 16, 31]); bdw = dr("bdw", [128, 16])
    lng = dr("lng", [128, 16]); lnb = dr("lnb", [128, 16]); w_pw2 = dr("w_pw2", [D, D])
    wr = dr("wr", [2, D, 72]); brr = dr("brr", [2, 72])
    if stop is None or stop >= 5:
        wg = dr("wg", [2, NE, D, 512]); wu = dr("wu", [2, NE, D, 512]); wd = dr("wd", [2, NE, 512, D])
    cst_f = dr("cst_f", [128, 9, 128])
    cst_b = dr("cst_b", [128, 3, 128], BF16)
    iot = dr("iot", [128, 65])
    out = nc.dram_tensor("out", [T, D], F32, kind="ExternalOutput").ap()
    Hs = nc.dram_tensor("Hs", [T, D], F32, kind=("ExternalOutput" if debug else "Internal")).ap()
    DER = nc.dram_tensor("DER", [2, 128, 6, D], F32, kind=("ExternalOutput" if debug else "Internal")).ap()
    DERC = nc.dram_tensor("DERC", [128, 2, D], F32, kind=("ExternalOutput" if debug else "Internal")).ap()
    OGT = nc.dram_tensor("OGT", [D, T], BF16, kind=("ExternalOutput" if debug else "Internal")).ap()
    Xs = nc.dram_tensor("Xs", [XS_ROWS + 128, D], BF16, kind=("ExternalOutput" if debug else "Internal")).ap()
    Ys = nc.dram_tensor("Ys", [XS_ROWS, D], BF16, kind=("ExternalOutput" if debug else "Internal")).ap()
    Vd = nc.dram_tensor("Vd", [D, T], F32, kind=("ExternalOutput" if debug else "Internal")).ap()
    dbg = {}
    if debug:
        for n in ["hmid0", "hend0", "hmid1"]:
            dbg[n] = nc.dram_tensor("dbg_" + n, [T, D], F32, kind="ExternalOutput").ap()

    try:
      with ExitStack() as es0:
          esems = {e: es0.enter_context(nc.semaphore("e_" + e)) for e in Prog.ENGS}
          dsems = {q: [es0.enter_context(nc.semaphore(f"d_{q}{i}")) for i in range(n)] for q, n in [("sp", 8), ("pool", 6), ("act", 2)]}
          P = Prog(nc, esems, dsems)
          es0.enter_context(nc.allow_low_precision("bf16 matmul operands, fp32 accumulation"))

          uid = [0]

          def SB(es, name, shape, dt=F32):
              uid[0] += 1
              name = f"{name}_{uid[0]}"
              return Tn(P, es.enter_context(nc.sbuf_tensor(name, shape, dt)), name)

          def PS(es, name, shape, dt=F32):
              uid[0] += 1
              name = f"{name}_{uid[0]}"
              t_ = Tn(P, es.enter_context(nc.psum_tensor(name, shape, dt)), name)
              t_.r.excl = True
              return t_

          phase_no = [0]

          def END_PHASE():
              P.emit_phase()
              phase_no[0] += 1
              if stop is not None and phase_no[0] >= stop:
                  raise _Stop()

          def rr(l):
              return [a.r if isinstance(a, Tn) else a for a in l]

          def ACT(out, in_, func, R, W, **kw):
              P.op("act", lambda e: e.activation(out=out, in_=in_, func=func, **kw), rr(R), rr(W))

          def TT(eng, out, in0, in1, op, R, W):
              P.op(eng, lambda e: e.tensor_tensor(out=out, in0=in0, in1=in1, op=op), rr(R), rr(W))

          def TS(eng, out, in0, s1, s2, op0, op1, R, W, **kw):
              if s2 is None:
                  P.op(eng, lambda e: e.tensor_scalar(out=out, in0=in0, scalar1=s1, scalar2=None, op0=op0, **kw), rr(R), rr(W))
              else:
                  P.op(eng, lambda e: e.tensor_scalar(out=out, in0=in0, scalar1=s1, scalar2=s2, op0=op0, op1=op1, **kw), rr(R), rr(W))

          def STT(eng, out, in0, sc, in1, op0, op1, R, W):
              P.op(eng, lambda e: e.scalar_tensor_tensor(out=out, in0=in0, scalar=sc, in1=in1, op0=op0, op1=op1), rr(R), rr(W))

          def CP(eng, out, in_, R, W):
              if eng == "act":
                  P.op("act", lambda e: e.copy(out=out, in_=in_), rr(R), rr(W))
              else:
                  P.op(eng, lambda e: e.tensor_copy(out=out, in_=in_), rr(R), rr(W))

          def MM(out, lhsT, rhs, start, stop, R, W):
              P.op("pe", lambda e: e.matmul(out, lhsT=lhsT, rhs=rhs, start=start, stop=stop), rr(R), rr(W))

          def TR(out, in_, ident, R, W):
              P.op("pe", lambda e: e.transpose(out=out, in_=in_, identity=ident), rr(R), rr(W))

          def DMA(q, out, in_, R, W):
              P.dma(q, lambda e: e.dma_start(out=out, in_=in_), rr(R), rr(W))

          def RED(eng, out, in_, op, R, W):
              P.op(eng, lambda e: e.tensor_reduce(out=out, in_=in_, axis=AX.X, op=op), rr(R), rr(W))

          def RECIP(out, in_, R, W):
              P.op("dve", lambda e: e.reciprocal(out=out, in_=in_), rr(R), rr(W))

          def MEMSET(eng, ap, v, W):
              P.op(eng, lambda e: e.memset(ap, v), [], rr(W))

          CF = SB(es0, "CF", [128, 9, 128]); CB = SB(es0, "CB", [128, 3, 128], BF16)
          IOT = SB(es0, "IOT", [128, 65]); EPSb = SB(es0, "EPSb", [128, 1])
          SLOTG = SB(es0, "SLOTG", [128, NT, 2], I32); GATES = SB(es0, "GATES", [128, NT, 2])
          identf = CF.t[:, 0, :]; LcT = [CF.t[:, 1, :], CF.t[:, 2, :]]; E2T = [CF.t[:, 3, :], CF.t[:, 4, :]]
          onesf = CF.t[:, 7, :]; IND = CF.t[:, 8, 0:2]
          identb = CB.t[:, 0, :]; onesb = CB.t[:, 1, :]; UTs = CB.t[:, 2, :]

          def load_consts():
              DMA("sp", CF.t[:], cst_f[:, :, :], [], [CF])
              DMA("sp", CB.t[:], cst_b[:, :, :], [], [CB])
              DMA("sp", IOT.t[:], iot[:, :], [], [IOT])
              MEMSET("pool", EPSb.t[:], EPS, [EPSb])

          def rms_rstd(xt_ap, ss, rstd, junk, R, n=D):
              ACT(junk.t[:, 0:n], xt_ap, AF.Square, R, [junk, ss], accum_out=ss.t[:])
              ACT(rstd.t[:], ss.t[:], AF.Sqrt, [ss, EPSb], [rstd], scale=1.0 / n, bias=EPSb.t[:])
              RECIP(rstd.t[:], rstd.t[:], [rstd], [rstd])

          with ExitStack() as es:
              load_consts()
              cc = SB(es, "cc", [128, 32]); sc = SB(es, "sc", [128, 32])
              Srep = SB(es, "Srep", [128, 32, 128])
              wa = [SB(es, f"wa{i}", [128, 16, 512]) for i in range(2)]
              ab = [SB(es, f"ab{i}", [128, 512]) for i in range(2)]
              MODt = SB(es, "MODt", [128, 6 * D]); MODc = SB(es, "MODc", [128, 2 * D])
              gt = [SB(es, f"gt{i}", [128, D]) for i in range(2)]
              pa = [PS(es, f"pa{i}", [128, 512]) for i in range(2)]
              pc = [PS(es, f"pc{i}", [128, 512]) for i in range(2)]
              DMA("sp", cc.t[:], ccol[:, :], [], [cc])
              ACT(sc.t[:], cc.t[:], AF.Silu, [cc], [sc])
              for k in range(32):
                  ACT(Srep.t[:, k, :], onesf, AF.Copy, [CF, sc], [Srep], scale=sc.t[:, k:k + 1])
              for l in range(2):
                  awv = ada_w[l].rearrange("(kc p) n -> p kc n", p=128)
                  for j in range(24):
                      w_ = wa[j % 2]; a_ = ab[j % 2]; p_ = pa[j % 2]; q_ = pc[j % 2]
                      DMA("sp", w_.t[:], awv[:, :, j * 512:(j + 1) * 512], [], [w_])
                      DMA("act", a_.t[:], ada_b[l:l + 1, j * 512:(j + 1) * 512].partition_broadcast(128), [], [a_])
                      for kc in range(16):
                          MM(p_.t[:], Srep.t[:, kc, :], w_.t[:, kc, :], kc == 0, kc == 15, [Srep, w_], [p_])
                      TT("dve", MODt.t[:, j * 512:(j + 1) * 512], p_.t[:], a_.t[:], ALU.add, [p_, a_], [MODt])
                      if l == 0 and j < 8:
                          for kc in range(16):
                              MM(q_.t[:], Srep.t[:, 16 + kc, :], w_.t[:, kc, :], kc == 0, kc == 15, [Srep, w_], [q_])
                          TT("dve", MODc.t[:, j * 512:(j + 1) * 512], q_.t[:], a_.t[:], ALU.add, [q_, a_], [MODc])
                  DMA("act", gt[0].t[:], n1g[l:l + 1, :].partition_broadcast(128), [], [gt[0]])
                  DMA("act", gt[1].t[:], n2g[l:l + 1, :].partition_broadcast(128), [], [gt[1]])
                  STT("dve", MODt.t[:, D:2 * D], MODt.t[:, D:2 * D], 1.0, gt[0].t[:], ALU.add, ALU.mult, [MODt, gt[0]], [MODt])
                  STT("dve", MODt.t[:, 4 * D:5 * D], MODt.t[:, 4 * D:5 * D], 1.0, gt[1].t[:], ALU.add, ALU.mult, [MODt, gt[1]], [MODt])
                  DMA("sp", DER[l].rearrange("p s d -> p (s d)"), MODt.t[:], [MODt], [P.reg("DER")])
                  if l == 0:
                      STT("dve", MODc.t[:, D:2 * D], MODc.t[:, D:2 * D], 1.0, gt[0].t[:], ALU.add, ALU.mult, [MODc, gt[0]], [MODc])
                      DMA("sp", DERC.rearrange("p s d -> p (s d)"), MODc.t[:], [MODc], [P.reg("DERC")])
              END_PHASE()

          def norm_mod_transpose(es, xt, A1, B1, dstT, col0, bufs):
              ss, rstd, junk, xn, ab_, pt = bufs
              rms_rstd(xt.t[:], ss, rstd, junk, [xt])
              ACT(xn.t[:], xt.t[:], AF.Copy, [xt, rstd], [xn], scale=rstd.t[:])
              TT("dve", xn.t[:], xn.t[:], A1.t[:], ALU.mult, [xn, A1], [xn])
              TT("pool", ab_.t[:], xn.t[:], B1.t[:], ALU.add, [xn, B1], [ab_])
              for hf in range(2):
                  p_ = pt[hf]
                  for k in range(8):
                      kc = hf * 8 + k
                      TR(p_.t[:, k, :], ab_.t[:, kc * 128:(kc + 1) * 128], identb, [ab_, CB], [p_])
                  CP("dve" if hf == 0 else "act", dstT.t[:, hf * 8:(hf + 1) * 8, col0:col0 + 128], p_.t[:], [p_], [dstT])

          with ExitStack() as esBC:
              aT = SB(esBC, "aT", [128, 16, CT + T], BF16)
              with ExitStack() as es:
                  A1 = SB(es, "A1", [128, D]); B1 = SB(es, "B1", [128, D]); A1c = SB(es, "A1c", [128, D]); B1c = SB(es, "B1c", [128, D])
                  xt = [SB(es, f"xt{i}", [128, D]) for i in range(2)]
                  ss = SB(es, "ss", [128, 1]); rstd = SB(es, "rstd", [128, 1]); junk = SB(es, "junk", [128, D], BF16)
                  xn = SB(es, "xn", [128, D]); ab_ = SB(es, "abf", [128, D], BF16)
                  pt = [PS(es, f"pt{i}", [128, 8, 128], BF16) for i in range(2)]
                  DMA("sp", B1.t[:], DER[0, :, 0, :], [], [B1]); DMA("sp", A1.t[:], DER[0, :, 1, :], [], [A1])
                  DMA("sp", B1c.t[:], DERC[:, 0, :], [], [B1c]); DMA("sp", A1c.t[:], DERC[:, 1, :], [], [A1c])
                  for i in range(NTC + NT):
                      x_ = xt[i % 2]
                      src = ctx[i * 128:(i + 1) * 128, :] if i < NTC else x[(i - NTC) * 128:(i - NTC + 1) * 128, :]
                      DMA("sp", x_.t[:], src, [], [x_])
                      norm_mod_transpose(es, x_, A1c if i < NTC else A1, B1c if i < NTC else B1, aT, i * 128, (ss, rstd, junk, xn, ab_, pt))
                  END_PHASE()

              with ExitStack() as es:
                  NTT = NTC + NT
                  wsl = [SB(es, f"wsl{i}", [128, 16, 640], BF16) for i in range(2)]
                  lbr = SB(es, "lbr", [128, 2, 3, 128]); lb2 = SB(es, "lb2", [128, 2, 128]); oml2 = SB(es, "oml2", [128, 2, 128])
                  lsum = SB(es, "lsum", [128, 2, 128])
                  ongb = SB(es, "ongb", [128, 128])
                  QTs = SB(es, "QTs", [128, 2, NT, 128], BF16)
                  ER = SB(es, "ER", [128, NTT, 4]); ERD = SB(es, "ERD", [128, NTT, 2])
                  DS = SB(es, "DS", [128, 2, NTT, 128]); OPb = SB(es, "OPb", [128, NT, 128]); SGS = SB(es, "SGS", [128, NT, 128])
                  OGh = SB(es, "OGh", [128, T], BF16)
                  qs = [SB(es, f"qs{i}", [128, 128]) for i in range(2)]
                  vb = [SB(es, f"vb{i}", [128, 128], BF16) for i in range(2)]
                  sig = [SB(es, f"sig{i}", [128, 256]) for i in range(2)]
                  gl = [SB(es, f"gl{i}", [128, 256]) for i in range(2)]
                  kk = [SB(es, f"kk{i}", [128, 256]) for i in range(2)]
                  Eb = [SB(es, f"Eb{i}", [128, 256]) for i in range(2)]
                  Ei = [SB(es, f"Ei{i}", [128, 256]) for i in range(2)]
                  E2 = [SB(es, f"E2{i}", [128, 256]) for i in range(2)]
                  qt = [SB(es, f"qt{i}", [128, 256], BF16) for i in range(2)]
                  kt = [SB(es, f"kt{i}", [128, 256], BF16) for i in range(2)]
                  kh = [SB(es, f"kh{i}", [128, 256], BF16) for i in range(2)]
                  qTt = [SB(es, f"qTt{i}", [128, 2, 128], BF16) for i in range(2)]
                  kTt = [SB(es, f"kTt{i}", [128, 2, 128], BF16) for i in range(2)]
                  sT = [SB(es, f"sT{i}", [128, 2, 128], BF16) for i in range(2)]
                  kTz = [SB(es, f"kTz{i}", [128, 2, 128], BF16) for i in range(2)]
                  for i_ in range(2):
                      MEMSET("pool", kTz[i_].t[:], 0.0, [kTz[i_]])
                  Sst = [SB(es, f"Sst{i}", [128, 128]) for i in range(2)]
                  Smid = [SB(es, f"Smid{i}", [128, 128], BF16) for i in range(2)]
                  o2 = SB(es, "o2", [128, T]); ssq = SB(es, "ssq", [128, NT]); rs16 = SB(es, "rs16", [128, NT])
                  on = SB(es, "on", [128, 128]); onb = SB(es, "onb", [128, 128], BF16)
                  PQ = [PS(es, f"PQ{i}", [128, 512]) for i in range(2)]
                  PQ2 = PS(es, "PQ2", [128, 4, 128])
                  PC = PS(es, "PC", [128, 4, 128])
                  PT = PS(es, "PT", [128, 8, 128], BF16)
                  PSs = PS(es, "PSs", [128, 4, 128])
                  PD = PS(es, "PD", [128, 4, 128])
                  PTo = PS(es, "PTo", [128, 8, 128], BF16)
                  MT = [CF.t[:, 5, :], CF.t[:, 6, :]]
                  DMA("act", ongb.t[:], ong[0:1, :].partition_broadcast(128), [], [ongb])
                  w_inv = w_in.rearrange("(kc p) n -> p kc n", p=128)

                  def load_head_w(h):
                      w_ = wsl[h % 2]
                      for s in range(5):
                          DMA("pool", w_.t[:, :, s * 128:(s + 1) * 128], w_inv[:, :, s * D + h * 128: s * D + (h + 1) * 128], [], [w_])
                  load_head_w(0)
                  for h in range(nheads):
                      if h + 1 < nheads:
                          load_head_w(h + 1)
                      w_ = wsl[h % 2]
                      for d_ in range(2):
                          for s_ in range(3):
                              DMA("act", lbr.t[:, d_, s_, :], hlb[d_, s_:s_ + 1, h * 128:(h + 1) * 128].partition_broadcast(128), [], [lbr])
                      lbrf = lbr.t[:].rearrange("p a s k -> p (a s k)")
                      ACT(lbrf, lbrf, AF.Exp, [lbr], [lbr])
                      TT("dve", lsum.t[:], lbr.t[:, :, 0, :], lbr.t[:, :, 1, :], ALU.add, [lbr], [lsum])
                      TT("dve", lsum.t[:], lsum.t[:], lbr.t[:, :, 2, :], ALU.add, [lbr, lsum], [lsum])
                      RECIP(lsum.t[:], lsum.t[:], [lsum], [lsum])
                      TT("dve", lb2.t[:], lbr.t[:, :, 0, :], lsum.t[:], ALU.mult, [lbr, lsum], [lb2])
                      TS("dve", oml2.t[:], lb2.t[:], -1.0, 1.0, ALU.mult, ALU.add, [lb2], [oml2])
                      lbf = lb2.t[:].rearrange("p a k -> p (a k)"); omf = oml2.t[:].rearrange("p a k -> p (a k)")
                      for i in range(NTT if CSTOP >= 4 else (0 if CSTOP < 2 else (1 if CSTOP == 2 else 3))):
                          b = i % 2
                          lat = i >= NTC
                          li = i - NTC
                          pq = PQ[b]
                          for kc in range(16):
                              MM(pq.t[:], aT.t[:, kc, i * 128:(i + 1) * 128], w_.t[:, kc, 0:512], kc == 0, kc == 15, [aT, w_], [pq])
                          if lat:
                              for kc in range(16):
                                  MM(PQ2.t[:, b, :], aT.t[:, kc, i * 128:(i + 1) * 128], w_.t[:, kc, 512:640], kc == 0, kc == 15, [aT, w_], [PQ2])
                              ACT(qs[b].t[:], pq.t[:, 0:128], AF.Silu, [pq], [qs[b]])
                              ACT(SGS.t[:, li, :], PQ2.t[:, b, :], AF.Silu, [PQ2], [SGS])
                          ACT(sig[b].t[:], pq.t[:, 256:512], AF.Sigmoid, [pq], [sig[b]])
                          CP("dve", vb[b].t[:], pq.t[:, 128:256], [pq], [vb[b]])
                          TT("dve", sig[b].t[:], sig[b].t[:], omf, ALU.mult, [sig[b], oml2], [sig[b]])
                          TT("pool", sig[b].t[:], sig[b].t[:], lbf, ALU.add, [sig[b], lb2], [sig[b]])
                          ACT(gl[b].t[:], sig[b].t[:], AF.Ln, [sig[b]], [gl[b]])
                          TS("pool", kk[b].t[:], sig[b].t[:], -1.0, 1.0, ALU.mult, ALU.add, [sig[b]], [kk[b]])
                          for d_ in range(2):
                              if lat:
                                  MM(PC.t[:, d_, :], LcT[d_], gl[b].t[:, d_ * 128:(d_ + 1) * 128], True, True, [CF, gl[b]], [PC])
                              MM(PC.t[:, 2 + d_, :], E2T[d_], gl[b].t[:, d_ * 128:(d_ + 1) * 128], True, True, [CF, gl[b]], [PC])
                              MM(PSs.t[:, 3, 2 * d_:2 * d_ + 2], gl[b].t[:, d_ * 128:(d_ + 1) * 128], IND, True, True, [CF, gl[b]], [PSs])
                          ACT(E2[b].t[:], PC.t[:, 2:4, :].rearrange("p a k -> p (a k)"), AF.Exp, [PC], [E2[b]])
                          ACT(ER.t[:, i, :], PSs.t[:, 3, 0:4], AF.Exp, [PSs], [ER])
                          TT("pool", kh[b].t[:], kk[b].t[:], E2[b].t[:], ALU.mult, [kk[b], E2[b]], [kh[b]])
                          for d_ in range(2):
                              MM(PD.t[:, d_, :], kh[b].t[:, d_ * 128:(d_ + 1) * 128], vb[b].t[:], True, True, [kh[b], vb[b]], [PD])
                          CP("dve", DS.t[:, :, i, :], PD.t[:, 0:2, :], [PD], [DS])
                          if not lat or LATSTOP < 2:
                              continue
                          if lat:
                              ACT(Eb[b].t[:], PC.t[:, 0:2, :].rearrange("p a k -> p (a k)"), AF.Exp, [PC], [Eb[b]])
                              ACT(Ei[b].t[:], PC.t[:, 0:2, :].rearrange("p a k -> p (a k)"), AF.Exp, [PC], [Ei[b]], scale=-1.0)
                              for d_ in range(2):
                                  TT("pool", qt[b].t[:, d_ * 128:(d_ + 1) * 128], qs[b].t[:], Eb[b].t[:, d_ * 128:(d_ + 1) * 128], ALU.mult, [qs[b], Eb[b]], [qt[b]])
                              TT("dve", kt[b].t[:], kk[b].t[:], Ei[b].t[:], ALU.mult, [kk[b], Ei[b]], [kt[b]])
                              if LATSTOP < 3:
                                  continue
                              for d_ in range(2):
                                  TR(PT.t[:, d_, :], qt[b].t[:, d_ * 128:(d_ + 1) * 128], identb, [qt[b], CB], [PT])
                                  TR(PT.t[:, 2 + d_, :], kt[b].t[:, d_ * 128:(d_ + 1) * 128], identb, [kt[b], CB], [PT])
                              if COPYSEL in (0, 1, 3):
                                  CP("dve", qTt[b].t[:], PT.t[:, 0:2, :], [PT], [qTt[b]])
                              if COPYSEL in (0, 2, 3):
                                  CP("act", kTt[b].t[:], PT.t[:, 2:4, :], [PT], [kTt[b]])
                              if COPYSEL in (0,):
                                  CP("dve", QTs.t[:, :, li, :], qTt[b].t[:], [qTt[b]], [QTs])
                              if LATSTOP < 4:
                                  continue
                              CP("dve", kTz[b].t[:, 0, 0:64], PT.t[:, 2, 0:64], [PT], [kTz[b]])
                              CP("dve", kTz[b].t[:, 1, 64:128], PT.t[:, 3, 64:128], [PT], [kTz[b]])
                              MM(PSs.t[:, 0, 64:128], kTt[b].t[:, 0, :], qTt[b].t[:, 0, 64:128], True, True, [kTt[b], qTt[b]], [PSs])
                              MM(PSs.t[:, 0, 0:64], kTz[b].t[:, 0, :], qTt[b].t[:, 0, 0:64], True, True, [kTz[b], qTt[b]], [PSs])
                              MM(PSs.t[:, 1, 0:64], kTt[b].t[:, 1, :], qTt[b].t[:, 1, 0:64], True, True, [kTt[b], qTt[b]], [PSs])
                              MM(PSs.t[:, 1, 64:128], kTz[b].t[:, 1, :], qTt[b].t[:, 1, 64:128], True, True, [kTz[b], qTt[b]], [PSs])
                              TT("dve", sT[b].t[:], PSs.t[:, 0:2, :], CF.t[:, 5:7, :], ALU.mult, [PSs, CF], [sT[b]])
                              if LATSTOP < 5:
                                  continue
                              for d_ in range(2):
                                  MM(PSs.t[:, 2, :], sT[b].t[:, d_, :], vb[b].t[:], d_ == 0, d_ == 1, [sT[b], vb[b]], [PSs])
                              CP("act", OPb.t[:, li, :], PSs.t[:, 2, :], [PSs], [OPb])
                      if CSTOP < 5:
                          continue
                      TT("dve", ERD.t[:, :, 0], ER.t[:, :, 0], ER.t[:, :, 1], ALU.mult, [ER], [ERD])
                      TT("dve", ERD.t[:, :, 1], ER.t[:, :, 3], ER.t[:, :, 2], ALU.mult, [ER], [ERD])
                      orders = [list(range(NTT)), [1, 0] + list(range(NTT - 1, NTC - 1, -1))]
                      for d_ in range(2):
                          MEMSET("pool", Sst[d_].t[:], 0.0, [Sst[d_]])
                      for step in range(NTT):
                          for d_ in range(2):
                              i = orders[d_][step]
                              er_col = 0 if d_ == 0 else 3
                              if i >= NTC and step > 0:
                                  li = i - NTC
                                  TS("dve" if d_ == 0 else "pool", Smid[d_].t[:], Sst[d_].t[:], ER.t[:, i, er_col:er_col + 1], None, ALU.mult, None, [Sst[d_], ER], [Smid[d_]])
                                  MM(PD.t[:, 2 + d_, :], QTs.t[:, d_, li, :], Smid[d_].t[:], True, True, [QTs, Smid[d_]], [PD])
                                  TT("dve", OPb.t[:, li, :], OPb.t[:, li, :], PD.t[:, 2 + d_, :], ALU.add, [OPb, PD], [OPb])
                              STT("dve", Sst[d_].t[:], Sst[d_].t[:], ERD.t[:, i, d_:d_ + 1], DS.t[:, d_, i, :], ALU.mult, ALU.add, [Sst[d_], ERD, DS], [Sst[d_]])
                      if CSTOP < 6:
                          continue
                      ACT(o2.t[:], OPb.t[:].rearrange("p a k -> p (a k)"), AF.Square, [OPb], [o2])
                      RED("dve", ssq.t[:], o2.t[:].rearrange("p (a k) -> p a k", k=128), ALU.add, [o2], [ssq])
                      ACT(rs16.t[:], ssq.t[:], AF.Sqrt, [ssq, EPSb], [rs16], scale=1.0 / 128, bias=EPSb.t[:])
                      RECIP(rs16.t[:], rs16.t[:], [rs16], [rs16])
                      for li in range(NT):
                          STT("dve", on.t[:], OPb.t[:, li, :], rs16.t[:, li:li + 1], SGS.t[:, li, :], ALU.mult, ALU.mult, [OPb, rs16, SGS], [on])
                          TT("pool", onb.t[:], on.t[:], ongb.t[:], ALU.mult, [on, ongb], [onb])
                          TR(PTo.t[:, li % 4, :], onb.t[:], identb, [onb, CB], [PTo])
                          CP("act" if li % 2 else "dve", OGh.t[:, li * 128:(li + 1) * 128], PTo.t[:, li % 4, :], [PTo], [OGh])
                      DMA("sp", OGT[h * 128:(h + 1) * 128, :], OGh.t[:], [OGh], [P.reg("OGT")])
                  END_PHASE()

          def post_mixer(l, wmat, hin, dbg_name):
              with ExitStack() as es:
                  atl = [SB(es, f"atl{i}", [128, 16, 128], BF16) for i in range(2)]
                  OGTv = OGT.rearrange("(kc p) t -> p kc t", p=128)
                  wo = SB(es, "wo", [128, 16, D], BF16)
                  G1 = SB(es, "G1", [128, D]); A2 = SB(es, "A2", [128, D]); B2 = SB(es, "B2", [128, D])
                  WR = SB(es, "WR", [128, 16, 72]); BR = SB(es, "BR", [128, 72])
                  xt = [SB(es, f"pxt{i}", [128, D]) for i in range(2)]
                  hn = [SB(es, f"phn{i}", [128, D]) for i in range(2)]
                  bfl = SB(es, "pbfl", [128, D]); bb = [SB(es, f"pbb{i}", [128, D], BF16) for i in range(2)]
                  ss = SB(es, "pss", [128, 1]); rstd = SB(es, "prstd", [128, 1])
                  bT = SB(es, "pbT", [128, 16, 128])
                  As = SB(es, "pAs", [128, NT, 64], BF16)
                  lg = SB(es, "plg", [128, 72]); sm = SB(es, "psm", [128, 40]); oh1 = SB(es, "poh1", [128, 8]); e1 = SB(es, "pe1", [128, 8])
                  sel = SB(es, "psel", [128, 8]); sel2 = SB(es, "psel2", [128, 8]); oha = SB(es, "poha", [128, 8]); ohb = SB(es, "pohb", [128, 8])
                  Aa = SB(es, "pAa", [128, 64]); Ab = SB(es, "pAb", [128, 64]); Af = SB(es, "pAf", [128, 64]); t64 = SB(es, "pt64", [128, 64])
                  sidx = [SB(es, f"psidx{i}", [128, 2], I32) for i in range(2)]
                  PY = [PS(es, f"PY{i}", [128, 512]) for i in range(4)]
                  PTf = [PS(es, f"PTf{i}", [128, 4, 128]) for i in range(2)]
                  PL = PS(es, "PL", [128, 512]); PR = PS(es, "PRk", [128, 512])
                  DMA("pool", wo.t[:], wmat.rearrange("(kc p) n -> p kc n", p=128), [], [wo])
                  DMA("sp", G1.t[:], DER[l, :, 2, :], [], [G1]); DMA("sp", B2.t[:], DER[l, :, 3, :], [], [B2]); DMA("sp", A2.t[:], DER[l, :, 4, :], [], [A2])
                  DMA("sp", WR.t[:], wr[l].rearrange("(kc p) n -> p kc n", p=128), [], [WR])
                  DMA("act", BR.t[:], brr[l:l + 1, :].partition_broadcast(128), [], [BR])
                  c = lambda k: sm.t[:, k:k + 1]
                  for i in range(NT):
                      b = i % 2
                      x_ = xt[b]; h_ = hn[b]
                      DMA("sp", x_.t[:], hin[i * 128:(i + 1) * 128, :], [P.reg("HIN", i)], [x_])
                      at_ = atl[b]
                      DMA("sp", at_.t[:], OGTv[:, :, i * 128:(i + 1) * 128], [P.reg("OGT")], [at_])
                      for ch in range(4):
                          for kc in range(16):
                              MM(PY[ch].t[:], at_.t[:, kc, :], wo.t[:, kc, ch * 512:(ch + 1) * 512], kc == 0, kc == 15, [at_, wo], [PY[ch]])
                          TT("dve", h_.t[:, ch * 512:(ch + 1) * 512], PY[ch].t[:], G1.t[:, ch * 512:(ch + 1) * 512], ALU.mult, [PY[ch], G1], [h_])
                      TT("pool", h_.t[:], h_.t[:], x_.t[:], ALU.add, [h_, x_], [h_])
                      DMA("sp", Hs[i * 128:(i + 1) * 128, :], h_.t[:], [h_], [P.reg("HS", i)])
                      if debug:
                          DMA("sp", dbg[dbg_name][i * 128:(i + 1) * 128, :], h_.t[:], [h_], [P.reg("DBG", i)])
                      rms_rstd(h_.t[:], ss, rstd, bb[b], [h_])
                      ACT(bfl.t[:], h_.t[:], AF.Copy, [h_, rstd], [bfl], scale=rstd.t[:])
                      TT("dve", bfl.t[:], bfl.t[:], A2.t[:], ALU.mult, [bfl, A2], [bfl])
                      TT("pool", bfl.t[:], bfl.t[:], B2.t[:], ALU.add, [bfl, B2], [bfl])
                      CP("act", bb[b].t[:], bfl.t[:], [bfl], [bb[b]])
                      for q4 in range(4):
                          p_ = PTf[q4 % 2]
                          for k in range(4):
                              kc = q4 * 4 + k
                              TR(p_.t[:, k, :], bfl.t[:, kc * 128:(kc + 1) * 128], identf, [bfl, CF], [p_])
                          CP("dve" if q4 % 2 == 0 else "act", bT.t[:, q4 * 4:(q4 + 1) * 4, :], p_.t[:], [p_], [bT])
                      for kc in range(16):
                          MM(PL.t[:, 0:72], bT.t[:, kc, :], WR.t[:, kc, :], kc == 0, kc == 15, [bT, WR], [PL])
                      TT("dve", lg.t[:], PL.t[:, 0:72], BR.t[:], ALU.add, [PL, BR], [lg])
                      RED("dve", c(0), lg.t[:, 0:8], ALU.max, [lg], [sm])
                      TS("dve", oh1.t[:], lg.t[:, 0:8], c(0), None, ALU.is_equal, None, [lg, sm], [oh1])
                      TS("dve", c(1), c(0), -1.0, None, ALU.mult, None, [sm], [sm])
                      ACT(e1.t[:], lg.t[:, 0:8], AF.Exp, [lg, sm], [e1, sm], bias=c(1), accum_out=c(2))
                      RECIP(c(3), c(2), [sm], [sm])
                      MEMSET("pool", sel.t[:], 0.0, [sel])
                      for g in range(8):
                          STT("dve", sel.t[:], lg.t[:, 8 + g * 8:16 + g * 8], oh1.t[:, g:g + 1], sel.t[:], ALU.mult, ALU.add, [lg, oh1, sel], [sel])
                      RED("dve", c(4), sel.t[:], ALU.max, [sel], [sm])
                      TS("dve", oha.t[:], sel.t[:], c(4), None, ALU.is_equal, None, [sel, sm], [oha])
                      STT("dve", sel2.t[:], oha.t[:], -1e30, sel.t[:], ALU.mult, ALU.add, [oha, sel], [sel2])
                      RED("dve", c(6), sel2.t[:], ALU.max, [sel2], [sm])
                      TS("dve", ohb.t[:], sel2.t[:], c(6), None, ALU.is_equal, None, [sel2, sm], [ohb])
                      TS("dve", c(5), c(4), -1.0, None, ALU.mult, None, [sm], [sm])
                      ACT(c(7), c(6), AF.Exp, [sm], [sm], bias=c(5))
                      TS("dve", c(8), c(7), 1.0, None, ALU.add, None, [sm], [sm])
                      RECIP(c(8), c(8), [sm], [sm])
                      TT("dve", c(9), c(3), c(8), ALU.mult, [sm], [sm])
                      TT("dve", c(10), c(9), c(7), ALU.mult, [sm], [sm])
                      TT("dve", t64.t[:, 0:8], oh1.t[:], IOT.t[:, 0:8], ALU.mult, [oh1, IOT], [t64]); RED("dve", c(11), t64.t[:, 0:8], ALU.add, [t64], [sm])
                      TT("dve", t64.t[:, 0:8], oha.t[:], IOT.t[:, 0:8], ALU.mult, [oha, IOT], [t64]); RED("dve", c(12), t64.t[:, 0:8], ALU.add, [t64], [sm])
                      TT("dve", t64.t[:, 0:8], ohb.t[:], IOT.t[:, 0:8], ALU.mult, [ohb, IOT], [t64]); RED("dve", c(13), t64.t[:, 0:8], ALU.add, [t64], [sm])
                      STT("dve", c(14), c(11), 8.0, c(12), ALU.mult, ALU.add, [sm], [sm])
                      STT("dve", c(15), c(11), 8.0, c(13), ALU.mult, ALU.add, [sm], [sm])
                      TS("dve", Aa.t[:], IOT.t[:, 0:64], c(14), None, ALU.is_equal, None, [IOT, sm], [Aa])
                      TS("dve", Ab.t[:], IOT.t[:, 0:64], c(15), None, ALU.is_equal, None, [IOT, sm], [Ab])
                      TT("dve", Af.t[:], Aa.t[:], Ab.t[:], ALU.add, [Aa, Ab], [Af])
                      CP("dve", As.t[:, i, :], Af.t[:], [Af], [As])
                      for j in range(i + 1):
                          MM(PR.t[:, 0:64], UTs if j == i else onesb, As.t[:, j, :], j == 0, j == i, [As, CB], [PR])
                      TT("dve", t64.t[:], PR.t[:, 0:64], Aa.t[:], ALU.mult, [PR, Aa], [t64]); RED("dve", c(16), t64.t[:], ALU.add, [t64], [sm])
                      TT("dve", t64.t[:], PR.t[:, 0:64], Ab.t[:], ALU.mult, [PR, Ab], [t64]); RED("dve", c(17), t64.t[:], ALU.add, [t64], [sm])
                      for k in range(2):
                          rk = c(16 + k); ok = c(18 + k); sg_ = c(20 + k); ssc = c(22 + k); ek = c(14 + k); gk = c(9 + k)
                          TS("dve", ok, rk, float(CAP) - 0.5, None, ALU.is_lt, None, [sm], [sm])
                          TS("dve", c(24), rk, float(CAP - 1), None, ALU.min, None, [sm], [sm])
                          STT("dve", sg_, ek, float(CAP), c(24), ALU.mult, ALU.add, [sm], [sm])
                          TT("dve", c(25), sg_, IOT.t[:, 64:65], ALU.subtract, [sm, IOT], [sm])
                          TT("dve", c(25), c(25), ok, ALU.mult, [sm], [sm])
                          TT("dve", ssc, c(25), IOT.t[:, 64:65], ALU.add, [sm, IOT], [sm])
                          TT("dve", GATES.t[:, i, k:k + 1], gk, ok, ALU.mult, [sm], [GATES])
                      CP("dve", SLOTG.t[:, i, :], sm.t[:, 20:22], [sm], [SLOTG])
                      CP("dve", sidx[b].t[:], sm.t[:, 22:24], [sm], [sidx[b]])
                      for k in range(2):
                          P.dma("pool", lambda e, b=b, k=k: e.indirect_dma_start(
                              out=Xs[:, :], out_offset=bass.IndirectOffsetOnAxis(ap=sidx[b].t[:, k:k + 1], axis=0),
                              in_=bb[b].t[:], in_offset=None),
                              rr([bb[b], sidx[b]]), [P.reg("XS")])
                  END_PHASE()

          def experts(l):
              with ExitStack() as es:
                  WG = [SB(es, f"WG{i}", [128, 16, 512], BF16) for i in range(2)]
                  WU = [SB(es, f"WU{i}", [128, 16, 512], BF16) for i in range(2)]
                  WD = [SB(es, f"WD{i}", [128, 4, D], BF16) for i in range(2)]
                  xe = [SB(es, f"xe{i}", [128, 2, D], BF16) for i in range(2)]
                  xeT = [SB(es, f"xeT{i}", [128, 2, 16, 128], BF16) for i in range(2)]
                  sgl = [SB(es, f"sgl{i}", [128, 512]) for i in range(2)]
                  hh = [SB(es, f"hh{i}", [128, 512], BF16) for i in range(2)]
                  hT = [SB(es, f"hT{i}", [128, 4, 128], BF16) for i in range(2)]
                  ye = [SB(es, f"ye{i}", [128, D], BF16) for i in range(2)]
                  PX = [PS(es, f"PX{i}", [128, 8, 128], BF16) for i in range(2)]
                  PG = PS(es, "PG", [128, 512]); PU = PS(es, "PU", [128, 512])
                  PH = PS(es, "PH", [128, 8, 128], BF16)
                  PYe = [PS(es, f"PYe{i}", [128, 512]) for i in range(2)]

                  def ldw(e):
                      b = e % 2
                      DMA("pool", WG[b].t[:], wg[l, e].rearrange("(kc p) n -> p kc n", p=128), [], [WG[b]])
                      DMA("pool", WU[b].t[:], wu[l, e].rearrange("(kc p) n -> p kc n", p=128), [], [WU[b]])
                      DMA("pool", WD[b].t[:], wd[l, e].rearrange("(kc p) n -> p kc n", p=128), [], [WD[b]])
                  ldw(0)
                  cnt = 0
                  for e in range(NE):
                      b = e % 2
                      if e + 1 < NE:
                          ldw(e + 1)
                      DMA("sp", xe[b].t[:], Xs[e * CAP:(e + 1) * CAP, :].rearrange("(hf p) d -> p hf d", p=128), [P.reg("XS")], [xe[b]])
                      for hf in range(2):
                          for q in range(2):
                              p_ = PX[q]
                              for k in range(8):
                                  kc = q * 8 + k
                                  TR(p_.t[:, k, :], xe[b].t[:, hf, kc * 128:(kc + 1) * 128], identb, [xe[b], CB], [p_])
                              CP("dve" if q == 0 else "act", xeT[b].t[:, hf, q * 8:(q + 1) * 8, :], p_.t[:], [p_], [xeT[b]])
                      for hf in range(2):
                          u = cnt % 2; cnt += 1
                          for kc in range(16):
                              MM(PG.t[:], xeT[b].t[:, hf, kc, :], WG[b].t[:, kc, :], kc == 0, kc == 15, [xeT[b], WG[b]], [PG])
                          for kc in range(16):
                              MM(PU.t[:], xeT[b].t[:, hf, kc, :], WU[b].t[:, kc, :], kc == 0, kc == 15, [xeT[b], WU[b]], [PU])
                          ACT(sgl[u].t[:], PG.t[:], AF.Silu, [PG], [sgl[u]])
                          TT("dve", hh[u].t[:], sgl[u].t[:], PU.t[:], ALU.mult, [sgl[u], PU], [hh[u]])
                          for k in range(4):
                              TR(PH.t[:, k, :], hh[u].t[:, k * 128:(k + 1) * 128], identb, [hh[u], CB], [PH])
                          CP("dve", hT[u].t[:], PH.t[:, 0:4, :], [PH], [hT[u]])
                          for ch in range(4):
                              py = PYe[ch % 2]
                              for k in range(4):
                                  MM(py.t[:], hT[u].t[:, k, :], WD[b].t[:, k, ch * 512:(ch + 1) * 512], k == 0, k == 3, [hT[u], WD[b]], [py])
                              CP("act" if ch % 2 == 0 else "dve", ye[u].t[:, ch * 512:(ch + 1) * 512], py.t[:], [py], [ye[u]])
                          r0 = e * CAP + hf * 128
                          DMA("sp", Ys[r0:r0 + 128, :], ye[u].t[:], [ye[u]], [P.reg("YS")])
                  END_PHASE()

          def combine(l, aT1, dbg_name):
              with ExitStack() as es:
                  G2 = SB(es, "G2", [128, D]); A1 = SB(es, "cA1", [128, D]); B1 = SB(es, "cB1", [128, D])
                  Y0 = [SB(es, f"Y0{i}", [128, D], BF16) for i in range(2)]
                  Y1 = [SB(es, f"Y1{i}", [128, D], BF16) for i in range(2)]
                  hn = [SB(es, f"chn{i}", [128, D]) for i in range(2)]
                  z = SB(es, "cz", [128, D]); h2 = [SB(es, f"ch2{i}", [128, D]) for i in range(2)]
                  ss = SB(es, "css", [128, 1]); rstd = SB(es, "crstd", [128, 1]); junk = SB(es, "cjunk", [128, D], BF16)
                  xn = SB(es, "cxn", [128, D]); ab_ = SB(es, "cab", [128, D], BF16)
                  pt = [PS(es, f"cpt{i}", [128, 8, 128], BF16) for i in range(2)]
                  DMA("sp", G2.t[:], DER[l, :, 5, :], [], [G2])
                  if l == 0:
                      DMA("sp", B1.t[:], DER[1, :, 0, :], [], [B1]); DMA("sp", A1.t[:], DER[1, :, 1, :], [], [A1])
                  else:
                      DMA("act", A1.t[:], fing[0:1, :].partition_broadcast(128), [], [A1])
                  for i in range(NT):
                      b = i % 2
                      for k, Yk in enumerate([Y0[b], Y1[b]]):
                          P.dma("pool", lambda e, Yk=Yk, i=i, k=k: e.indirect_dma_start(
                              out=Yk.t[:], out_offset=None, in_=Ys[:, :],
                              in_offset=bass.IndirectOffsetOnAxis(ap=SLOTG.t[:, i, k:k + 1], axis=0)),
                              rr([SLOTG, P.reg("YS")]), rr([Yk]))
                      DMA("sp", hn[b].t[:], Hs[i * 128:(i + 1) * 128, :], [P.reg("HS", i)], [hn[b]])
                      TS("dve", z.t[:], Y0[b].t[:], GATES.t[:, i, 0:1], None, ALU.mult, None, [Y0[b], GATES], [z])
                      STT("dve", z.t[:], Y1[b].t[:], GATES.t[:, i, 1:2], z.t[:], ALU.mult, ALU.add, [Y1[b], GATES, z], [z])
                      TT("dve", z.t[:], z.t[:], G2.t[:], ALU.mult, [z, G2], [z])
                      TT("pool", h2[b].t[:], z.t[:], hn[b].t[:], ALU.add, [z, hn[b]], [h2[b]])
                      if debug and dbg_name in dbg:
                          DMA("sp", dbg[dbg_name][i * 128:(i + 1) * 128, :], h2[b].t[:], [h2[b]], [P.reg("DBG", i)])
                      if l == 0:
                          DMA("sp", Hs[i * 128:(i + 1) * 128, :], h2[b].t[:], [h2[b]], [P.reg("HS", i)])
                          norm_mod_transpose(es, h2[b], A1, B1, aT1, i * 128, (ss, rstd, junk, xn, ab_, pt))
                      else:
                          rms_rstd(h2[b].t[:], ss, rstd, junk, [h2[b]])
                          ACT(xn.t[:], h2[b].t[:], AF.Copy, [h2[b], rstd], [xn], scale=rstd.t[:])
                          TT("dve", z.t[:], xn.t[:], A1.t[:], ALU.mult, [xn, A1], [z])
                          DMA("sp", out[i * 128:(i + 1) * 128, :], z.t[:], [z], [P.reg("OUT", i)])
                  END_PHASE()

          post_mixer(0, w_out, x, "hmid0")
          experts(0)
          with ExitStack() as esM:
              MEAN = SB(esM, "MEAN", [128, T]); RSTD = SB(esM, "RSTD", [128, T])
              with ExitStack() as esG:
                  aT1 = SB(esG, "aT1", [128, 16, T], BF16)
                  combine(0, aT1, "hend0")
                  with ExitStack() as es:
                      wv = [SB(es, f"wv{i}", [128, 16, 128], BF16) for i in range(2)]
                      wgt = [SB(es, f"wgt{i}", [128, 16, 128], BF16) for i in range(2)]
                      WDW = SB(es, "WDW", [128, 16, 31]); BDW = SB(es, "BDW", [128, 16])
                      sgm = [SB(es, f"sgm{i}", [128, 512]) for i in range(2)]
                      uT = [SB(es, f"uT{i}", [128, T]) for i in range(2)]
                      acc = [SB(es, f"acc{i}", [128, T]) for i in range(2)]
                      v2 = SB(es, "v2", [128, T]); MQ = SB(es, "MQ", [128, T])
                      PV = [PS(es, f"PV{i}", [128, 512]) for i in range(2)]
                      PGt = [PS(es, f"PGt{i}", [128, 512]) for i in range(2)]
                      PS1 = [PS(es, f"PS1{i}", [128, 512]) for i in range(2)]
                      DMA("sp", WDW.t[:], wdw[:, :, :], [], [WDW]); DMA("sp", BDW.t[:], bdw[:, :], [], [BDW])
                      pwv = w_pw1.rearrange("(kc p) n -> p kc n", p=128)

                      def ldcw(cc_):
                          DMA("pool", wv[cc_ % 2].t[:], pwv[:, :, cc_ * 128:(cc_ + 1) * 128], [], [wv[cc_ % 2]])
                          DMA("pool", wgt[cc_ % 2].t[:], pwv[:, :, D + cc_ * 128:D + (cc_ + 1) * 128], [], [wgt[cc_ % 2]])
                      ldcw(0)
                      MEMSET("pool", MEAN.t[:], 0.0, [MEAN]); MEMSET("pool", MQ.t[:], 0.0, [MQ])
                      for cc_ in range(16):
                          b = cc_ % 2
                          if cc_ + 1 < 16:
                              ldcw(cc_ + 1)
                          for tq in range(4):
                              pv = PV[tq % 2]; pg = PGt[tq % 2]
                              for kc in range(16):
                                  MM(pv.t[:], wv[b].t[:, kc, :], aT1.t[:, kc, tq * 512:(tq + 1) * 512], kc == 0, kc == 15, [wv[b], aT1], [pv])
                              for kc in range(16):
                                  MM(pg.t[:], wgt[b].t[:, kc, :], aT1.t[:, kc, tq * 512:(tq + 1) * 512], kc == 0, kc == 15, [wgt[b], aT1], [pg])
                              ACT(sgm[tq % 2].t[:], pg.t[:], AF.Sigmoid, [pg], [sgm[tq % 2]])
                              TT("dve", uT[b].t[:, tq * 512:(tq + 1) * 512], pv.t[:], sgm[tq % 2].t[:], ALU.mult, [pv, sgm[tq % 2]], [uT[b]])
                          eng = "dve"
                          u_ = uT[b]; a_ = acc[b]
                          TS(eng, a_.t[:], u_.t[:], WDW.t[:, cc_, 15:16], BDW.t[:, cc_:cc_ + 1], ALU.mult, ALU.add, [u_, WDW, BDW], [a_])
                          for j in range(31):
                              s = j - 15
                              if s == 0:
                                  continue
                              wj = WDW.t[:, cc_, j:j + 1]
                              if cc_ < 8:
                                  c0 = max(0, -s); c1 = min(64, 64 - s)
                                  a3 = a_.t[:].rearrange("p (r c) -> p r c", c=64); u3 = u_.t[:].rearrange("p (r c) -> p r c", c=64)
                                  STT(eng, a3[:, :, c0:c1], u3[:, :, c0 + s:c1 + s], wj, a3[:, :, c0:c1], ALU.mult, ALU.add, [u_, WDW, a_], [a_])
                              else:
                                  r0 = max(0, -s); r1 = min(32, 32 - s)
                                  STT(eng, a_.t[:, r0 * 64:r1 * 64], u_.t[:, (r0 + s) * 64:(r1 + s) * 64], wj, a_.t[:, r0 * 64:r1 * 64], ALU.mult, ALU.add, [u_, WDW, a_], [a_])
                          DMA("sp", Vd[cc_ * 128:(cc_ + 1) * 128, :], a_.t[:], [a_], [P.reg("VD")])
                          ACT(v2.t[:], a_.t[:], AF.Square, [a_], [v2])
                          for tq in range(4):
                              MM(PS1[0].t[:], onesf, a_.t[:, tq * 512:(tq + 1) * 512], True, True, [CF, a_], [PS1[0]])
                              TT("dve", MEAN.t[:, tq * 512:(tq + 1) * 512], MEAN.t[:, tq * 512:(tq + 1) * 512], PS1[0].t[:], ALU.add, [MEAN, PS1[0]], [MEAN])
                              MM(PS1[1].t[:], onesf, v2.t[:, tq * 512:(tq + 1) * 512], True, True, [CF, v2], [PS1[1]])
                              TT("dve", MQ.t[:, tq * 512:(tq + 1) * 512], MQ.t[:, tq * 512:(tq + 1) * 512], PS1[1].t[:], ALU.add, [MQ, PS1[1]], [MQ])
                      TS("dve", MEAN.t[:], MEAN.t[:], 1.0 / D, None, ALU.mult, None, [MEAN], [MEAN])
                      TT("dve", v2.t[:], MEAN.t[:], MEAN.t[:], ALU.mult, [MEAN], [v2])
                      STT("dve", MQ.t[:], MQ.t[:], 1.0 / D, v2.t[:], ALU.mult, ALU.subtract, [MQ, v2], [MQ])
                      ACT(RSTD.t[:], MQ.t[:], AF.Sqrt, [MQ, EPSb], [RSTD], bias=EPSb.t[:])
                      RECIP(RSTD.t[:], RSTD.t[:], [RSTD], [RSTD])
                      END_PHASE()
              with ExitStack() as es:
                  LNG = SB(es, "LNG", [128, 16]); LNB = SB(es, "LNB", [128, 16])
                  vt = [SB(es, f"vt{i}", [128, T]) for i in range(2)]
                  sTc = [SB(es, f"sTc{i}", [128, T], BF16) for i in range(2)]
                  DMA("sp", LNG.t[:], lng[:, :], [], [LNG]); DMA("sp", LNB.t[:], lnb[:, :], [], [LNB])
                  for cc_ in range(16):
                      b = cc_ % 2
                      DMA("sp", vt[b].t[:], Vd[cc_ * 128:(cc_ + 1) * 128, :], [P.reg("VD")], [vt[b]])
                      TT("dve", vt[b].t[:], vt[b].t[:], MEAN.t[:], ALU.subtract, [vt[b], MEAN], [vt[b]])
                      TT("pool", vt[b].t[:], vt[b].t[:], RSTD.t[:], ALU.mult, [vt[b], RSTD], [vt[b]])
                      ACT(sTc[b].t[:], vt[b].t[:], AF.Silu, [vt[b], LNG, LNB], [sTc[b]], scale=LNG.t[:, cc_:cc_ + 1], bias=LNB.t[:, cc_:cc_ + 1])
                      DMA("sp", OGT[cc_ * 128:(cc_ + 1) * 128, :], sTc[b].t[:], [sTc[b]], [P.reg("OGT")])
                  END_PHASE()
          post_mixer(1, w_pw2, Hs, "hmid1")
          experts(1)
          combine(1, None, "hend1")
    except _Stop:
        pass
    return nc


def _consts():
    s = np.arange(128)[:, None]; t = np.arange(128)[None, :]
    cf = np.zeros((128, 9, 128), np.float32)
    cf[:, 0] = np.eye(128)
    cf[:, 1] = (s <= t).astype(np.float32) - (s <= 63).astype(np.float32)
    cf[:, 2] = (s >= t).astype(np.float32) - (s >= 64).astype(np.float32)
    cf[:, 3] = (s > t)
    cf[:, 4] = (s < t)
    cf[:, 5] = (s <= t)
    cf[:, 6] = (s >= t)
    cf[:, 7] = 1.0
    cf[:, 8, 0] = (np.arange(128) <= 63); cf[:, 8, 1] = (np.arange(128) >= 64)
    cb = np.zeros((128, 3, 128), np.float32)
    cb[:, 0] = np.eye(128); cb[:, 1] = 1.0; cb[:, 2] = (s < t)
    iot = np.tile(np.arange(65, dtype=np.float32)[None, :], (128, 1))
    iot[:, 64] = XS_ROWS + np.arange(128)
    return cf, cb.astype(ml_dtypes.bfloat16), iot


def _col(v):
    return np.ascontiguousarray(np.asarray(v, np.float32).reshape(16, 128).T)


def kernel(x, c, ctx, c_ctx, ada_w, ada_b, norm1_g, norm2_g, hgrn_w_in, hgrn_lb, hgrn_onorm_g,
           hgrn_w_out, conv_w_pw1, conv_w_dw, conv_b_dw, conv_ln_g, conv_ln_b, conv_w_pw2,
           moe_w_r1, moe_b_r1, moe_w_r2, moe_b_r2, moe_w_gate, moe_w_up, moe_w_down, final_g, _debug=False, _stop=None, _ncores=8):
    f = lambda a: np.ascontiguousarray(np.asarray(a, np.float32))
    key = ("nc", bool(_debug), _stop)
    if key not in _cache:
        _cache[key] = build_program(debug=_debug, stop=_stop)
    nc = _cache[key]
    cf, cb, iot = _consts()
    w_r2 = np.asarray(moe_w_r2, np.float32)
    wr_ = np.concatenate([np.asarray(moe_w_r1, np.float32), w_r2.transpose(0, 2, 1, 3).reshape(2, D, 64)], axis=2)
    br_ = np.concatenate([np.asarray(moe_b_r1, np.float32), np.asarray(moe_b_r2, np.float32).reshape(2, 64)], axis=1)
    wdw_ = np.ascontiguousarray(np.asarray(conv_w_dw, np.float32)[0].T.reshape(16, 128, 31).transpose(1, 0, 2))
    shared = {
        "ada_w": f(ada_w), "ada_b": f(ada_b), "n1g": f(norm1_g), "n2g": f(norm2_g), "fing": f(final_g).reshape(1, D),
        "w_in": f(hgrn_w_in)[0], "hlb": f(hgrn_lb)[:, :, :], "ong": f(hgrn_onorm_g).reshape(1, 128), "w_out": f(hgrn_w_out)[0],
        "w_pw1": f(conv_w_pw1)[0], "wdw": wdw_, "bdw": _col(np.asarray(conv_b_dw)[0]), "lng": _col(np.asarray(conv_ln_g)[0]),
        "lnb": _col(np.asarray(conv_ln_b)[0]), "w_pw2": f(conv_w_pw2)[0],
        "wr": np.ascontiguousarray(wr_), "brr": np.ascontiguousarray(br_),
        "cst_f": cf, "cst_b": cb, "iot": iot,
    }
    if _stop is None or _stop >= 5:
        shared.update({"wg": f(moe_w_gate), "wu": f(moe_w_up), "wd": f(moe_w_down)})
    xx = f(x); cx = f(ctx); cc = np.asarray(c, np.float32); ccx = np.asarray(c_ctx, np.float32)
    in_maps = []
    for b in range(_ncores):
        m = dict(shared)
        m["x"] = xx[b]; m["ctx"] = cx[b]
        m["ccol"] = np.ascontiguousarray(np.concatenate([_col(cc[b]), _col(ccx)], axis=1))
        in_maps.append(m)
    res = run_bass_kernel_spmd(nc, in_maps, core_ids=list(range(_ncores)))
    outp = np.stack([np.asarray(r["out"]) for r in res.results], axis=0).astype(np.float32)
    if _debug:
        return outp, res.results
    return outp
```

```python
import numpy as np
import concourse.bass as bass
import concourse.mybir as mybir

F32 = mybir.dt.float32
BF16 = mybir.dt.bfloat16
I32 = mybir.dt.int32
AF = mybir.ActivationFunctionType
ALU = mybir.AluOpType
AX = mybir.AxisListType

SAME_ENG_SYNC = False


class Reg:
    __slots__ = ("name", "w", "r", "excl")

    def __init__(self, name):
        self.name = name
        self.excl = False
        self.w = None
        self.r = {}


def _tok_key(t):
    return t[0:2]


class Prog:
    ENGS = ["pe", "act", "dve", "pool", "sp"]

    def __init__(self, nc, esems, dsems):
        self.nc = nc
        self.esems = esems
        self.dsems = dsems
        self.dcount = {q: [0] * len(v) for q, v in dsems.items()}
        self.drr = {q: 0 for q in dsems}
        self.ecount = {e: 0 for e in self.ENGS}
        self.regs = {}
        self.reset_phase()

    def reg(self, *key):
        r = self.regs.get(key)
        if r is None:
            r = self.regs[key] = Reg(key)
        return r

    def reset_phase(self):
        self.ops = {e: [] for e in self.ENGS}
        for r in self.regs.values():
            r.w = None
            r.r = {}

    def _deps(self, reads, writes):
        deps = {}

        def add(t):
            k = _tok_key(t)
            if k not in deps or deps[k][2] < t[2]:
                deps[k] = t
        for r in reads:
            if r.w is not None:
                add(r.w)
        for w in writes:
            if w.w is not None:
                add(w.w)
            for t in w.r.values():
                add(t)
        return deps

    def _commit(self, tok, reads, writes):
        k = _tok_key(tok)
        for r in reads:
            r.r[k] = tok
        for w in writes:
            w.w = tok
            w.r = {}

    def op(self, eng, fn, reads=(), writes=()):
        writes = list(writes) + [r for r in reads if r.excl]
        reads = [r for r in reads if not r.excl]
        deps = self._deps(reads, writes)
        idx = len(self.ops[eng])
        tok = ("E", eng, idx)
        t = deps.get(("E", eng))
        if t is not None and (eng == "pe" or idx - t[2] > 3):
            deps.pop(("E", eng), None)
        self.ops[eng].append(dict(fn=fn, deps=list(deps.values()), dma=None, inc=False))
        self._commit(tok, reads, writes)

    def dma(self, q, fn, reads=(), writes=()):
        deps = self._deps(reads, writes)
        i = self.drr[q]
        self.drr[q] = (i + 1) % len(self.dsems[q])
        prev = self.dcount[q][i]
        if prev > 0:
            t = ("D", (q, i), prev)
            k = _tok_key(t)
            if k not in deps or deps[k][2] < prev:
                deps[k] = t
        self.dcount[q][i] = prev + 16
        tok = ("D", (q, i), prev + 16)
        self.ops[q].append(dict(fn=fn, deps=list(deps.values()), dma=(q, i), inc=False))
        self._commit(tok, reads, writes)

    def emit_phase(self, name=None):
        nc = self.nc
        finals = {e: [] for e in self.ENGS}
        for q in self.dsems:
            for i, c in enumerate(self.dcount[q]):
                if c > 0:
                    finals[q].append((self.dsems[q][i], c))
        for e in self.ENGS:
            for o in self.ops[e]:
                for t in o["deps"]:
                    if t[0] == "E":
                        self.ops[t[1]][t[2]]["inc"] = True
        cum = {}
        for e in self.ENGS:
            c = self.ecount[e]
            arr = []
            for o in self.ops[e]:
                if o["inc"] and o["dma"] is None:
                    c += 1
                arr.append(c)
            cum[e] = arr
        ops = self.ops
        esems, dsems = self.esems, self.dsems

        def run(eng_name, eng):
            waited = {}
            for o in ops[eng_name]:
                for t in o["deps"]:
                    if t[0] == "E":
                        sem = esems[t[1]]
                        val = cum[t[1]][t[2]]
                        key = ("E", t[1])
                    else:
                        sem = dsems[t[1][0]][t[1][1]]
                        val = t[2]
                        key = t[1]
                    if waited.get(key, -1) >= val:
                        continue
                    waited[key] = val
                    eng.wait_ge(sem, val)
                ins = o["fn"](eng)
                if o["dma"] is not None:
                    q, i = o["dma"]
                    ins.then_inc(dsems[q][i], 16)
                elif o["inc"]:
                    ins.then_inc(esems[eng_name], 1)
            for sem, c in finals[eng_name]:
                eng.wait_ge(sem, c)

        with nc.Block() as block:
            @block.tensor
            def _(e):
                run("pe", e)

            @block.scalar
            def _(e):
                run("act", e)

            @block.vector
            def _(e):
                run("dve", e)

            @block.gpsimd
            def _(e):
                run("pool", e)

            @block.sync
            def _(e):
                run("sp", e)
        for e in self.ENGS:
            if cum[e]:
                self.ecount[e] = cum[e][-1]
        n = {e: len(self.ops[e]) for e in self.ENGS}
        self.reset_phase()
        return n


from contextlib import ExitStack
import ml_dtypes
from concourse.bass_utils import run_bass_kernel_spmd

T = 2048; D = 2048; CT = 256; NT = 16; NTC = 2; CAP = 256; NE = 64
EPS = 1e-6
XS_ROWS = NE * CAP

_cache = {}
CSTOP = 99
LATSTOP = 99
COPYSEL = 0


class Tn:
    def __init__(self, P, t, name):
        self.t = t
        self.r = P.reg(name)


class _Stop(Exception):
    pass


def build_program(debug=False, stop=None, nheads=16):
    nc = bass.Bass("TRN2", target_bir_lowering=False)
    dr = lambda n, s, d=F32: nc.dram_tensor(n, s, d, kind="ExternalInput").ap()
    x = dr("x", [T, D]); ctx = dr("ctx", [CT, D]); ccol = dr("ccol", [128, 32])
    ada_w = dr("ada_w", [2, D, 6 * D]); ada_b = dr("ada_b", [2, 6 * D])
    n1g = dr("n1g", [2, D]); n2g = dr("n2g", [2, D]); fing = dr("fing", [1, D])
    w_in = dr("w_in", [16, D, 640]); hlb = dr("hlb", [2, 3, D]); ong = dr("ong", [1, 128]); w_out = dr("w_out", [D, D])
    w_pw1 = dr("w_pw1", [D, 2 * D]); wdw = dr("wdw", [128, 16, 31]); bdw = dr("bdw", [128, 16])
    lng = dr("lng", [128, 16]); lnb = dr("lnb", [128, 16]); w_pw2 = dr("w_pw2", [D, D])
    wr = dr("wr", [2, D, 72]); brr = dr("brr", [2, 72])
    if stop is None or stop >= 5:
        wg = dr("wg", [2, NE, D, 512]); wu = dr("wu", [2, NE, D, 512]); wd = dr("wd", [2, NE, 512, D])
    cst_f = dr("cst_f", [128, 9, 128])
    cst_b = dr("cst_b", [128, 3, 128], BF16)
    iot = dr("iot", [128, 65])
    out = nc.dram_tensor("out", [T, D], F32, kind="ExternalOutput").ap()
    Hs = nc.dram_tensor("Hs", [T, D], F32, kind=("ExternalOutput" if debug else "Internal")).ap()
    DER = nc.dram_tensor("DER", [2, 128, 6, D], F32, kind=("ExternalOutput" if debug else "Internal")).ap()
    DERC = nc.dram_tensor("DERC", [128, 2, D], F32, kind=("ExternalOutput" if debug else "Internal")).ap()
    OGT = nc.dram_tensor("OGT", [D, T], BF16, kind=("ExternalOutput" if debug else "Internal")).ap()
    Xs = nc.dram_tensor("Xs", [XS_ROWS + 128, D], BF16, kind=("ExternalOutput" if debug else "Internal")).ap()
    Ys = nc.dram_tensor("Ys", [XS_ROWS, D], BF16, kind=("ExternalOutput" if debug else "Internal")).ap()
    BBd = nc.dram_tensor("BBd", [T, D], BF16).ap()
    Vd = nc.dram_tensor("Vd", [D, T], F32, kind=("ExternalOutput" if debug else "Internal")).ap()
    dbg = {}
    if debug:
        for n in ["hmid0", "hend0", "hmid1"]:
            dbg[n] = nc.dram_tensor("dbg_" + n, [T, D], F32, kind="ExternalOutput").ap()

    try:
      with ExitStack() as es0:
          esems = {e: es0.enter_context(nc.semaphore("e_" + e)) for e in Prog.ENGS}
          dsems = {q: [es0.enter_context(nc.semaphore(f"d_{q}{i}")) for i in range(n)] for q, n in [("sp", 8), ("pool", 6), ("act", 2)]}
          P = Prog(nc, esems, dsems)
          es0.enter_context(nc.allow_low_precision("bf16 matmul operands, fp32 accumulation"))

          uid = [0]

          def SB(es, name, shape, dt=F32):
              uid[0] += 1
              name = f"{name}_{uid[0]}"
              return Tn(P, es.enter_context(nc.sbuf_tensor(name, shape, dt)), name)

          def PS(es, name, shape, dt=F32):
              uid[0] += 1
              name = f"{name}_{uid[0]}"
              t_ = Tn(P, es.enter_context(nc.psum_tensor(name, shape, dt)), name)
              t_.r.excl = True
              return t_

          phase_no = [0]

          def END_PHASE():
              P.emit_phase()
              phase_no[0] += 1
              if stop is not None and phase_no[0] >= stop:
                  raise _Stop()

          def rr(l):
              return [a.r if isinstance(a, Tn) else a for a in l]

          def ACT(out, in_, func, R, W, **kw):
              P.op("act", lambda e: e.activation(out=out, in_=in_, func=func, **kw), rr(R), rr(W))

          def TT(eng, out, in0, in1, op, R, W):
              P.op(eng, lambda e: e.tensor_tensor(out=out, in0=in0, in1=in1, op=op), rr(R), rr(W))

          def TS(eng, out, in0, s1, s2, op0, op1, R, W, **kw):
              if s2 is None:
                  P.op(eng, lambda e: e.tensor_scalar(out=out, in0=in0, scalar1=s1, scalar2=None, op0=op0, **kw), rr(R), rr(W))
              else:
                  P.op(eng, lambda e: e.tensor_scalar(out=out, in0=in0, scalar1=s1, scalar2=s2, op0=op0, op1=op1, **kw), rr(R), rr(W))

          def STT(eng, out, in0, sc, in1, op0, op1, R, W):
              P.op(eng, lambda e: e.scalar_tensor_tensor(out=out, in0=in0, scalar=sc, in1=in1, op0=op0, op1=op1), rr(R), rr(W))

          def CP(eng, out, in_, R, W):
              if eng == "act":
                  P.op("act", lambda e: e.copy(out=out, in_=in_), rr(R), rr(W))
              else:
                  P.op(eng, lambda e: e.tensor_copy(out=out, in_=in_), rr(R), rr(W))

          def MM(out, lhsT, rhs, start, stop, R, W):
              P.op("pe", lambda e: e.matmul(out, lhsT=lhsT, rhs=rhs, start=start, stop=stop), rr(R), rr(W))

          def TR(out, in_, ident, R, W):
              P.op("pe", lambda e: e.transpose(out=out, in_=in_, identity=ident), rr(R), rr(W))

          def DMA(q, out, in_, R, W):
              P.dma(q, lambda e: e.dma_start(out=out, in_=in_), rr(R), rr(W))

          def RED(eng, out, in_, op, R, W):
              P.op(eng, lambda e: e.tensor_reduce(out=out, in_=in_, axis=AX.X, op=op), rr(R), rr(W))

          def RECIP(out, in_, R, W):
              P.op("dve", lambda e: e.reciprocal(out=out, in_=in_), rr(R), rr(W))

          def MEMSET(eng, ap, v, W):
              P.op(eng, lambda e: e.memset(ap, v), [], rr(W))

          CF = SB(es0, "CF", [128, 9, 128]); CB = SB(es0, "CB", [128, 3, 128], BF16)
          IOT = SB(es0, "IOT", [128, 65]); EPSb = SB(es0, "EPSb", [128, 1])
          SLOTG = SB(es0, "SLOTG", [128, NT, 2], I32); GATES = SB(es0, "GATES", [128, NT, 2])
          identf = CF.t[:, 0, :]; LcT = [CF.t[:, 1, :], CF.t[:, 2, :]]; E2T = [CF.t[:, 3, :], CF.t[:, 4, :]]
          onesf = CF.t[:, 7, :]; IND = CF.t[:, 8, 0:2]
          identb = CB.t[:, 0, :]; onesb = CB.t[:, 1, :]; UTs = CB.t[:, 2, :]

          def load_consts():
              DMA("sp", CF.t[:], cst_f[:, :, :], [], [CF])
              DMA("sp", CB.t[:], cst_b[:, :, :], [], [CB])
              DMA("sp", IOT.t[:], iot[:, :], [], [IOT])
              MEMSET("pool", EPSb.t[:], EPS, [EPSb])

          def rms_rstd(xt_ap, ss, rstd, junk, R, n=D):
              ACT(junk.t[:, 0:n], xt_ap, AF.Square, R, [junk, ss], accum_out=ss.t[:])
              ACT(rstd.t[:], ss.t[:], AF.Sqrt, [ss, EPSb], [rstd], scale=1.0 / n, bias=EPSb.t[:])
              RECIP(rstd.t[:], rstd.t[:], [rstd], [rstd])

          with ExitStack() as es:
              load_consts()
              cc = SB(es, "cc", [128, 32]); sc = SB(es, "sc", [128, 32])
              Srep = SB(es, "Srep", [128, 32, 128])
              wa = [SB(es, f"wa{i}", [128, 16, 512]) for i in range(2)]
              ab = [SB(es, f"ab{i}", [128, 512]) for i in range(2)]
              MODt = SB(es, "MODt", [128, 6 * D]); MODc = SB(es, "MODc", [128, 2 * D])
              gt = [SB(es, f"gt{i}", [128, D]) for i in range(2)]
              pa = [PS(es, f"pa{i}", [128, 512]) for i in range(2)]
              pc = [PS(es, f"pc{i}", [128, 512]) for i in range(2)]
              DMA("sp", cc.t[:], ccol[:, :], [], [cc])
              ACT(sc.t[:], cc.t[:], AF.Silu, [cc], [sc])
              for k in range(32):
                  ACT(Srep.t[:, k, :], onesf, AF.Copy, [CF, sc], [Srep], scale=sc.t[:, k:k + 1])
              for l in range(2):
                  awv = ada_w[l].rearrange("(kc p) n -> p kc n", p=128)
                  for j in range(24):
                      w_ = wa[j % 2]; a_ = ab[j % 2]; p_ = pa[j % 2]; q_ = pc[j % 2]
                      DMA("sp", w_.t[:], awv[:, :, j * 512:(j + 1) * 512], [], [w_])
                      DMA("act", a_.t[:], ada_b[l:l + 1, j * 512:(j + 1) * 512].partition_broadcast(128), [], [a_])
                      for kc in range(16):
                          MM(p_.t[:], Srep.t[:, kc, :], w_.t[:, kc, :], kc == 0, kc == 15, [Srep, w_], [p_])
                      TT("dve", MODt.t[:, j * 512:(j + 1) * 512], p_.t[:], a_.t[:], ALU.add, [p_, a_], [MODt])
                      if l == 0 and j < 8:
                          for kc in range(16):
                              MM(q_.t[:], Srep.t[:, 16 + kc, :], w_.t[:, kc, :], kc == 0, kc == 15, [Srep, w_], [q_])
                          TT("dve", MODc.t[:, j * 512:(j + 1) * 512], q_.t[:], a_.t[:], ALU.add, [q_, a_], [MODc])
                  DMA("act", gt[0].t[:], n1g[l:l + 1, :].partition_broadcast(128), [], [gt[0]])
                  DMA("act", gt[1].t[:], n2g[l:l + 1, :].partition_broadcast(128), [], [gt[1]])
                  STT("dve", MODt.t[:, D:2 * D], MODt.t[:, D:2 * D], 1.0, gt[0].t[:], ALU.add, ALU.mult, [MODt, gt[0]], [MODt])
                  STT("dve", MODt.t[:, 4 * D:5 * D], MODt.t[:, 4 * D:5 * D], 1.0, gt[1].t[:], ALU.add, ALU.mult, [MODt, gt[1]], [MODt])
                  DMA("sp", DER[l].rearrange("p s d -> p (s d)"), MODt.t[:], [MODt], [P.reg("DER")])
                  if l == 0:
                      STT("dve", MODc.t[:, D:2 * D], MODc.t[:, D:2 * D], 1.0, gt[0].t[:], ALU.add, ALU.mult, [MODc, gt[0]], [MODc])
                      DMA("sp", DERC.rearrange("p s d -> p (s d)"), MODc.t[:], [MODc], [P.reg("DERC")])
              END_PHASE()

          def norm_mod_transpose(es, xt, A1, B1, dstT, col0, bufs):
              ss, rstd, junk, xn, ab_, pt = bufs
              rms_rstd(xt.t[:], ss, rstd, junk, [xt])
              ACT(xn.t[:], xt.t[:], AF.Copy, [xt, rstd], [xn], scale=rstd.t[:])
              TT("dve", xn.t[:], xn.t[:], A1.t[:], ALU.mult, [xn, A1], [xn])
              TT("pool", ab_.t[:], xn.t[:], B1.t[:], ALU.add, [xn, B1], [ab_])
              for hf in range(2):
                  p_ = pt[hf]
                  for k in range(8):
                      kc = hf * 8 + k
                      TR(p_.t[:, k, :], ab_.t[:, kc * 128:(kc + 1) * 128], identb, [ab_, CB], [p_])
                  CP("dve" if hf == 0 else "act", dstT.t[:, hf * 8:(hf + 1) * 8, col0:col0 + 128], p_.t[:], [p_], [dstT])

          with ExitStack() as esBC:
              aT = SB(esBC, "aT", [128, 16, CT + T], BF16)
              with ExitStack() as es:
                  A1 = SB(es, "A1", [128, D]); B1 = SB(es, "B1", [128, D]); A1c = SB(es, "A1c", [128, D]); B1c = SB(es, "B1c", [128, D])
                  xt = [SB(es, f"xt{i}", [128, D]) for i in range(2)]
                  ss = SB(es, "ss", [128, 1]); rstd = SB(es, "rstd", [128, 1]); junk = SB(es, "junk", [128, D], BF16)
                  xn = SB(es, "xn", [128, D]); ab_ = SB(es, "abf", [128, D], BF16)
                  pt = [PS(es, f"pt{i}", [128, 8, 128], BF16) for i in range(2)]
                  DMA("sp", B1.t[:], DER[0, :, 0, :], [], [B1]); DMA("sp", A1.t[:], DER[0, :, 1, :], [], [A1])
                  DMA("sp", B1c.t[:], DERC[:, 0, :], [], [B1c]); DMA("sp", A1c.t[:], DERC[:, 1, :], [], [A1c])
                  for i in range(NTC + NT):
                      x_ = xt[i % 2]
                      src = ctx[i * 128:(i + 1) * 128, :] if i < NTC else x[(i - NTC) * 128:(i - NTC + 1) * 128, :]
                      DMA("sp", x_.t[:], src, [], [x_])
                      norm_mod_transpose(es, x_, A1c if i < NTC else A1, B1c if i < NTC else B1, aT, i * 128, (ss, rstd, junk, xn, ab_, pt))
                  END_PHASE()

              with ExitStack() as es:
                  NTT = NTC + NT
                  wsl = [SB(es, f"wsl{i}", [128, 16, 640], BF16) for i in range(2)]
                  lbr = SB(es, "lbr", [128, 2, 3, 128])
                  lb2 = [SB(es, "lb2", [128, 2, 128])] * 2; oml2 = [SB(es, "oml2", [128, 2, 128])] * 2
                  ongb = SB(es, "ongb", [128, 128])
                  QTb = [SB(es, f"QTb{i}", [128, NT, 128], BF16) for i in range(2)]
                  ER = [SB(es, f"ER{i}", [128, NTT, 4]) for i in range(2)]; ERD = [SB(es, f"ERD{i}", [128, NTT, 2]) for i in range(2)]
                  DSb = [SB(es, f"DSb{i}", [128, NTT, 128]) for i in range(2)]
                  OPb = [SB(es, f"OPb{i}", [128, NT, 128]) for i in range(2)]
                  SGS = [SB(es, f"SGS{i}", [128, NT, 128]) for i in range(2)]
                  OGh = [SB(es, "OGh", [128, T], BF16)] * 2
                  qs = [SB(es, f"qs{i}", [128, 128]) for i in range(2)]
                  sgt = [SB(es, "sgt", [128, 128])] * 2
                  vb = [SB(es, f"vb{i}", [128, 128], BF16) for i in range(3)]
                  sig = [SB(es, "sig", [128, 256])] * 2
                  gl = [SB(es, f"gl{i}", [128, 256]) for i in range(2)]
                  kk = [SB(es, f"kk{i}", [128, 256]) for i in range(2)]
                  Eb = [SB(es, "Eb", [128, 256])] * 2
                  Ei = [SB(es, "Ei", [128, 256])] * 2
                  E2 = [SB(es, "E2", [128, 256])] * 2
                  qt = [SB(es, f"qt{i}", [128, 256], BF16) for i in range(2)]
                  kt = [SB(es, f"kt{i}", [128, 256], BF16) for i in range(2)]
                  kh = [SB(es, f"kh{i}", [128, 256], BF16) for i in range(2)]
                  qTt = [SB(es, f"qTt{i}", [128, 2, 128], BF16) for i in range(2)]
                  kTt = [SB(es, f"kTt{i}", [128, 2, 128], BF16) for i in range(2)]
                  sT = [SB(es, f"sT{i}", [128, 2, 128], BF16) for i in range(2)]
                  kTz = [SB(es, f"kTz{i}", [128, 2, 128], BF16) for i in range(2)]
                  SmF = [SB(es, f"SmF{i}", [128, 128], BF16) for i in range(2)]
                  for i_ in range(2):
                      MEMSET("pool", kTz[i_].t[:], 0.0, [kTz[i_]])
                  SstF = SB(es, "SstF", [128, 128]); SstB = SB(es, "SstB", [128, 128]); SmB = SB(es, "SmB", [128, 128], BF16)
                  sqj = SB(es, "sqj", [128, 128]); ssq = SB(es, "ssq", [128, NT]); rs16 = SB(es, "rs16", [128, NT])
                  on = [SB(es, "on", [128, 128])] * 2; onb = [SB(es, "onb", [128, 128], BF16)] * 2
                  PQ = [PS(es, f"PQ{i}", [128, 512]) for i in range(2)]
                  PQ2 = PS(es, "PQ2", [128, 4, 128])
                  PC = PS(es, "PC", [128, 4, 128])
                  PT = PS(es, "PT", [128, 8, 128], BF16)
                  PSs = PS(es, "PSs", [128, 4, 128])
                  PD = PS(es, "PD", [128, 4, 128])
                  PCH = PS(es, "PCH", [128, 4, 128])
                  DMA("act", ongb.t[:], ong[0:1, :].partition_broadcast(128), [], [ongb])
                  def load_head_w(h):
                      w_ = wsl[h % 2]
                      DMA("pool", w_.t[:], w_in[h].rearrange("(kc p) n -> p kc n", p=128), [], [w_])

                  def head_prologue(h):
                      hb = h % 2
                      for d_ in range(2):
                          for s_ in range(3):
                              DMA("act", lbr.t[:, d_, s_, :], hlb[d_, s_:s_ + 1, h * 128:(h + 1) * 128].partition_broadcast(128), [], [lbr])
                      lbrf = lbr.t[:].rearrange("p a s k -> p (a s k)")
                      ACT(lbrf, lbrf, AF.Exp, [lbr], [lbr])
                      lsum = kk[0]; lsv = kk[0].t[:].rearrange("p (a k) -> p a k", k=128)
                      TT("dve", lsv, lbr.t[:, :, 0, :], lbr.t[:, :, 1, :], ALU.add, [lbr], [lsum])
                      TT("dve", lsv, lsv, lbr.t[:, :, 2, :], ALU.add, [lbr, lsum], [lsum])
                      RECIP(lsv, lsv, [lsum], [lsum])
                      TT("dve", lb2[hb].t[:], lbr.t[:, :, 0, :], lsv, ALU.mult, [lbr, lsum], [lb2[hb]])
                      TS("dve", oml2[hb].t[:], lb2[hb].t[:], -1.0, 1.0, ALU.mult, ALU.add, [lb2[hb]], [oml2[hb]])
                      MEMSET("pool", SstF.t[:], 0.0, [SstF])

                  def S1(h, i):
                      hb = h % 2; b = i % 2; lat = i >= NTC; li = i - NTC
                      w_ = wsl[hb]; pq = PQ[b]; v_ = vb[i % 3]
                      lbf = lb2[hb].t[:].rearrange("p a k -> p (a k)"); omf = oml2[hb].t[:].rearrange("p a k -> p (a k)")
                      for kc in range(16):
                          MM(pq.t[:], aT.t[:, kc, i * 128:(i + 1) * 128], w_.t[:, kc, 0:512], kc == 0, kc == 15, [aT, w_], [pq])
                      if lat:
                          for kc in range(16):
                              MM(PQ2.t[:, b, :], aT.t[:, kc, i * 128:(i + 1) * 128], w_.t[:, kc, 512:640], kc == 0, kc == 15, [aT, w_], [PQ2])
                      ACT(sig[b].t[:], pq.t[:, 256:512], AF.Exp, [pq], [sig[b]], scale=-1.0)
                      if lat:
                          ACT(qs[b].t[:], pq.t[:, 0:128], AF.Exp, [pq], [qs[b]], scale=-1.0)
                          ACT(sgt[b].t[:], PQ2.t[:, b, :], AF.Exp, [PQ2], [sgt[b]], scale=-1.0)
                      CP("dve", v_.t[:], pq.t[:, 128:256], [pq], [v_])
                      TS("pool", sig[b].t[:], sig[b].t[:], 1.0, None, ALU.add, None, [sig[b]], [sig[b]])
                      RECIP(sig[b].t[:], sig[b].t[:], [sig[b]], [sig[b]])
                      if lat:
                          TS("pool", qs[b].t[:], qs[b].t[:], 1.0, None, ALU.add, None, [qs[b]], [qs[b]])
                          RECIP(qs[b].t[:], qs[b].t[:], [qs[b]], [qs[b]])
                          TT("dve", qs[b].t[:], qs[b].t[:], pq.t[:, 0:128], ALU.mult, [qs[b], pq], [qs[b]])
                          TS("pool", sgt[b].t[:], sgt[b].t[:], 1.0, None, ALU.add, None, [sgt[b]], [sgt[b]])
                          RECIP(sgt[b].t[:], sgt[b].t[:], [sgt[b]], [sgt[b]])
                          TT("dve", SGS[hb].t[:, li, :], sgt[b].t[:], PQ2.t[:, b, :], ALU.mult, [sgt[b], PQ2], [SGS[hb]])
                      TT("dve", sig[b].t[:], sig[b].t[:], omf, ALU.mult, [sig[b], oml2[hb]], [sig[b]])
                      TT("pool", sig[b].t[:], sig[b].t[:], lbf, ALU.add, [sig[b], lb2[hb]], [sig[b]])
                      ACT(gl[b].t[:], sig[b].t[:], AF.Ln, [sig[b]], [gl[b]])
                      TS("pool", kk[b].t[:], sig[b].t[:], -1.0, 1.0, ALU.mult, ALU.add, [sig[b]], [kk[b]])

                  def S2(h, i):
                      hb = h % 2; b = i % 2; lat = i >= NTC; v_ = vb[i % 3]
                      for d_ in range(2):
                          if lat:
                              MM(PC.t[:, d_, :], LcT[d_], gl[b].t[:, d_ * 128:(d_ + 1) * 128], True, True, [CF, gl[b]], [PC])
                          MM(PC.t[:, 2 + d_, :], E2T[d_], gl[b].t[:, d_ * 128:(d_ + 1) * 128], True, True, [CF, gl[b]], [PC])
                          MM(PSs.t[:, 3, 2 * d_:2 * d_ + 2], gl[b].t[:, d_ * 128:(d_ + 1) * 128], IND, True, True, [CF, gl[b]], [PSs])
                      ACT(E2[b].t[:], PC.t[:, 2:4, :].rearrange("p a k -> p (a k)"), AF.Exp, [PC], [E2[b]])
                      ACT(ER[hb].t[:, i, :], PSs.t[:, 3, 0:4], AF.Exp, [PSs], [ER[hb]])
                      TT("pool", kh[b].t[:], kk[b].t[:], E2[b].t[:], ALU.mult, [kk[b], E2[b]], [kh[b]])
                      for d_ in range(2):
                          MM(PD.t[:, d_, :], kh[b].t[:, d_ * 128:(d_ + 1) * 128], v_.t[:], True, True, [kh[b], v_], [PD])
                      if lat:
                          ACT(Eb[b].t[:], PC.t[:, 0:2, :].rearrange("p a k -> p (a k)"), AF.Exp, [PC], [Eb[b]])
                          ACT(Ei[b].t[:], PC.t[:, 0:2, :].rearrange("p a k -> p (a k)"), AF.Exp, [PC], [Ei[b]], scale=-1.0)
                      TT("pool", ERD[hb].t[:, i, 0:1], ER[hb].t[:, i, 0:1], ER[hb].t[:, i, 1:2], ALU.mult, [ER[hb]], [ERD[hb]])
                      TT("pool", ERD[hb].t[:, i, 1:2], ER[hb].t[:, i, 3:4], ER[hb].t[:, i, 2:3], ALU.mult, [ER[hb]], [ERD[hb]])
                      if lat:
                          TS("dve", SmF[b].t[:], SstF.t[:], ER[hb].t[:, i, 0:1], None, ALU.mult, None, [SstF, ER[hb]], [SmF[b]])
                      STT("dve", SstF.t[:], SstF.t[:], ERD[hb].t[:, i, 0:1], PD.t[:, 0, :], ALU.mult, ALU.add, [SstF, ERD[hb], PD], [SstF])
                      CP("act", DSb[hb].t[:, i, :], PD.t[:, 1, :], [PD], [DSb[hb]])
                      if lat:
                          for d_ in range(2):
                              TT("pool", qt[b].t[:, d_ * 128:(d_ + 1) * 128], qs[b].t[:], Eb[b].t[:, d_ * 128:(d_ + 1) * 128], ALU.mult, [qs[b], Eb[b]], [qt[b]])
                          TT("dve", kt[b].t[:], kk[b].t[:], Ei[b].t[:], ALU.mult, [kk[b], Ei[b]], [kt[b]])

                  def S3(h, i):
                      hb = h % 2; b = i % 2; lat = i >= NTC; li = i - NTC; v_ = vb[i % 3]
                      if not lat:
                          return
                      for d_ in range(2):
                          TR(PT.t[:, d_, :], qt[b].t[:, d_ * 128:(d_ + 1) * 128], identb, [qt[b], CB], [PT])
                          TR(PT.t[:, 2 + d_, :], kt[b].t[:, d_ * 128:(d_ + 1) * 128], identb, [kt[b], CB], [PT])
                      CP("dve", qTt[b].t[:], PT.t[:, 0:2, :], [PT], [qTt[b]])
                      CP("act", kTt[b].t[:], PT.t[:, 2:4, :], [PT], [kTt[b]])
                      CP("dve", kTz[b].t[:, 0, 0:64], PT.t[:, 2, 0:64], [PT], [kTz[b]])
                      CP("dve", kTz[b].t[:, 1, 64:128], PT.t[:, 3, 64:128], [PT], [kTz[b]])
                      CP("pool", QTb[hb].t[:, li, :], qTt[b].t[:, 1, :], [qTt[b]], [QTb[hb]])
                      MM(PSs.t[:, 0, 64:128], kTt[b].t[:, 0, :], qTt[b].t[:, 0, 64:128], True, True, [kTt[b], qTt[b]], [PSs])
                      MM(PSs.t[:, 0, 0:64], kTz[b].t[:, 0, :], qTt[b].t[:, 0, 0:64], True, True, [kTz[b], qTt[b]], [PSs])
                      MM(PSs.t[:, 1, 0:64], kTt[b].t[:, 1, :], qTt[b].t[:, 1, 0:64], True, True, [kTt[b], qTt[b]], [PSs])
                      MM(PSs.t[:, 1, 64:128], kTz[b].t[:, 1, :], qTt[b].t[:, 1, 64:128], True, True, [kTz[b], qTt[b]], [PSs])
                      TT("dve", sT[b].t[:], PSs.t[:, 0:2, :], CF.t[:, 5:7, :], ALU.mult, [PSs, CF], [sT[b]])
                      MM(PSs.t[:, 2, :], sT[b].t[:, 0, :], v_.t[:], True, False, [sT[b], v_], [PSs])
                      MM(PSs.t[:, 2, :], sT[b].t[:, 1, :], v_.t[:], False, False, [sT[b], v_], [PSs])
                      MM(PSs.t[:, 2, :], qTt[b].t[:, 0, :], SmF[b].t[:], False, True, [qTt[b], SmF[b]], [PSs])
                      CP("act", OPb[hb].t[:, li, :], PSs.t[:, 2, :], [PSs], [OPb[hb]])

                  bw_order = [1, 0] + list(range(NTT - 1, NTC - 1, -1))

                  def deferred(h, u):
                      hb = h % 2
                      if u == 0:
                          MEMSET("pool", SstB.t[:], 0.0, [SstB])
                      i = bw_order[u]
                      if i >= NTC:
                          li = i - NTC; k = li % 2
                          TS("pool", SmB.t[:], SstB.t[:], ER[hb].t[:, i, 3:4], None, ALU.mult, None, [SstB, ER[hb]], [SmB])
                          MM(PCH.t[:, 0, :], QTb[hb].t[:, li, :], SmB.t[:], True, True, [QTb[hb], SmB], [PCH])
                          TT("dve", OPb[hb].t[:, li, :], OPb[hb].t[:, li, :], PCH.t[:, 0, :], ALU.add, [OPb[hb], PCH], [OPb[hb]])
                      STT("dve", SstB.t[:], SstB.t[:], ERD[hb].t[:, i, 1:2], DSb[hb].t[:, i, :], ALU.mult, ALU.add, [SstB, ERD[hb], DSb[hb]], [SstB])
                      if i >= NTC:
                          TT("pool", sqj.t[:], OPb[hb].t[:, li, :], OPb[hb].t[:, li, :], ALU.mult, [OPb[hb]], [sqj])
                          RED("dve", ssq.t[:, li:li + 1], sqj.t[:], ALU.add, [sqj], [ssq])
                          ACT(rs16.t[:, li:li + 1], ssq.t[:, li:li + 1], AF.Ln, [ssq, EPSb], [rs16], scale=1.0 / 128, bias=EPSb.t[:])
                          ACT(rs16.t[:, li:li + 1], rs16.t[:, li:li + 1], AF.Exp, [rs16], [rs16], scale=-0.5)
                          STT("dve", on[k].t[:], OPb[hb].t[:, li, :], rs16.t[:, li:li + 1], SGS[hb].t[:, li, :], ALU.mult, ALU.mult, [OPb[hb], rs16, SGS[hb]], [on[k]])
                          TT("pool", onb[k].t[:], on[k].t[:], ongb.t[:], ALU.mult, [on[k], ongb], [onb[k]])
                          TR(PT.t[:, 4 + li % 4, :], onb[k].t[:], identb, [onb[k], CB], [PT])
                          CP("act", OGh[hb].t[:, li * 128:(li + 1) * 128], PT.t[:, 4 + li % 4, :], [PT], [OGh[hb]])
                      if u == NTT - 1:
                          DMA("sp", OGT[h * 128:(h + 1) * 128, :], OGh[hb].t[:], [OGh[hb]], [P.reg("OGT")])

                  load_head_w(0)
                  for h in range(nheads + 1):
                      if h < nheads:
                          if h + 1 < nheads:
                              load_head_w(h + 1)
                          head_prologue(h)
                      for step in range(NTT + 2):
                          if h < nheads:
                              if step < NTT:
                                  S1(h, step)
                              if 0 <= step - 1 < NTT:
                                  S2(h, step - 1)
                              if 0 <= step - 2 < NTT:
                                  S3(h, step - 2)
                          if h >= 1 and step < NTT:
                              deferred(h - 1, step)
                  END_PHASE()

          def post_mixer(l, wmat, hin, dbg_name):
              with ExitStack() as es:
                  atl = [SB(es, f"atl{i}", [128, 16, 128], BF16) for i in range(2)]
                  OGTv = OGT.rearrange("(kc p) t -> p kc t", p=128)
                  wo = SB(es, "wo", [128, 16, D], BF16)
                  G1 = SB(es, "G1", [128, D]); A2 = SB(es, "A2", [128, D]); B2 = SB(es, "B2", [128, D])
                  WR = SB(es, "WR", [128, 16, 72]); BR = SB(es, "BR", [128, 72])
                  xt = [SB(es, f"pxt{i}", [128, D]) for i in range(2)]
                  hn = [SB(es, "phn", [128, D])] * 2
                  bfl = SB(es, "pbfl", [128, D]); bb = [SB(es, f"pbb{i}", [128, D], BF16) for i in range(2)]
                  LG = SB(es, "pLG", [128, NT, 72]); sm2 = SB(es, "psm2", [128, 26, NT]); t8 = SB(es, "pt8", [128, NT, 8])
                  PRs = SB(es, "pPRs", [128, NT, 64]); SIDX = SB(es, "pSIDX", [128, NT, 2], I32)
                  ss = SB(es, "pss", [128, 1]); rstd = SB(es, "prstd", [128, 1]); junkb = SB(es, "pjunkb", [128, D], BF16)
                  bT = SB(es, "pbT", [128, 16, 128])
                  As = SB(es, "pAs", [128, NT, 64], BF16)
                  oh1 = SB(es, "poh1", [128, NT, 8])
                  sel = SB(es, "psel", [128, NT, 8]); sel2 = SB(es, "psel2", [128, NT, 8]); oha = SB(es, "poha", [128, NT, 8]); ohb = SB(es, "pohb", [128, NT, 8])
                  Aa = SB(es, "pAa", [128, NT, 64]); Ab = SB(es, "pAb", [128, NT, 64]); t64 = SB(es, "pt64", [128, NT, 64])
                  PY = [PS(es, f"PY{i}", [128, 512]) for i in range(4)]
                  PTf = [PS(es, f"PTf{i}", [128, 4, 128]) for i in range(2)]
                  PL = PS(es, "PL", [128, 512]); PR = PS(es, "PRk", [128, 512])
                  DMA("pool", wo.t[:], wmat.rearrange("(kc p) n -> p kc n", p=128), [], [wo])
                  DMA("sp", G1.t[:], DER[l, :, 2, :], [], [G1]); DMA("sp", B2.t[:], DER[l, :, 3, :], [], [B2]); DMA("sp", A2.t[:], DER[l, :, 4, :], [], [A2])
                  DMA("sp", WR.t[:], wr[l].rearrange("(kc p) n -> p kc n", p=128), [], [WR])
                  DMA("act", BR.t[:], brr[l:l + 1, :].partition_broadcast(128), [], [BR])
                  c = lambda k: sm.t[:, k:k + 1]
                  for i in range(NT):
                      b = i % 2
                      x_ = xt[b]; h_ = hn[b]
                      DMA("sp", x_.t[:], hin[i * 128:(i + 1) * 128, :], [P.reg("HIN", i)], [x_])
                      at_ = atl[b]
                      DMA("sp", at_.t[:], OGTv[:, :, i * 128:(i + 1) * 128], [P.reg("OGT")], [at_])
                      for ch in range(4):
                          for kc in range(16):
                              MM(PY[ch].t[:], at_.t[:, kc, :], wo.t[:, kc, ch * 512:(ch + 1) * 512], kc == 0, kc == 15, [at_, wo], [PY[ch]])
                          TT("dve", h_.t[:, ch * 512:(ch + 1) * 512], PY[ch].t[:], G1.t[:, ch * 512:(ch + 1) * 512], ALU.mult, [PY[ch], G1], [h_])
                      TT("pool", h_.t[:], h_.t[:], x_.t[:], ALU.add, [h_, x_], [h_])
                      DMA("sp", Hs[i * 128:(i + 1) * 128, :], h_.t[:], [h_], [P.reg("HS", i)])
                      if debug:
                          DMA("sp", dbg[dbg_name][i * 128:(i + 1) * 128, :], h_.t[:], [h_], [P.reg("DBG", i)])
                      rms_rstd(h_.t[:], ss, rstd, junkb, [h_])
                      ACT(bfl.t[:], h_.t[:], AF.Copy, [h_, rstd], [bfl], scale=rstd.t[:])
                      TT("dve", bfl.t[:], bfl.t[:], A2.t[:], ALU.mult, [bfl, A2], [bfl])
                      TT("pool", bfl.t[:], bfl.t[:], B2.t[:], ALU.add, [bfl, B2], [bfl])
                      CP("act", bb[b].t[:], bfl.t[:], [bfl], [bb[b]])
                      DMA("sp", BBd[i * 128:(i + 1) * 128, :], bb[b].t[:], [bb[b]], [P.reg("BBD", i)])
                      for q4 in range(4):
                          p_ = PTf[q4 % 2]
                          for k in range(4):
                              kc = q4 * 4 + k
                              TR(p_.t[:, k, :], bfl.t[:, kc * 128:(kc + 1) * 128], identf, [bfl, CF], [p_])
                          CP("dve" if q4 % 2 == 0 else "act", bT.t[:, q4 * 4:(q4 + 1) * 4, :], p_.t[:], [p_], [bT])
                      for kc in range(16):
                          MM(PL.t[:, 0:72], bT.t[:, kc, :], WR.t[:, kc, :], kc == 0, kc == 15, [bT, WR], [PL])
                      TT("dve", LG.t[:, i, :], PL.t[:, 0:72], BR.t[:], ALU.add, [PL, BR], [LG])
                  def bc8(ap2):
                      return ap2.unsqueeze(2).broadcast_to([128, NT, 8])
                  L1 = LG.t[:, :, 0:8]
                  c2 = lambda k: sm2.t[:, k, :]
                  io8 = IOT.t[:, 0:8].unsqueeze(1).broadcast_to([128, NT, 8])
                  io64 = IOT.t[:, 0:64].unsqueeze(1).broadcast_to([128, NT, 64])
                  dumpc = IOT.t[:, 64:65].broadcast_to([128, NT])
                  RED("dve", c2(0), L1, ALU.max, [LG], [sm2])
                  TT("dve", oh1.t[:], L1, bc8(c2(0)), ALU.is_equal, [LG, sm2], [oh1])
                  TT("dve", t8.t[:], L1, bc8(c2(0)), ALU.subtract, [LG, sm2], [t8])
                  ACT(t8.t[:], t8.t[:], AF.Exp, [t8], [t8])
                  RED("dve", c2(2), t8.t[:], ALU.add, [t8], [sm2])
                  RECIP(c2(3), c2(2), [sm2], [sm2])
                  for g in range(8):
                      TT("dve", t8.t[:], LG.t[:, :, 8 + g * 8:16 + g * 8], bc8(oh1.t[:, :, g]), ALU.mult, [LG, oh1], [t8])
                      if g == 0:
                          CP("dve", sel.t[:], t8.t[:], [t8], [sel])
                      else:
                          TT("dve", sel.t[:], sel.t[:], t8.t[:], ALU.add, [sel, t8], [sel])
                  RED("dve", c2(4), sel.t[:], ALU.max, [sel], [sm2])
                  TT("dve", oha.t[:], sel.t[:], bc8(c2(4)), ALU.is_equal, [sel, sm2], [oha])
                  STT("dve", sel2.t[:], oha.t[:], -1e30, sel.t[:], ALU.mult, ALU.add, [oha, sel], [sel2])
                  RED("dve", c2(6), sel2.t[:], ALU.max, [sel2], [sm2])
                  TT("dve", ohb.t[:], sel2.t[:], bc8(c2(6)), ALU.is_equal, [sel2, sm2], [ohb])
                  TT("dve", c2(7), c2(6), c2(4), ALU.subtract, [sm2], [sm2])
                  ACT(c2(7), c2(7), AF.Exp, [sm2], [sm2])
                  TS("dve", c2(8), c2(7), 1.0, None, ALU.add, None, [sm2], [sm2])
                  RECIP(c2(8), c2(8), [sm2], [sm2])
                  TT("dve", c2(9), c2(3), c2(8), ALU.mult, [sm2], [sm2])
                  TT("dve", c2(10), c2(9), c2(7), ALU.mult, [sm2], [sm2])
                  for src_, dst_ in ((oh1, 11), (oha, 12), (ohb, 13)):
                      TT("dve", t8.t[:], src_.t[:], io8, ALU.mult, [src_, IOT], [t8])
                      RED("dve", c2(dst_), t8.t[:], ALU.add, [t8], [sm2])
                  STT("dve", c2(14), c2(11), 8.0, c2(12), ALU.mult, ALU.add, [sm2], [sm2])
                  STT("dve", c2(15), c2(11), 8.0, c2(13), ALU.mult, ALU.add, [sm2], [sm2])
                  TT("dve", Aa.t[:], io64, c2(14).unsqueeze(2).broadcast_to([128, NT, 64]), ALU.is_equal, [IOT, sm2], [Aa])
                  TT("dve", Ab.t[:], io64, c2(15).unsqueeze(2).broadcast_to([128, NT, 64]), ALU.is_equal, [IOT, sm2], [Ab])
                  TT("dve", As.t[:], Aa.t[:], Ab.t[:], ALU.add, [Aa, Ab], [As])
                  for i in range(NT):
                      pr_ = PY[i // 8]
                      for j in range(i + 1):
                          MM(pr_.t[:, (i % 8) * 64:(i % 8 + 1) * 64], UTs if j == i else onesb, As.t[:, j, :], j == 0, j == i, [As, CB], [pr_])
                  for hf_ in range(2):
                      CP("dve" if hf_ == 0 else "act", PRs.t[:, hf_ * 8:(hf_ + 1) * 8, :], PY[hf_].t[:].rearrange("p (a e) -> p a e", e=64), [PY[hf_]], [PRs])
                  TT("dve", t64.t[:], PRs.t[:], Aa.t[:], ALU.mult, [PRs, Aa], [t64]); RED("dve", c2(16), t64.t[:], ALU.add, [t64], [sm2])
                  TT("dve", t64.t[:], PRs.t[:], Ab.t[:], ALU.mult, [PRs, Ab], [t64]); RED("dve", c2(17), t64.t[:], ALU.add, [t64], [sm2])
                  for k in range(2):
                      rk = c2(16 + k); ok = c2(18 + k); sg_ = c2(20 + k); ssc = c2(22 + k); ek = c2(14 + k); gk = c2(9 + k)
                      TS("dve", ok, rk, float(CAP) - 0.5, None, ALU.is_lt, None, [sm2], [sm2])
                      TS("dve", c2(24), rk, float(CAP - 1), None, ALU.min, None, [sm2], [sm2])
                      STT("dve", sg_, ek, float(CAP), c2(24), ALU.mult, ALU.add, [sm2], [sm2])
                      TT("dve", c2(25), sg_, dumpc, ALU.subtract, [sm2, IOT], [sm2])
                      TT("dve", c2(25), c2(25), ok, ALU.mult, [sm2], [sm2])
                      TT("dve", ssc, c2(25), dumpc, ALU.add, [sm2, IOT], [sm2])
                      TT("dve", GATES.t[:, :, k], gk, ok, ALU.mult, [sm2], [GATES])
                      CP("dve", SLOTG.t[:, :, k], sg_, [sm2], [SLOTG])
                      CP("dve", SIDX.t[:, :, k], ssc, [sm2], [SIDX])
                  for i in range(NT):
                      b = i % 2
                      DMA("sp", bb[b].t[:], BBd[i * 128:(i + 1) * 128, :], [P.reg("BBD", i)], [bb[b]])
                      for k in range(2):
                          P.dma("pool", lambda e, i=i, k=k, b=b: e.indirect_dma_start(
                              out=Xs[:, :], out_offset=bass.IndirectOffsetOnAxis(ap=SIDX.t[:, i, k:k + 1], axis=0),
                              in_=bb[b].t[:], in_offset=None),
                              rr([bb[b], SIDX]), [P.reg("XS")])
                  END_PHASE()

          def experts(l):
              with ExitStack() as es:
                  WG = [SB(es, f"WG{i}", [128, 16, 512], BF16) for i in range(2)]
                  WU = [SB(es, f"WU{i}", [128, 16, 512], BF16) for i in range(2)]
                  WD = [SB(es, f"WD{i}", [128, 4, D], BF16) for i in range(2)]
                  xe = [SB(es, f"xe{i}", [128, 2, D], BF16) for i in range(2)]
                  xeT = [SB(es, f"xeT{i}", [128, 2, 16, 128], BF16) for i in range(2)]
                  sgl = [SB(es, f"sgl{i}", [128, 512]) for i in range(2)]
                  hh = [SB(es, f"hh{i}", [128, 512], BF16) for i in range(2)]
                  hT = [SB(es, f"hT{i}", [128, 4, 128], BF16) for i in range(2)]
                  ye = [SB(es, f"ye{i}", [128, D], BF16) for i in range(2)]
                  PX = [PS(es, f"PX{i}", [128, 8, 128], BF16) for i in range(2)]
                  PG = PS(es, "PG", [128, 512]); PU = PS(es, "PU", [128, 512])
                  PH = PS(es, "PH", [128, 8, 128], BF16)
                  PYe = [PS(es, f"PYe{i}", [128, 512]) for i in range(2)]

                  def ldw(e):
                      b = e % 2
                      DMA("pool", WG[b].t[:], wg[l, e].rearrange("(kc p) n -> p kc n", p=128), [], [WG[b]])
                      DMA("pool", WU[b].t[:], wu[l, e].rearrange("(kc p) n -> p kc n", p=128), [], [WU[b]])
                      DMA("pool", WD[b].t[:], wd[l, e].rearrange("(kc p) n -> p kc n", p=128), [], [WD[b]])
                  ldw(0)
                  cnt = 0
                  for e in range(NE):
                      b = e % 2
                      if e + 1 < NE:
                          ldw(e + 1)
                      DMA("sp", xe[b].t[:], Xs[e * CAP:(e + 1) * CAP, :].rearrange("(hf p) d -> p hf d", p=128), [P.reg("XS")], [xe[b]])
                      for hf in range(2):
                          for q in range(2):
                              p_ = PX[q]
                              for k in range(8):
                                  kc = q * 8 + k
                                  TR(p_.t[:, k, :], xe[b].t[:, hf, kc * 128:(kc + 1) * 128], identb, [xe[b], CB], [p_])
                              CP("dve" if q == 0 else "act", xeT[b].t[:, hf, q * 8:(q + 1) * 8, :], p_.t[:], [p_], [xeT[b]])
                      for hf in range(2):
                          u = cnt % 2; cnt += 1
                          for kc in range(16):
                              MM(PG.t[:], xeT[b].t[:, hf, kc, :], WG[b].t[:, kc, :], kc == 0, kc == 15, [xeT[b], WG[b]], [PG])
                          for kc in range(16):
                              MM(PU.t[:], xeT[b].t[:, hf, kc, :], WU[b].t[:, kc, :], kc == 0, kc == 15, [xeT[b], WU[b]], [PU])
                          ACT(sgl[u].t[:], PG.t[:], AF.Silu, [PG], [sgl[u]])
                          TT("dve", hh[u].t[:], sgl[u].t[:], PU.t[:], ALU.mult, [sgl[u], PU], [hh[u]])
                          for k in range(4):
                              TR(PH.t[:, k, :], hh[u].t[:, k * 128:(k + 1) * 128], identb, [hh[u], CB], [PH])
                          CP("dve", hT[u].t[:], PH.t[:, 0:4, :], [PH], [hT[u]])
                          for ch in range(4):
                              py = PYe[ch % 2]
                              for k in range(4):
                                  MM(py.t[:], hT[u].t[:, k, :], WD[b].t[:, k, ch * 512:(ch + 1) * 512], k == 0, k == 3, [hT[u], WD[b]], [py])
                              CP("act" if ch % 2 == 0 else "dve", ye[u].t[:, ch * 512:(ch + 1) * 512], py.t[:], [py], [ye[u]])
                          r0 = e * CAP + hf * 128
                          DMA("sp", Ys[r0:r0 + 128, :], ye[u].t[:], [ye[u]], [P.reg("YS")])
                  END_PHASE()

          def combine(l, aT1, dbg_name):
              with ExitStack() as es:
                  G2 = SB(es, "G2", [128, D]); A1 = SB(es, "cA1", [128, D]); B1 = SB(es, "cB1", [128, D])
                  Y0 = [SB(es, f"Y0{i}", [128, D], BF16) for i in range(2)]
                  Y1 = [SB(es, f"Y1{i}", [128, D], BF16) for i in range(2)]
                  hn = [SB(es, f"chn{i}", [128, D]) for i in range(2)]
                  z = SB(es, "cz", [128, D]); h2 = [SB(es, f"ch2{i}", [128, D]) for i in range(2)]
                  ss = SB(es, "css", [128, 1]); rstd = SB(es, "crstd", [128, 1]); junk = SB(es, "cjunk", [128, D], BF16)
                  xn = SB(es, "cxn", [128, D]); ab_ = SB(es, "cab", [128, D], BF16)
                  pt = [PS(es, f"cpt{i}", [128, 8, 128], BF16) for i in range(2)]
                  DMA("sp", G2.t[:], DER[l, :, 5, :], [], [G2])
                  if l == 0:
                      DMA("sp", B1.t[:], DER[1, :, 0, :], [], [B1]); DMA("sp", A1.t[:], DER[1, :, 1, :], [], [A1])
                  else:
                      DMA("act", A1.t[:], fing[0:1, :].partition_broadcast(128), [], [A1])
                  for i in range(NT):
                      b = i % 2
                      for k, Yk in enumerate([Y0[b], Y1[b]]):
                          P.dma("pool", lambda e, Yk=Yk, i=i, k=k: e.indirect_dma_start(
                              out=Yk.t[:], out_offset=None, in_=Ys[:, :],
                              in_offset=bass.IndirectOffsetOnAxis(ap=SLOTG.t[:, i, k:k + 1], axis=0)),
                              rr([SLOTG, P.reg("YS")]), rr([Yk]))
                      DMA("sp", hn[b].t[:], Hs[i * 128:(i + 1) * 128, :], [P.reg("HS", i)], [hn[b]])
                      TS("dve", z.t[:], Y0[b].t[:], GATES.t[:, i, 0:1], None, ALU.mult, None, [Y0[b], GATES], [z])
                      STT("dve", z.t[:], Y1[b].t[:], GATES.t[:, i, 1:2], z.t[:], ALU.mult, ALU.add, [Y1[b], GATES, z], [z])
                      TT("dve", z.t[:], z.t[:], G2.t[:], ALU.mult, [z, G2], [z])
                      TT("pool", h2[b].t[:], z.t[:], hn[b].t[:], ALU.add, [z, hn[b]], [h2[b]])
                      if debug and dbg_name in dbg:
                          DMA("sp", dbg[dbg_name][i * 128:(i + 1) * 128, :], h2[b].t[:], [h2[b]], [P.reg("DBG", i)])
                      if l == 0:
                          DMA("sp", Hs[i * 128:(i + 1) * 128, :], h2[b].t[:], [h2[b]], [P.reg("HS", i)])
                          norm_mod_transpose(es, h2[b], A1, B1, aT1, i * 128, (ss, rstd, junk, xn, ab_, pt))
                      else:
                          rms_rstd(h2[b].t[:], ss, rstd, junk, [h2[b]])
                          ACT(xn.t[:], h2[b].t[:], AF.Copy, [h2[b], rstd], [xn], scale=rstd.t[:])
                          TT("dve", z.t[:], xn.t[:], A1.t[:], ALU.mult, [xn, A1], [z])
                          DMA("sp", out[i * 128:(i + 1) * 128, :], z.t[:], [z], [P.reg("OUT", i)])
                  END_PHASE()

          post_mixer(0, w_out, x, "hmid0")
          experts(0)
          with ExitStack() as esM:
              MEAN = SB(esM, "MEAN", [128, T]); RSTD = SB(esM, "RSTD", [128, T])
              with ExitStack() as esG:
                  aT1 = SB(esG, "aT1", [128, 16, T], BF16)
                  combine(0, aT1, "hend0")
                  with ExitStack() as es:
                      wv = [SB(es, f"wv{i}", [128, 16, 128], BF16) for i in range(2)]
                      wgt = [SB(es, f"wgt{i}", [128, 16, 128], BF16) for i in range(2)]
                      WDW = SB(es, "WDW", [128, 16, 31]); BDW = SB(es, "BDW", [128, 16])
                      sgm = [SB(es, f"sgm{i}", [128, 512]) for i in range(2)]
                      uT = [SB(es, f"uT{i}", [128, T]) for i in range(2)]
                      acc = [SB(es, f"acc{i}", [128, T]) for i in range(2)]
                      v2 = SB(es, "v2", [128, T]); MQ = SB(es, "MQ", [128, T])
                      PV = [PS(es, f"PV{i}", [128, 512]) for i in range(2)]
                      PGt = [PS(es, f"PGt{i}", [128, 512]) for i in range(2)]
                      PS1 = [PS(es, f"PS1{i}", [128, 512]) for i in range(2)]
                      DMA("sp", WDW.t[:], wdw[:, :, :], [], [WDW]); DMA("sp", BDW.t[:], bdw[:, :], [], [BDW])
                      pwv = w_pw1.rearrange("(kc p) n -> p kc n", p=128)

                      def ldcw(cc_):
                          DMA("pool", wv[cc_ % 2].t[:], pwv[:, :, cc_ * 128:(cc_ + 1) * 128], [], [wv[cc_ % 2]])
                          DMA("pool", wgt[cc_ % 2].t[:], pwv[:, :, D + cc_ * 128:D + (cc_ + 1) * 128], [], [wgt[cc_ % 2]])
                      ldcw(0)
                      MEMSET("pool", MEAN.t[:], 0.0, [MEAN]); MEMSET("pool", MQ.t[:], 0.0, [MQ])
                      for cc_ in range(16):
                          b = cc_ % 2
                          if cc_ + 1 < 16:
                              ldcw(cc_ + 1)
                          for tq in range(4):
                              pv = PV[tq % 2]; pg = PGt[tq % 2]
                              for kc in range(16):
                                  MM(pv.t[:], wv[b].t[:, kc, :], aT1.t[:, kc, tq * 512:(tq + 1) * 512], kc == 0, kc == 15, [wv[b], aT1], [pv])
                              for kc in range(16):
                                  MM(pg.t[:], wgt[b].t[:, kc, :], aT1.t[:, kc, tq * 512:(tq + 1) * 512], kc == 0, kc == 15, [wgt[b], aT1], [pg])
                              ACT(sgm[tq % 2].t[:], pg.t[:], AF.Sigmoid, [pg], [sgm[tq % 2]])
                              TT("dve", uT[b].t[:, tq * 512:(tq + 1) * 512], pv.t[:], sgm[tq % 2].t[:], ALU.mult, [pv, sgm[tq % 2]], [uT[b]])
                          eng = "dve"
                          u_ = uT[b]; a_ = acc[b]
                          TS(eng, a_.t[:], u_.t[:], WDW.t[:, cc_, 15:16], BDW.t[:, cc_:cc_ + 1], ALU.mult, ALU.add, [u_, WDW, BDW], [a_])
                          for j in range(31):
                              s = j - 15
                              if s == 0:
                                  continue
                              wj = WDW.t[:, cc_, j:j + 1]
                              if cc_ < 8:
                                  c0 = max(0, -s); c1 = min(64, 64 - s)
                                  a3 = a_.t[:].rearrange("p (r c) -> p r c", c=64); u3 = u_.t[:].rearrange("p (r c) -> p r c", c=64)
                                  STT(eng, a3[:, :, c0:c1], u3[:, :, c0 + s:c1 + s], wj, a3[:, :, c0:c1], ALU.mult, ALU.add, [u_, WDW, a_], [a_])
                              else:
                                  r0 = max(0, -s); r1 = min(32, 32 - s)
                                  STT(eng, a_.t[:, r0 * 64:r1 * 64], u_.t[:, (r0 + s) * 64:(r1 + s) * 64], wj, a_.t[:, r0 * 64:r1 * 64], ALU.mult, ALU.add, [u_, WDW, a_], [a_])
                          DMA("sp", Vd[cc_ * 128:(cc_ + 1) * 128, :], a_.t[:], [a_], [P.reg("VD")])
                          ACT(v2.t[:], a_.t[:], AF.Square, [a_], [v2])
                          for tq in range(4):
                              MM(PS1[0].t[:], onesf, a_.t[:, tq * 512:(tq + 1) * 512], True, True, [CF, a_], [PS1[0]])
                              TT("dve", MEAN.t[:, tq * 512:(tq + 1) * 512], MEAN.t[:, tq * 512:(tq + 1) * 512], PS1[0].t[:], ALU.add, [MEAN, PS1[0]], [MEAN])
                              MM(PS1[1].t[:], onesf, v2.t[:, tq * 512:(tq + 1) * 512], True, True, [CF, v2], [PS1[1]])
                              TT("dve", MQ.t[:, tq * 512:(tq + 1) * 512], MQ.t[:, tq * 512:(tq + 1) * 512], PS1[1].t[:], ALU.add, [MQ, PS1[1]], [MQ])
                      TS("dve", MEAN.t[:], MEAN.t[:], 1.0 / D, None, ALU.mult, None, [MEAN], [MEAN])
                      TT("dve", v2.t[:], MEAN.t[:], MEAN.t[:], ALU.mult, [MEAN], [v2])
                      STT("dve", MQ.t[:], MQ.t[:], 1.0 / D, v2.t[:], ALU.mult, ALU.subtract, [MQ, v2], [MQ])
                      ACT(RSTD.t[:], MQ.t[:], AF.Sqrt, [MQ, EPSb], [RSTD], bias=EPSb.t[:])
                      RECIP(RSTD.t[:], RSTD.t[:], [RSTD], [RSTD])
                      END_PHASE()
              with ExitStack() as es:
                  LNG = SB(es, "LNG", [128, 16]); LNB = SB(es, "LNB", [128, 16])
                  vt = [SB(es, f"vt{i}", [128, T]) for i in range(2)]
                  sTc = [SB(es, f"sTc{i}", [128, T], BF16) for i in range(2)]
                  DMA("sp", LNG.t[:], lng[:, :], [], [LNG]); DMA("sp", LNB.t[:], lnb[:, :], [], [LNB])
                  for cc_ in range(16):
                      b = cc_ % 2
                      DMA("sp", vt[b].t[:], Vd[cc_ * 128:(cc_ + 1) * 128, :], [P.reg("VD")], [vt[b]])
                      TT("dve", vt[b].t[:], vt[b].t[:], MEAN.t[:], ALU.subtract, [vt[b], MEAN], [vt[b]])
                      TT("pool", vt[b].t[:], vt[b].t[:], RSTD.t[:], ALU.mult, [vt[b], RSTD], [vt[b]])
                      ACT(sTc[b].t[:], vt[b].t[:], AF.Silu, [vt[b], LNG, LNB], [sTc[b]], scale=LNG.t[:, cc_:cc_ + 1], bias=LNB.t[:, cc_:cc_ + 1])
                      DMA("sp", OGT[cc_ * 128:(cc_ + 1) * 128, :], sTc[b].t[:], [sTc[b]], [P.reg("OGT")])
                  END_PHASE()
          post_mixer(1, w_pw2, Hs, "hmid1")
          experts(1)
          combine(1, None, "hend1")
    except _Stop:
        pass
    return nc


def _consts():
    s = np.arange(128)[:, None]; t = np.arange(128)[None, :]
    cf = np.zeros((128, 9, 128), np.float32)
    cf[:, 0] = np.eye(128)
    cf[:, 1] = (s <= t).astype(np.float32) - (s <= 63).astype(np.float32)
    cf[:, 2] = (s >= t).astype(np.float32) - (s >= 64).astype(np.float32)
    cf[:, 3] = (s > t)
    cf[:, 4] = (s < t)
    cf[:, 5] = (s <= t)
    cf[:, 6] = (s >= t)
    cf[:, 7] = 1.0
    cf[:, 8, 0] = (np.arange(128) <= 63); cf[:, 8, 1] = (np.arange(128) >= 64)
    cb = np.zeros((128, 3, 128), np.float32)
    cb[:, 0] = np.eye(128); cb[:, 1] = 1.0; cb[:, 2] = (s < t)
    iot = np.tile(np.arange(65, dtype=np.float32)[None, :], (128, 1))
    iot[:, 64] = XS_ROWS + np.arange(128)
    return cf, cb.astype(ml_dtypes.bfloat16), iot


def _col(v):
    return np.ascontiguousarray(np.asarray(v, np.float32).reshape(16, 128).T)


def kernel(x, c, ctx, c_ctx, ada_w, ada_b, norm1_g, norm2_g, hgrn_w_in, hgrn_lb, hgrn_onorm_g,
           hgrn_w_out, conv_w_pw1, conv_w_dw, conv_b_dw, conv_ln_g, conv_ln_b, conv_w_pw2,
           moe_w_r1, moe_b_r1, moe_w_r2, moe_b_r2, moe_w_gate, moe_w_up, moe_w_down, final_g, _debug=False, _stop=None, _ncores=8, _trace=False):
    f = lambda a: np.ascontiguousarray(np.asarray(a, np.float32))
    key = ("nc", bool(_debug), _stop)
    if key not in _cache:
        _cache[key] = build_program(debug=_debug, stop=_stop)
    nc = _cache[key]
    cf, cb, iot = _consts()
    w_r2 = np.asarray(moe_w_r2, np.float32)
    wr_ = np.concatenate([np.asarray(moe_w_r1, np.float32), w_r2.transpose(0, 2, 1, 3).reshape(2, D, 64)], axis=2)
    br_ = np.concatenate([np.asarray(moe_b_r1, np.float32), np.asarray(moe_b_r2, np.float32).reshape(2, 64)], axis=1)
    wdw_ = np.ascontiguousarray(np.asarray(conv_w_dw, np.float32)[0].T.reshape(16, 128, 31).transpose(1, 0, 2))
    shared = {
        "ada_w": f(ada_w), "ada_b": f(ada_b), "n1g": f(norm1_g), "n2g": f(norm2_g), "fing": f(final_g).reshape(1, D),
        "w_in": np.ascontiguousarray(f(hgrn_w_in)[0].reshape(D, 5, 16, 128).transpose(2, 0, 1, 3).reshape(16, D, 640)), "hlb": f(hgrn_lb)[:, :, :], "ong": f(hgrn_onorm_g).reshape(1, 128), "w_out": f(hgrn_w_out)[0],
        "w_pw1": f(conv_w_pw1)[0], "wdw": wdw_, "bdw": _col(np.asarray(conv_b_dw)[0]), "lng": _col(np.asarray(conv_ln_g)[0]),
        "lnb": _col(np.asarray(conv_ln_b)[0]), "w_pw2": f(conv_w_pw2)[0],
        "wr": np.ascontiguousarray(wr_), "brr": np.ascontiguousarray(br_),
        "cst_f": cf, "cst_b": cb, "iot": iot,
    }
    if _stop is None or _stop >= 5:
        shared.update({"wg": f(moe_w_gate), "wu": f(moe_w_up), "wd": f(moe_w_down)})
    xx = f(x); cx = f(ctx); cc = np.asarray(c, np.float32); ccx = np.asarray(c_ctx, np.float32)
    in_maps = []
    for b in range(_ncores):
        m = dict(shared)
        m["x"] = xx[b]; m["ctx"] = cx[b]
        m["ccol"] = np.ascontiguousarray(np.concatenate([_col(cc[b]), _col(ccx)], axis=1))
        in_maps.append(m)
    res = run_bass_kernel_spmd(nc, in_maps, core_ids=list(range(_ncores)), **({'trace': True} if _trace else {}))
    if _trace:
        print('EXEC_NS', _stop, res.exec_time_ns)
    outp = np.stack([np.asarray(r["out"]) for r in res.results], axis=0).astype(np.float32)
    if _debug:
        return outp, res.results
    return outp
```

```python
import numpy as np
import concourse.bass as bass
import concourse.mybir as mybir

F32 = mybir.dt.float32
BF16 = mybir.dt.bfloat16
I32 = mybir.dt.int32
AF = mybir.ActivationFunctionType
ALU = mybir.AluOpType
AX = mybir.AxisListType

SAME_ENG_SYNC = False


class Reg:
    __slots__ = ("name", "w", "r", "excl")

    def __init__(self, name):
        self.name = name
        self.excl = False
        self.w = None
        self.r = {}


def _tok_key(t):
    return t[0:2]


class Prog:
    ENGS = ["pe", "act", "dve", "pool", "sp"]

    def __init__(self, nc, esems, dsems):
        self.nc = nc
        self.esems = esems
        self.dsems = dsems
        self.dcount = {q: [0] * len(v) for q, v in dsems.items()}
        self.drr = {q: 0 for q in dsems}
        self.ecount = {e: 0 for e in self.ENGS}
        self.regs = {}
        self.reset_phase()

    def reg(self, *key):
        r = self.regs.get(key)
        if r is None:
            r = self.regs[key] = Reg(key)
        return r

    def reset_phase(self):
        self.ops = {e: [] for e in self.ENGS}
        for r in self.regs.values():
            r.w = None
            r.r = {}

    def _deps(self, reads, writes):
        deps = {}

        def add(t):
            k = _tok_key(t)
            if k not in deps or deps[k][2] < t[2]:
                deps[k] = t
        for r in reads:
            if r.w is not None:
                add(r.w)
        for w in writes:
            if w.w is not None:
                add(w.w)
            for t in w.r.values():
                add(t)
        return deps

    def _commit(self, tok, reads, writes):
        k = _tok_key(tok)
        for r in reads:
            r.r[k] = tok
        for w in writes:
            w.w = tok
            w.r = {}

    def op(self, eng, fn, reads=(), writes=()):
        writes = list(writes) + [r for r in reads if r.excl]
        reads = [r for r in reads if not r.excl]
        deps = self._deps(reads, writes)
        idx = len(self.ops[eng])
        tok = ("E", eng, idx)
        t = deps.get(("E", eng))
        if t is not None and (eng == "pe" or idx - t[2] > 3):
            deps.pop(("E", eng), None)
        self.ops[eng].append(dict(fn=fn, deps=list(deps.values()), dma=None, inc=False))
        self._commit(tok, reads, writes)

    def dma(self, q, fn, reads=(), writes=()):
        deps = self._deps(reads, writes)
        i = self.drr[q]
        self.drr[q] = (i + 1) % len(self.dsems[q])
        prev = self.dcount[q][i]
        if prev > 0:
            t = ("D", (q, i), prev)
            k = _tok_key(t)
            if k not in deps or deps[k][2] < prev:
                deps[k] = t
        self.dcount[q][i] = prev + 16
        tok = ("D", (q, i), prev + 16)
        self.ops[q].append(dict(fn=fn, deps=list(deps.values()), dma=(q, i), inc=False))
        self._commit(tok, reads, writes)

    def emit_phase(self, name=None):
        nc = self.nc
        finals = {e: [] for e in self.ENGS}
        for q in self.dsems:
            for i, c in enumerate(self.dcount[q]):
                if c > 0:
                    finals[q].append((self.dsems[q][i], c))
        for e in self.ENGS:
            for o in self.ops[e]:
                for t in o["deps"]:
                    if t[0] == "E":
                        self.ops[t[1]][t[2]]["inc"] = True
        cum = {}
        for e in self.ENGS:
            c = self.ecount[e]
            arr = []
            for o in self.ops[e]:
                if o["inc"] and o["dma"] is None:
                    c += 1
                arr.append(c)
            cum[e] = arr
        ops = self.ops
        esems, dsems = self.esems, self.dsems

        def run(eng_name, eng):
            waited = {}
            for o in ops[eng_name]:
                for t in o["deps"]:
                    if t[0] == "E":
                        sem = esems[t[1]]
                        val = cum[t[1]][t[2]]
                        key = ("E", t[1])
                    else:
                        sem = dsems[t[1][0]][t[1][1]]
                        val = t[2]
                        key = t[1]
                    if waited.get(key, -1) >= val:
                        continue
                    waited[key] = val
                    eng.wait_ge(sem, val)
                ins = o["fn"](eng)
                if o["dma"] is not None:
                    q, i = o["dma"]
                    ins.then_inc(dsems[q][i], 16)
                elif o["inc"]:
                    ins.then_inc(esems[eng_name], 1)
            for sem, c in finals[eng_name]:
                eng.wait_ge(sem, c)

        with nc.Block() as block:
            @block.tensor
            def _(e):
                run("pe", e)

            @block.scalar
            def _(e):
                run("act", e)

            @block.vector
            def _(e):
                run("dve", e)

            @block.gpsimd
            def _(e):
                run("pool", e)

            @block.sync
            def _(e):
                run("sp", e)
        for e in self.ENGS:
            if cum[e]:
                self.ecount[e] = cum[e][-1]
        n = {e: len(self.ops[e]) for e in self.ENGS}
        self.reset_phase()
        return n


from contextlib import ExitStack
import ml_dtypes
from concourse.bass_utils import run_bass_kernel_spmd

T = 2048; D = 2048; CT = 256; NT = 16; NTC = 2; CAP = 256; NE = 64
EPS = 1e-6
XS_ROWS = NE * CAP

_cache = {}
CSTOP = 99
LATSTOP = 99
COPYSEL = 0


class Tn:
    def __init__(self, P, t, name):
        self.t = t
        self.r = P.reg(name)


class _Stop(Exception):
    pass


def build_program(debug=False, stop=None, nheads=16):
    nc = bass.Bass("TRN2", target_bir_lowering=False)
    dr = lambda n, s, d=F32: nc.dram_tensor(n, s, d, kind="ExternalInput").ap()
    x = dr("x", [T, D]); ctx = dr("ctx", [CT, D]); ccol = dr("ccol", [128, 32])
    ada_w = dr("ada_w", [2, D, 6 * D]); ada_b = dr("ada_b", [2, 6 * D])
    n1g = dr("n1g", [2, D]); n2g = dr("n2g", [2, D]); fing = dr("fing", [1, D])
    w_in = dr("w_in", [16, D, 640]); hlb = dr("hlb", [2, 3, D]); ong = dr("ong", [1, 128]); w_out = dr("w_out", [D, D])
    w_pw1 = dr("w_pw1", [D, 2 * D]); wdw = dr("wdw", [128, 16, 31]); bdw = dr("bdw", [128, 16])
    lng = dr("lng", [128, 16]); lnb = dr("lnb", [128, 16]); w_pw2 = dr("w_pw2", [D, D])
    wr = dr("wr", [2, D, 72]); brr = dr("brr", [2, 72])
    if stop is None or stop >= 5:
        wg = dr("wg", [2, NE, D, 512]); wu = dr("wu", [2, NE, D, 512]); wd = dr("wd", [2, NE, 512, D])
    cst_f = dr("cst_f", [128, 9, 128])
    cst_b = dr("cst_b", [128, 3, 128], BF16)
    iot = dr("iot", [128, 65])
    out = nc.dram_tensor("out", [T, D], F32, kind="ExternalOutput").ap()
    Hs = nc.dram_tensor("Hs", [T, D], F32, kind=("ExternalOutput" if debug else "Internal")).ap()
    DER = nc.dram_tensor("DER", [2, 128, 6, D], F32, kind=("ExternalOutput" if debug else "Internal")).ap()
    DERC = nc.dram_tensor("DERC", [128, 2, D], F32, kind=("ExternalOutput" if debug else "Internal")).ap()
    OGT = nc.dram_tensor("OGT", [D, T], BF16, kind=("ExternalOutput" if debug else "Internal")).ap()
    Xs = nc.dram_tensor("Xs", [XS_ROWS + 128, D], BF16, kind=("ExternalOutput" if debug else "Internal")).ap()
    Ys = nc.dram_tensor("Ys", [XS_ROWS, D], BF16, kind=("ExternalOutput" if debug else "Internal")).ap()
    BBd = nc.dram_tensor("BBd", [T, D], BF16).ap()
    Vd = nc.dram_tensor("Vd", [D, T], F32, kind=("ExternalOutput" if debug else "Internal")).ap()
    dbg = {}
    if debug:
        for n in ["hmid0", "hend0", "hmid1"]:
            dbg[n] = nc.dram_tensor("dbg_" + n, [T, D], F32, kind="ExternalOutput").ap()

    try:
      with ExitStack() as es0:
          esems = {e: es0.enter_context(nc.semaphore("e_" + e)) for e in Prog.ENGS}
          dsems = {q: [es0.enter_context(nc.semaphore(f"d_{q}{i}")) for i in range(n)] for q, n in [("sp", 8), ("pool", 6), ("act", 2)]}
          P = Prog(nc, esems, dsems)
          es0.enter_context(nc.allow_low_precision("bf16 matmul operands, fp32 accumulation"))

          uid = [0]

          def SB(es, name, shape, dt=F32):
              uid[0] += 1
              name = f"{name}_{uid[0]}"
              return Tn(P, es.enter_context(nc.sbuf_tensor(name, shape, dt)), name)

          def PS(es, name, shape, dt=F32):
              uid[0] += 1
              name = f"{name}_{uid[0]}"
              t_ = Tn(P, es.enter_context(nc.psum_tensor(name, shape, dt)), name)
              t_.r.excl = True
              return t_

          phase_no = [0]

          def END_PHASE():
              P.emit_phase()
              phase_no[0] += 1
              if stop is not None and phase_no[0] >= stop:
                  raise _Stop()

          def rr(l):
              return [a.r if isinstance(a, Tn) else a for a in l]

          def ACT(out, in_, func, R, W, **kw):
              P.op("act", lambda e: e.activation(out=out, in_=in_, func=func, **kw), rr(R), rr(W))

          def TT(eng, out, in0, in1, op, R, W):
              P.op(eng, lambda e: e.tensor_tensor(out=out, in0=in0, in1=in1, op=op), rr(R), rr(W))

          def TS(eng, out, in0, s1, s2, op0, op1, R, W, **kw):
              if s2 is None:
                  P.op(eng, lambda e: e.tensor_scalar(out=out, in0=in0, scalar1=s1, scalar2=None, op0=op0, **kw), rr(R), rr(W))
              else:
                  P.op(eng, lambda e: e.tensor_scalar(out=out, in0=in0, scalar1=s1, scalar2=s2, op0=op0, op1=op1, **kw), rr(R), rr(W))

          def STT(eng, out, in0, sc, in1, op0, op1, R, W):
              P.op(eng, lambda e: e.scalar_tensor_tensor(out=out, in0=in0, scalar=sc, in1=in1, op0=op0, op1=op1), rr(R), rr(W))

          def CP(eng, out, in_, R, W):
              if eng == "act":
                  P.op("act", lambda e: e.copy(out=out, in_=in_), rr(R), rr(W))
              else:
                  P.op(eng, lambda e: e.tensor_copy(out=out, in_=in_), rr(R), rr(W))

          def MM(out, lhsT, rhs, start, stop, R, W):
              P.op("pe", lambda e: e.matmul(out, lhsT=lhsT, rhs=rhs, start=start, stop=stop), rr(R), rr(W))

          def TR(out, in_, ident, R, W):
              P.op("pe", lambda e: e.transpose(out=out, in_=in_, identity=ident), rr(R), rr(W))

          def DMA(q, out, in_, R, W):
              P.dma(q, lambda e: e.dma_start(out=out, in_=in_), rr(R), rr(W))

          def RED(eng, out, in_, op, R, W):
              P.op(eng, lambda e: e.tensor_reduce(out=out, in_=in_, axis=AX.X, op=op), rr(R), rr(W))

          def RECIP(out, in_, R, W):
              P.op("dve", lambda e: e.reciprocal(out=out, in_=in_), rr(R), rr(W))

          def MEMSET(eng, ap, v, W):
              P.op(eng, lambda e: e.memset(ap, v), [], rr(W))

          CF = SB(es0, "CF", [128, 9, 128]); CB = SB(es0, "CB", [128, 3, 128], BF16)
          IOT = SB(es0, "IOT", [128, 65]); EPSb = SB(es0, "EPSb", [128, 1])
          SLOTG = SB(es0, "SLOTG", [128, NT, 2], I32); GATES = SB(es0, "GATES", [128, NT, 2])
          identf = CF.t[:, 0, :]; LcT = [CF.t[:, 1, :], CF.t[:, 2, :]]; E2T = [CF.t[:, 3, :], CF.t[:, 4, :]]
          onesf = CF.t[:, 7, :]; IND = CF.t[:, 8, 0:2]
          identb = CB.t[:, 0, :]; onesb = CB.t[:, 1, :]; UTs = CB.t[:, 2, :]

          def load_consts():
              DMA("sp", CF.t[:], cst_f[:, :, :], [], [CF])
              DMA("sp", CB.t[:], cst_b[:, :, :], [], [CB])
              DMA("sp", IOT.t[:], iot[:, :], [], [IOT])
              MEMSET("pool", EPSb.t[:], EPS, [EPSb])

          def rms_rstd(xt_ap, ss, rstd, junk, R, n=D):
              ACT(junk.t[:, 0:n], xt_ap, AF.Square, R, [junk, ss], accum_out=ss.t[:])
              ACT(rstd.t[:], ss.t[:], AF.Sqrt, [ss, EPSb], [rstd], scale=1.0 / n, bias=EPSb.t[:])
              RECIP(rstd.t[:], rstd.t[:], [rstd], [rstd])

          with ExitStack() as es:
              load_consts()
              cc = SB(es, "cc", [128, 32]); sc = SB(es, "sc", [128, 32])
              Srep = SB(es, "Srep", [128, 32, 128])
              wa = [SB(es, f"wa{i}", [128, 16, 512]) for i in range(2)]
              ab = [SB(es, f"ab{i}", [128, 512]) for i in range(2)]
              MODt = SB(es, "MODt", [128, 6 * D]); MODc = SB(es, "MODc", [128, 2 * D])
              gt = [SB(es, f"gt{i}", [128, D]) for i in range(2)]
              pa = [PS(es, f"pa{i}", [128, 512]) for i in range(2)]
              pc = [PS(es, f"pc{i}", [128, 512]) for i in range(2)]
              DMA("sp", cc.t[:], ccol[:, :], [], [cc])
              ACT(sc.t[:], cc.t[:], AF.Silu, [cc], [sc])
              for k in range(32):
                  ACT(Srep.t[:, k, :], onesf, AF.Copy, [CF, sc], [Srep], scale=sc.t[:, k:k + 1])
              for l in range(2):
                  awv = ada_w[l].rearrange("(kc p) n -> p kc n", p=128)
                  for j in range(24):
                      w_ = wa[j % 2]; a_ = ab[j % 2]; p_ = pa[j % 2]; q_ = pc[j % 2]
                      DMA("sp", w_.t[:], awv[:, :, j * 512:(j + 1) * 512], [], [w_])
                      DMA("act", a_.t[:], ada_b[l:l + 1, j * 512:(j + 1) * 512].partition_broadcast(128), [], [a_])
                      for kc in range(16):
                          MM(p_.t[:], Srep.t[:, kc, :], w_.t[:, kc, :], kc == 0, kc == 15, [Srep, w_], [p_])
                      TT("dve", MODt.t[:, j * 512:(j + 1) * 512], p_.t[:], a_.t[:], ALU.add, [p_, a_], [MODt])
                      if l == 0 and j < 8:
                          for kc in range(16):
                              MM(q_.t[:], Srep.t[:, 16 + kc, :], w_.t[:, kc, :], kc == 0, kc == 15, [Srep, w_], [q_])
                          TT("dve", MODc.t[:, j * 512:(j + 1) * 512], q_.t[:], a_.t[:], ALU.add, [q_, a_], [MODc])
                  DMA("act", gt[0].t[:], n1g[l:l + 1, :].partition_broadcast(128), [], [gt[0]])
                  DMA("act", gt[1].t[:], n2g[l:l + 1, :].partition_broadcast(128), [], [gt[1]])
                  STT("dve", MODt.t[:, D:2 * D], MODt.t[:, D:2 * D], 1.0, gt[0].t[:], ALU.add, ALU.mult, [MODt, gt[0]], [MODt])
                  STT("dve", MODt.t[:, 4 * D:5 * D], MODt.t[:, 4 * D:5 * D], 1.0, gt[1].t[:], ALU.add, ALU.mult, [MODt, gt[1]], [MODt])
                  DMA("sp", DER[l].rearrange("p s d -> p (s d)"), MODt.t[:], [MODt], [P.reg("DER")])
                  if l == 0:
                      STT("dve", MODc.t[:, D:2 * D], MODc.t[:, D:2 * D], 1.0, gt[0].t[:], ALU.add, ALU.mult, [MODc, gt[0]], [MODc])
                      DMA("sp", DERC.rearrange("p s d -> p (s d)"), MODc.t[:], [MODc], [P.reg("DERC")])
              END_PHASE()

          def norm_mod_transpose(es, xt, A1, B1, dstT, col0, bufs):
              ss, rstd, junk, xn, ab_, pt = bufs
              rms_rstd(xt.t[:], ss, rstd, junk, [xt])
              ACT(xn.t[:], xt.t[:], AF.Copy, [xt, rstd], [xn], scale=rstd.t[:])
              TT("dve", xn.t[:], xn.t[:], A1.t[:], ALU.mult, [xn, A1], [xn])
              TT("pool", ab_.t[:], xn.t[:], B1.t[:], ALU.add, [xn, B1], [ab_])
              for hf in range(2):
                  p_ = pt[hf]
                  for k in range(8):
                      kc = hf * 8 + k
                      TR(p_.t[:, k, :], ab_.t[:, kc * 128:(kc + 1) * 128], identb, [ab_, CB], [p_])
                  CP("dve" if hf == 0 else "act", dstT.t[:, hf * 8:(hf + 1) * 8, col0:col0 + 128], p_.t[:], [p_], [dstT])

          with ExitStack() as esBC:
              aT = SB(esBC, "aT", [128, 16, CT + T], BF16)
              with ExitStack() as es:
                  A1 = SB(es, "A1", [128, D]); B1 = SB(es, "B1", [128, D]); A1c = SB(es, "A1c", [128, D]); B1c = SB(es, "B1c", [128, D])
                  xt = [SB(es, f"xt{i}", [128, D]) for i in range(2)]
                  ss = SB(es, "ss", [128, 1]); rstd = SB(es, "rstd", [128, 1]); junk = SB(es, "junk", [128, D], BF16)
                  xn = SB(es, "xn", [128, D]); ab_ = SB(es, "abf", [128, D], BF16)
                  pt = [PS(es, f"pt{i}", [128, 8, 128], BF16) for i in range(2)]
                  DMA("sp", B1.t[:], DER[0, :, 0, :], [], [B1]); DMA("sp", A1.t[:], DER[0, :, 1, :], [], [A1])
                  DMA("sp", B1c.t[:], DERC[:, 0, :], [], [B1c]); DMA("sp", A1c.t[:], DERC[:, 1, :], [], [A1c])
                  for i in range(NTC + NT):
                      x_ = xt[i % 2]
                      src = ctx[i * 128:(i + 1) * 128, :] if i < NTC else x[(i - NTC) * 128:(i - NTC + 1) * 128, :]
                      DMA("sp", x_.t[:], src, [], [x_])
                      norm_mod_transpose(es, x_, A1c if i < NTC else A1, B1c if i < NTC else B1, aT, i * 128, (ss, rstd, junk, xn, ab_, pt))
                  END_PHASE()

              with ExitStack() as es:
                  NTT = NTC + NT
                  wsl = [SB(es, f"wsl{i}", [128, 16, 640], BF16) for i in range(2)]
                  lbr = SB(es, "lbr", [128, 2, 3, 128])
                  lb2 = [SB(es, "lb2", [128, 2, 128])] * 2; oml2 = [SB(es, "oml2", [128, 2, 128])] * 2
                  ongb = SB(es, "ongb", [128, 128])
                  QTb = [SB(es, f"QTb{i}", [128, NT, 128], BF16) for i in range(2)]
                  ER = [SB(es, f"ER{i}", [128, NTT, 4]) for i in range(2)]; ERD = [SB(es, f"ERD{i}", [128, NTT, 2]) for i in range(2)]
                  DSb = [SB(es, f"DSb{i}", [128, NTT, 128]) for i in range(2)]
                  OPb = [SB(es, f"OPb{i}", [128, NT, 128]) for i in range(2)]
                  SGS = [SB(es, f"SGS{i}", [128, NT, 128]) for i in range(2)]
                  OGh = [SB(es, "OGh", [128, T], BF16)] * 2
                  qs = [SB(es, f"qs{i}", [128, 128]) for i in range(2)]
                  sgt = [SB(es, "sgt", [128, 128])] * 2
                  vb = [SB(es, f"vb{i}", [128, 128], BF16) for i in range(3)]
                  sig = [SB(es, "sig", [128, 256])] * 2
                  gl = [SB(es, f"gl{i}", [128, 256]) for i in range(2)]
                  kk = [SB(es, f"kk{i}", [128, 256]) for i in range(2)]
                  Eb = [SB(es, "Eb", [128, 256])] * 2
                  Ei = [SB(es, "Ei", [128, 256])] * 2
                  E2 = [SB(es, "E2", [128, 256])] * 2
                  qt = [SB(es, f"qt{i}", [128, 256], BF16) for i in range(2)]
                  kt = [SB(es, f"kt{i}", [128, 256], BF16) for i in range(2)]
                  kh = [SB(es, f"kh{i}", [128, 256], BF16) for i in range(2)]
                  qTt = [SB(es, f"qTt{i}", [128, 2, 128], BF16) for i in range(2)]
                  kTt = [SB(es, f"kTt{i}", [128, 2, 128], BF16) for i in range(2)]
                  sT = [SB(es, f"sT{i}", [128, 2, 128], BF16) for i in range(2)]
                  kTz = [SB(es, f"kTz{i}", [128, 2, 128], BF16) for i in range(2)]
                  SmF = [SB(es, f"SmF{i}", [128, 128], BF16) for i in range(2)]
                  for i_ in range(2):
                      MEMSET("pool", kTz[i_].t[:], 0.0, [kTz[i_]])
                  SstF = SB(es, "SstF", [128, 128]); SstB = SB(es, "SstB", [128, 128]); SmB = SB(es, "SmB", [128, 128], BF16)
                  sqj = SB(es, "sqj", [128, 128]); ssq = SB(es, "ssq", [128, NT]); rs16 = SB(es, "rs16", [128, NT])
                  on = [SB(es, "on", [128, 128])] * 2; onb = [SB(es, "onb", [128, 128], BF16)] * 2
                  PQ = [PS(es, f"PQ{i}", [128, 512]) for i in range(2)]
                  PQ2 = PS(es, "PQ2", [128, 4, 128])
                  PC = PS(es, "PC", [128, 4, 128])
                  PT = PS(es, "PT", [128, 8, 128], BF16)
                  PSs = PS(es, "PSs", [128, 4, 128])
                  PD = PS(es, "PD", [128, 4, 128])
                  PCH = PS(es, "PCH", [128, 4, 128])
                  DMA("act", ongb.t[:], ong[0:1, :].partition_broadcast(128), [], [ongb])
                  def load_head_w(h):
                      w_ = wsl[h % 2]
                      DMA("pool", w_.t[:], w_in[h].rearrange("(kc p) n -> p kc n", p=128), [], [w_])

                  def head_prologue(h):
                      hb = h % 2
                      for d_ in range(2):
                          for s_ in range(3):
                              DMA("act", lbr.t[:, d_, s_, :], hlb[d_, s_:s_ + 1, h * 128:(h + 1) * 128].partition_broadcast(128), [], [lbr])
                      lbrf = lbr.t[:].rearrange("p a s k -> p (a s k)")
                      ACT(lbrf, lbrf, AF.Exp, [lbr], [lbr])
                      lsum = kk[0]; lsv = kk[0].t[:].rearrange("p (a k) -> p a k", k=128)
                      TT("dve", lsv, lbr.t[:, :, 0, :], lbr.t[:, :, 1, :], ALU.add, [lbr], [lsum])
                      TT("dve", lsv, lsv, lbr.t[:, :, 2, :], ALU.add, [lbr, lsum], [lsum])
                      RECIP(lsv, lsv, [lsum], [lsum])
                      TT("dve", lb2[hb].t[:], lbr.t[:, :, 0, :], lsv, ALU.mult, [lbr, lsum], [lb2[hb]])
                      TS("dve", oml2[hb].t[:], lb2[hb].t[:], -1.0, 1.0, ALU.mult, ALU.add, [lb2[hb]], [oml2[hb]])
                      MEMSET("pool", SstF.t[:], 0.0, [SstF])

                  def S1(h, i, part):
                      hb = h % 2; b = i % 2; lat = i >= NTC; li = i - NTC
                      w_ = wsl[hb]; pq = PQ[b]; v_ = vb[i % 3]
                      lbf = lb2[hb].t[:].rearrange("p a k -> p (a k)"); omf = oml2[hb].t[:].rearrange("p a k -> p (a k)")
                      if part == 0:
                          for kc in range(16):
                              MM(pq.t[:], aT.t[:, kc, i * 128:(i + 1) * 128], w_.t[:, kc, 0:512], kc == 0, kc == 15, [aT, w_], [pq])
                          if lat:
                              for kc in range(16):
                                  MM(PQ2.t[:, b, :], aT.t[:, kc, i * 128:(i + 1) * 128], w_.t[:, kc, 512:640], kc == 0, kc == 15, [aT, w_], [PQ2])
                          return
                      ACT(sig[b].t[:], pq.t[:, 256:512], AF.Sigmoid, [pq], [sig[b]])
                      if lat:
                          ACT(qs[b].t[:], pq.t[:, 0:128], AF.Sigmoid, [pq], [qs[b]])
                          ACT(sgt[b].t[:], PQ2.t[:, b, :], AF.Sigmoid, [PQ2], [sgt[b]])
                      CP("dve", v_.t[:], pq.t[:, 128:256], [pq], [v_])
                      if lat:
                          TT("dve", qs[b].t[:], qs[b].t[:], pq.t[:, 0:128], ALU.mult, [qs[b], pq], [qs[b]])
                          TT("dve", SGS[hb].t[:, li, :], sgt[b].t[:], PQ2.t[:, b, :], ALU.mult, [sgt[b], PQ2], [SGS[hb]])
                      TT("dve", sig[b].t[:], sig[b].t[:], omf, ALU.mult, [sig[b], oml2[hb]], [sig[b]])
                      TT("pool", sig[b].t[:], sig[b].t[:], lbf, ALU.add, [sig[b], lb2[hb]], [sig[b]])
                      ACT(gl[b].t[:], sig[b].t[:], AF.Ln, [sig[b]], [gl[b]])
                      TS("pool", kk[b].t[:], sig[b].t[:], -1.0, 1.0, ALU.mult, ALU.add, [sig[b]], [kk[b]])

                  def S2(h, i, part):
                      hb = h % 2; b = i % 2; lat = i >= NTC; v_ = vb[i % 3]
                      if part == 0:
                          for d_ in range(2):
                              if lat:
                                  MM(PC.t[:, d_, :], LcT[d_], gl[b].t[:, d_ * 128:(d_ + 1) * 128], True, True, [CF, gl[b]], [PC])
                              MM(PC.t[:, 2 + d_, :], E2T[d_], gl[b].t[:, d_ * 128:(d_ + 1) * 128], True, True, [CF, gl[b]], [PC])
                              MM(PSs.t[:, 3, 2 * d_:2 * d_ + 2], gl[b].t[:, d_ * 128:(d_ + 1) * 128], IND, True, True, [CF, gl[b]], [PSs])
                          return
                      ACT(E2[b].t[:], PC.t[:, 2:4, :].rearrange("p a k -> p (a k)"), AF.Exp, [PC], [E2[b]])
                      ACT(ER[hb].t[:, i, :], PSs.t[:, 3, 0:4], AF.Exp, [PSs], [ER[hb]])
                      TT("pool", kh[b].t[:], kk[b].t[:], E2[b].t[:], ALU.mult, [kk[b], E2[b]], [kh[b]])
                      for d_ in range(2):
                          MM(PD.t[:, d_, :], kh[b].t[:, d_ * 128:(d_ + 1) * 128], v_.t[:], True, True, [kh[b], v_], [PD])
                      if lat:
                          ACT(Eb[b].t[:], PC.t[:, 0:2, :].rearrange("p a k -> p (a k)"), AF.Exp, [PC], [Eb[b]])
                          ACT(Ei[b].t[:], PC.t[:, 0:2, :].rearrange("p a k -> p (a k)"), AF.Exp, [PC], [Ei[b]], scale=-1.0)
                      TT("pool", ERD[hb].t[:, i, 0:1], ER[hb].t[:, i, 0:1], ER[hb].t[:, i, 1:2], ALU.mult, [ER[hb]], [ERD[hb]])
                      TT("pool", ERD[hb].t[:, i, 1:2], ER[hb].t[:, i, 3:4], ER[hb].t[:, i, 2:3], ALU.mult, [ER[hb]], [ERD[hb]])
                      if lat:
                          ACT(SmF[b].t[:], SstF.t[:], AF.Copy, [SstF, ER[hb]], [SmF[b]], scale=ER[hb].t[:, i, 0:1])
                      STT("dve", SstF.t[:], SstF.t[:], ERD[hb].t[:, i, 0:1], PD.t[:, 0, :], ALU.mult, ALU.add, [SstF, ERD[hb], PD], [SstF])
                      CP("act", DSb[hb].t[:, i, :], PD.t[:, 1, :], [PD], [DSb[hb]])
                      if lat:
                          for d_ in range(2):
                              TT("pool", qt[b].t[:, d_ * 128:(d_ + 1) * 128], qs[b].t[:], Eb[b].t[:, d_ * 128:(d_ + 1) * 128], ALU.mult, [qs[b], Eb[b]], [qt[b]])
                          TT("dve", kt[b].t[:], kk[b].t[:], Ei[b].t[:], ALU.mult, [kk[b], Ei[b]], [kt[b]])

                  def S3(h, i, part):
                      hb = h % 2; b = i % 2; lat = i >= NTC; li = i - NTC; v_ = vb[i % 3]
                      if not lat:
                          return
                      if part == 0:
                          for d_ in range(2):
                              TR(PT.t[:, d_, :], qt[b].t[:, d_ * 128:(d_ + 1) * 128], identb, [qt[b], CB], [PT])
                              TR(PT.t[:, 2 + d_, :], kt[b].t[:, d_ * 128:(d_ + 1) * 128], identb, [kt[b], CB], [PT])
                          return
                      CP("dve", qTt[b].t[:], PT.t[:, 0:2, :], [PT], [qTt[b]])
                      CP("act", kTt[b].t[:], PT.t[:, 2:4, :], [PT], [kTt[b]])
                      CP("dve", kTz[b].t[:, 0, 0:64], PT.t[:, 2, 0:64], [PT], [kTz[b]])
                      CP("dve", kTz[b].t[:, 1, 64:128], PT.t[:, 3, 64:128], [PT], [kTz[b]])
                      CP("pool", QTb[hb].t[:, li, :], qTt[b].t[:, 1, :], [qTt[b]], [QTb[hb]])
                      MM(PSs.t[:, 0, 64:128], kTt[b].t[:, 0, :], qTt[b].t[:, 0, 64:128], True, True, [kTt[b], qTt[b]], [PSs])
                      MM(PSs.t[:, 0, 0:64], kTz[b].t[:, 0, :], qTt[b].t[:, 0, 0:64], True, True, [kTz[b], qTt[b]], [PSs])
                      MM(PSs.t[:, 1, 0:64], kTt[b].t[:, 1, :], qTt[b].t[:, 1, 0:64], True, True, [kTt[b], qTt[b]], [PSs])
                      MM(PSs.t[:, 1, 64:128], kTz[b].t[:, 1, :], qTt[b].t[:, 1, 64:128], True, True, [kTz[b], qTt[b]], [PSs])
                      TT("dve", sT[b].t[:], PSs.t[:, 0:2, :], CF.t[:, 5:7, :], ALU.mult, [PSs, CF], [sT[b]])
                      MM(PSs.t[:, 2, :], sT[b].t[:, 0, :], v_.t[:], True, False, [sT[b], v_], [PSs])
                      MM(PSs.t[:, 2, :], sT[b].t[:, 1, :], v_.t[:], False, False, [sT[b], v_], [PSs])
                      MM(PSs.t[:, 2, :], qTt[b].t[:, 0, :], SmF[b].t[:], False, True, [qTt[b], SmF[b]], [PSs])
                      CP("act", OPb[hb].t[:, li, :], PSs.t[:, 2, :], [PSs], [OPb[hb]])

                  bw_order = [1, 0] + list(range(NTT - 1, NTC - 1, -1))

                  def deferred(h, u, part):
                      hb = h % 2
                      i = bw_order[u]
                      li = i - NTC; k = li % 2
                      if part == 0:
                          if u == 0:
                              MEMSET("pool", SstB.t[:], 0.0, [SstB])
                          if i >= NTC:
                              ACT(SmB.t[:], SstB.t[:], AF.Copy, [SstB, ER[hb]], [SmB], scale=ER[hb].t[:, i, 3:4])
                              MM(PCH.t[:, 0, :], QTb[hb].t[:, li, :], SmB.t[:], True, True, [QTb[hb], SmB], [PCH])
                          return
                      if i >= NTC:
                          TT("dve", OPb[hb].t[:, li, :], OPb[hb].t[:, li, :], PCH.t[:, 0, :], ALU.add, [OPb[hb], PCH], [OPb[hb]])
                      STT("dve", SstB.t[:], SstB.t[:], ERD[hb].t[:, i, 1:2], DSb[hb].t[:, i, :], ALU.mult, ALU.add, [SstB, ERD[hb], DSb[hb]], [SstB])
                      if i >= NTC:
                          TT("pool", sqj.t[:], OPb[hb].t[:, li, :], OPb[hb].t[:, li, :], ALU.mult, [OPb[hb]], [sqj])
                          RED("dve", ssq.t[:, li:li + 1], sqj.t[:], ALU.add, [sqj], [ssq])
                          ACT(rs16.t[:, li:li + 1], ssq.t[:, li:li + 1], AF.Ln, [ssq, EPSb], [rs16], scale=1.0 / 128, bias=EPSb.t[:])
                          ACT(rs16.t[:, li:li + 1], rs16.t[:, li:li + 1], AF.Exp, [rs16], [rs16], scale=-0.5)
                          STT("dve", on[k].t[:], OPb[hb].t[:, li, :], rs16.t[:, li:li + 1], SGS[hb].t[:, li, :], ALU.mult, ALU.mult, [OPb[hb], rs16, SGS[hb]], [on[k]])
                          TT("pool", onb[k].t[:], on[k].t[:], ongb.t[:], ALU.mult, [on[k], ongb], [onb[k]])
                          TR(PT.t[:, 4 + li % 4, :], onb[k].t[:], identb, [onb[k], CB], [PT])
                          CP("act", OGh[hb].t[:, li * 128:(li + 1) * 128], PT.t[:, 4 + li % 4, :], [PT], [OGh[hb]])
                      if u == NTT - 1:
                          DMA("sp", OGT[h * 128:(h + 1) * 128, :], OGh[hb].t[:], [OGh[hb]], [P.reg("OGT")])

                  load_head_w(0)
                  for h in range(nheads + 1):
                      if h < nheads:
                          if h + 1 < nheads:
                              load_head_w(h + 1)
                          head_prologue(h)
                      for step in range(NTT + 3):
                          cur = h < nheads
                          d_on = h >= 1 and step < NTT
                          ok_ = lambda t: cur and 0 <= t < NTT
                          if ok_(step):
                              S1(h, step, 0)
                          if ok_(step - 2):
                              S2(h, step - 2, 0)
                          if ok_(step - 3):
                              S3(h, step - 3, 0)
                          if d_on:
                              deferred(h - 1, step, 0)
                          if ok_(step - 1):
                              S1(h, step - 1, 1)
                          if ok_(step - 2):
                              S2(h, step - 2, 1)
                          if ok_(step - 3):
                              S3(h, step - 3, 1)
                          if d_on:
                              deferred(h - 1, step, 1)
                  END_PHASE()

          def post_mixer(l, wmat, hin, dbg_name):
              with ExitStack() as es:
                  atl = [SB(es, f"atl{i}", [128, 16, 128], BF16) for i in range(2)]
                  OGTv = OGT.rearrange("(kc p) t -> p kc t", p=128)
                  wo = SB(es, "wo", [128, 16, D], BF16)
                  G1 = SB(es, "G1", [128, D]); A2 = SB(es, "A2", [128, D]); B2 = SB(es, "B2", [128, D])
                  WR = SB(es, "WR", [128, 16, 72]); BR = SB(es, "BR", [128, 72])
                  xt = [SB(es, f"pxt{i}", [128, D]) for i in range(2)]
                  hn = [SB(es, "phn", [128, D])] * 2
                  bfl = SB(es, "pbfl", [128, D]); bb = [SB(es, f"pbb{i}", [128, D], BF16) for i in range(2)]
                  LG = SB(es, "pLG", [128, NT, 72]); sm2 = SB(es, "psm2", [128, 26, NT]); t8 = SB(es, "pt8", [128, NT, 8])
                  PRs = SB(es, "pPRs", [128, NT, 64]); SIDX = SB(es, "pSIDX", [128, NT, 2], I32)
                  ss = SB(es, "pss", [128, 1]); rstd = SB(es, "prstd", [128, 1]); junkb = SB(es, "pjunkb", [128, D], BF16)
                  bT = SB(es, "pbT", [128, 16, 128])
                  As = SB(es, "pAs", [128, NT, 64], BF16)
                  oh1 = SB(es, "poh1", [128, NT, 8])
                  sel = SB(es, "psel", [128, NT, 8]); sel2 = SB(es, "psel2", [128, NT, 8]); oha = SB(es, "poha", [128, NT, 8]); ohb = SB(es, "pohb", [128, NT, 8])
                  Aa = SB(es, "pAa", [128, NT, 64]); Ab = SB(es, "pAb", [128, NT, 64]); t64 = SB(es, "pt64", [128, NT, 64])
                  PY = [PS(es, f"PY{i}", [128, 512]) for i in range(4)]
                  PTf = [PS(es, f"PTf{i}", [128, 4, 128]) for i in range(2)]
                  PL = PS(es, "PL", [128, 512]); PR = PS(es, "PRk", [128, 512])
                  DMA("pool", wo.t[:], wmat.rearrange("(kc p) n -> p kc n", p=128), [], [wo])
                  DMA("sp", G1.t[:], DER[l, :, 2, :], [], [G1]); DMA("sp", B2.t[:], DER[l, :, 3, :], [], [B2]); DMA("sp", A2.t[:], DER[l, :, 4, :], [], [A2])
                  DMA("sp", WR.t[:], wr[l].rearrange("(kc p) n -> p kc n", p=128), [], [WR])
                  DMA("act", BR.t[:], brr[l:l + 1, :].partition_broadcast(128), [], [BR])
                  c = lambda k: sm.t[:, k:k + 1]
                  for i in range(NT):
                      b = i % 2
                      x_ = xt[b]; h_ = hn[b]
                      DMA("sp", x_.t[:], hin[i * 128:(i + 1) * 128, :], [P.reg("HIN", i)], [x_])
                      at_ = atl[b]
                      DMA("sp", at_.t[:], OGTv[:, :, i * 128:(i + 1) * 128], [P.reg("OGT")], [at_])
                      for ch in range(4):
                          for kc in range(16):
                              MM(PY[ch].t[:], at_.t[:, kc, :], wo.t[:, kc, ch * 512:(ch + 1) * 512], kc == 0, kc == 15, [at_, wo], [PY[ch]])
                          TT("dve", h_.t[:, ch * 512:(ch + 1) * 512], PY[ch].t[:], G1.t[:, ch * 512:(ch + 1) * 512], ALU.mult, [PY[ch], G1], [h_])
                      TT("pool", h_.t[:], h_.t[:], x_.t[:], ALU.add, [h_, x_], [h_])
                      DMA("sp", Hs[i * 128:(i + 1) * 128, :], h_.t[:], [h_], [P.reg("HS", i)])
                      if debug:
                          DMA("sp", dbg[dbg_name][i * 128:(i + 1) * 128, :], h_.t[:], [h_], [P.reg("DBG", i)])
                      rms_rstd(h_.t[:], ss, rstd, junkb, [h_])
                      ACT(bfl.t[:], h_.t[:], AF.Copy, [h_, rstd], [bfl], scale=rstd.t[:])
                      TT("dve", bfl.t[:], bfl.t[:], A2.t[:], ALU.mult, [bfl, A2], [bfl])
                      TT("pool", bfl.t[:], bfl.t[:], B2.t[:], ALU.add, [bfl, B2], [bfl])
                      CP("act", bb[b].t[:], bfl.t[:], [bfl], [bb[b]])
                      DMA("sp", BBd[i * 128:(i + 1) * 128, :], bb[b].t[:], [bb[b]], [P.reg("BBD", i)])
                      for q4 in range(4):
                          p_ = PTf[q4 % 2]
                          for k in range(4):
                              kc = q4 * 4 + k
                              TR(p_.t[:, k, :], bfl.t[:, kc * 128:(kc + 1) * 128], identf, [bfl, CF], [p_])
                          CP("dve" if q4 % 2 == 0 else "act", bT.t[:, q4 * 4:(q4 + 1) * 4, :], p_.t[:], [p_], [bT])
                      for kc in range(16):
                          MM(PL.t[:, 0:72], bT.t[:, kc, :], WR.t[:, kc, :], kc == 0, kc == 15, [bT, WR], [PL])
                      TT("dve", LG.t[:, i, :], PL.t[:, 0:72], BR.t[:], ALU.add, [PL, BR], [LG])
                  def bc8(ap2):
                      return ap2.unsqueeze(2).broadcast_to([128, NT, 8])
                  L1 = LG.t[:, :, 0:8]
                  c2 = lambda k: sm2.t[:, k, :]
                  io8 = IOT.t[:, 0:8].unsqueeze(1).broadcast_to([128, NT, 8])
                  io64 = IOT.t[:, 0:64].unsqueeze(1).broadcast_to([128, NT, 64])
                  dumpc = IOT.t[:, 64:65].broadcast_to([128, NT])
                  RED("dve", c2(0), L1, ALU.max, [LG], [sm2])
                  TT("dve", oh1.t[:], L1, bc8(c2(0)), ALU.is_equal, [LG, sm2], [oh1])
                  TT("dve", t8.t[:], L1, bc8(c2(0)), ALU.subtract, [LG, sm2], [t8])
                  ACT(t8.t[:], t8.t[:], AF.Exp, [t8], [t8])
                  RED("dve", c2(2), t8.t[:], ALU.add, [t8], [sm2])
                  RECIP(c2(3), c2(2), [sm2], [sm2])
                  for g in range(8):
                      TT("dve", t8.t[:], LG.t[:, :, 8 + g * 8:16 + g * 8], bc8(oh1.t[:, :, g]), ALU.mult, [LG, oh1], [t8])
                      if g == 0:
                          CP("dve", sel.t[:], t8.t[:], [t8], [sel])
                      else:
                          TT("dve", sel.t[:], sel.t[:], t8.t[:], ALU.add, [sel, t8], [sel])
                  RED("dve", c2(4), sel.t[:], ALU.max, [sel], [sm2])
                  TT("dve", oha.t[:], sel.t[:], bc8(c2(4)), ALU.is_equal, [sel, sm2], [oha])
                  STT("dve", sel2.t[:], oha.t[:], -1e30, sel.t[:], ALU.mult, ALU.add, [oha, sel], [sel2])
                  RED("dve", c2(6), sel2.t[:], ALU.max, [sel2], [sm2])
                  TT("dve", ohb.t[:], sel2.t[:], bc8(c2(6)), ALU.is_equal, [sel2, sm2], [ohb])
                  TT("dve", c2(7), c2(6), c2(4), ALU.subtract, [sm2], [sm2])
                  ACT(c2(7), c2(7), AF.Exp, [sm2], [sm2])
                  TS("dve", c2(8), c2(7), 1.0, None, ALU.add, None, [sm2], [sm2])
                  RECIP(c2(8), c2(8), [sm2], [sm2])
                  TT("dve", c2(9), c2(3), c2(8), ALU.mult, [sm2], [sm2])
                  TT("dve", c2(10), c2(9), c2(7), ALU.mult, [sm2], [sm2])
                  for src_, dst_ in ((oh1, 11), (oha, 12), (ohb, 13)):
                      TT("dve", t8.t[:], src_.t[:], io8, ALU.mult, [src_, IOT], [t8])
                      RED("dve", c2(dst_), t8.t[:], ALU.add, [t8], [sm2])
                  STT("dve", c2(14), c2(11), 8.0, c2(12), ALU.mult, ALU.add, [sm2], [sm2])
                  STT("dve", c2(15), c2(11), 8.0, c2(13), ALU.mult, ALU.add, [sm2], [sm2])
                  TT("dve", Aa.t[:], io64, c2(14).unsqueeze(2).broadcast_to([128, NT, 64]), ALU.is_equal, [IOT, sm2], [Aa])
                  TT("dve", Ab.t[:], io64, c2(15).unsqueeze(2).broadcast_to([128, NT, 64]), ALU.is_equal, [IOT, sm2], [Ab])
                  TT("dve", As.t[:], Aa.t[:], Ab.t[:], ALU.add, [Aa, Ab], [As])
                  for i in range(NT):
                      pr_ = PY[i // 8]
                      for j in range(i + 1):
                          MM(pr_.t[:, (i % 8) * 64:(i % 8 + 1) * 64], UTs if j == i else onesb, As.t[:, j, :], j == 0, j == i, [As, CB], [pr_])
                  for hf_ in range(2):
                      CP("dve" if hf_ == 0 else "act", PRs.t[:, hf_ * 8:(hf_ + 1) * 8, :], PY[hf_].t[:].rearrange("p (a e) -> p a e", e=64), [PY[hf_]], [PRs])
                  TT("dve", t64.t[:], PRs.t[:], Aa.t[:], ALU.mult, [PRs, Aa], [t64]); RED("dve", c2(16), t64.t[:], ALU.add, [t64], [sm2])
                  TT("dve", t64.t[:], PRs.t[:], Ab.t[:], ALU.mult, [PRs, Ab], [t64]); RED("dve", c2(17), t64.t[:], ALU.add, [t64], [sm2])
                  for k in range(2):
                      rk = c2(16 + k); ok = c2(18 + k); sg_ = c2(20 + k); ssc = c2(22 + k); ek = c2(14 + k); gk = c2(9 + k)
                      TS("dve", ok, rk, float(CAP) - 0.5, None, ALU.is_lt, None, [sm2], [sm2])
                      TS("dve", c2(24), rk, float(CAP - 1), None, ALU.min, None, [sm2], [sm2])
                      STT("dve", sg_, ek, float(CAP), c2(24), ALU.mult, ALU.add, [sm2], [sm2])
                      TT("dve", c2(25), sg_, dumpc, ALU.subtract, [sm2, IOT], [sm2])
                      TT("dve", c2(25), c2(25), ok, ALU.mult, [sm2], [sm2])
                      TT("dve", ssc, c2(25), dumpc, ALU.add, [sm2, IOT], [sm2])
                      TT("dve", GATES.t[:, :, k], gk, ok, ALU.mult, [sm2], [GATES])
                      CP("dve", SLOTG.t[:, :, k], sg_, [sm2], [SLOTG])
                      CP("dve", SIDX.t[:, :, k], ssc, [sm2], [SIDX])
                  for i in range(NT):
                      b = i % 2
                      DMA("sp", bb[b].t[:], BBd[i * 128:(i + 1) * 128, :], [P.reg("BBD", i)], [bb[b]])
                      for k in range(2):
                          P.dma("pool", lambda e, i=i, k=k, b=b: e.indirect_dma_start(
                              out=Xs[:, :], out_offset=bass.IndirectOffsetOnAxis(ap=SIDX.t[:, i, k:k + 1], axis=0),
                              in_=bb[b].t[:], in_offset=None),
                              rr([bb[b], SIDX]), [P.reg("XS")])
                  END_PHASE()

          def experts(l):
              with ExitStack() as es:
                  WG = [SB(es, f"WG{i}", [128, 16, 512], BF16) for i in range(2)]
                  WU = [SB(es, f"WU{i}", [128, 16, 512], BF16) for i in range(2)]
                  WD = [SB(es, f"WD{i}", [128, 4, D], BF16) for i in range(2)]
                  xe = [SB(es, f"xe{i}", [128, 2, D], BF16) for i in range(2)]
                  xeT = [SB(es, f"xeT{i}", [128, 2, 16, 128], BF16) for i in range(2)]
                  sgl = [SB(es, f"sgl{i}", [128, 512]) for i in range(2)]
                  hh = [SB(es, f"hh{i}", [128, 512], BF16) for i in range(2)]
                  hT = [SB(es, f"hT{i}", [128, 4, 128], BF16) for i in range(2)]
                  ye = [SB(es, f"ye{i}", [128, D], BF16) for i in range(2)]
                  PX = [PS(es, f"PX{i}", [128, 8, 128], BF16) for i in range(2)]
                  PG = PS(es, "PG", [128, 512]); PU = PS(es, "PU", [128, 512])
                  PH = PS(es, "PH", [128, 8, 128], BF16)
                  PYe = [PS(es, f"PYe{i}", [128, 512]) for i in range(2)]

                  def ldw(e):
                      b = e % 2
                      DMA("pool", WG[b].t[:], wg[l, e].rearrange("(kc p) n -> p kc n", p=128), [], [WG[b]])
                      DMA("pool", WU[b].t[:], wu[l, e].rearrange("(kc p) n -> p kc n", p=128), [], [WU[b]])
                      DMA("pool", WD[b].t[:], wd[l, e].rearrange("(kc p) n -> p kc n", p=128), [], [WD[b]])
                  ldw(0)
                  cnt = 0
                  for e in range(NE):
                      b = e % 2
                      if e + 1 < NE:
                          ldw(e + 1)
                      DMA("sp", xe[b].t[:], Xs[e * CAP:(e + 1) * CAP, :].rearrange("(hf p) d -> p hf d", p=128), [P.reg("XS")], [xe[b]])
                      for hf in range(2):
                          for q in range(2):
                              p_ = PX[q]
                              for k in range(8):
                                  kc = q * 8 + k
                                  TR(p_.t[:, k, :], xe[b].t[:, hf, kc * 128:(kc + 1) * 128], identb, [xe[b], CB], [p_])
                              CP("dve" if q == 0 else "act", xeT[b].t[:, hf, q * 8:(q + 1) * 8, :], p_.t[:], [p_], [xeT[b]])
                      for hf in range(2):
                          u = cnt % 2; cnt += 1
                          for kc in range(16):
                              MM(PG.t[:], xeT[b].t[:, hf, kc, :], WG[b].t[:, kc, :], kc == 0, kc == 15, [xeT[b], WG[b]], [PG])
                          for kc in range(16):
                              MM(PU.t[:], xeT[b].t[:, hf, kc, :], WU[b].t[:, kc, :], kc == 0, kc == 15, [xeT[b], WU[b]], [PU])
                          ACT(sgl[u].t[:], PG.t[:], AF.Silu, [PG], [sgl[u]])
                          TT("dve", hh[u].t[:], sgl[u].t[:], PU.t[:], ALU.mult, [sgl[u], PU], [hh[u]])
                          for k in range(4):
                              TR(PH.t[:, k, :], hh[u].t[:, k * 128:(k + 1) * 128], identb, [hh[u], CB], [PH])
                          CP("dve", hT[u].t[:], PH.t[:, 0:4, :], [PH], [hT[u]])
                          for ch in range(4):
                              py = PYe[ch % 2]
                              for k in range(4):
                                  MM(py.t[:], hT[u].t[:, k, :], WD[b].t[:, k, ch * 512:(ch + 1) * 512], k == 0, k == 3, [hT[u], WD[b]], [py])
                              CP("act" if ch % 2 == 0 else "dve", ye[u].t[:, ch * 512:(ch + 1) * 512], py.t[:], [py], [ye[u]])
                          r0 = e * CAP + hf * 128
                          DMA("sp", Ys[r0:r0 + 128, :], ye[u].t[:], [ye[u]], [P.reg("YS")])
                  END_PHASE()

          def combine(l, aT1, dbg_name):
              with ExitStack() as es:
                  G2 = SB(es, "G2", [128, D]); A1 = SB(es, "cA1", [128, D]); B1 = SB(es, "cB1", [128, D])
                  Y0 = [SB(es, f"Y0{i}", [128, D], BF16) for i in range(2)]
                  Y1 = [SB(es, f"Y1{i}", [128, D], BF16) for i in range(2)]
                  hn = [SB(es, f"chn{i}", [128, D]) for i in range(2)]
                  z = SB(es, "cz", [128, D]); h2 = [SB(es, f"ch2{i}", [128, D]) for i in range(2)]
                  ss = SB(es, "css", [128, 1]); rstd = SB(es, "crstd", [128, 1]); junk = SB(es, "cjunk", [128, D], BF16)
                  xn = SB(es, "cxn", [128, D]); ab_ = SB(es, "cab", [128, D], BF16)
                  pt = [PS(es, f"cpt{i}", [128, 8, 128], BF16) for i in range(2)]
                  DMA("sp", G2.t[:], DER[l, :, 5, :], [], [G2])
                  if l == 0:
                      DMA("sp", B1.t[:], DER[1, :, 0, :], [], [B1]); DMA("sp", A1.t[:], DER[1, :, 1, :], [], [A1])
                  else:
                      DMA("act", A1.t[:], fing[0:1, :].partition_broadcast(128), [], [A1])
                  for i in range(NT):
                      b = i % 2
                      for k, Yk in enumerate([Y0[b], Y1[b]]):
                          P.dma("pool", lambda e, Yk=Yk, i=i, k=k: e.indirect_dma_start(
                              out=Yk.t[:], out_offset=None, in_=Ys[:, :],
                              in_offset=bass.IndirectOffsetOnAxis(ap=SLOTG.t[:, i, k:k + 1], axis=0)),
                              rr([SLOTG, P.reg("YS")]), rr([Yk]))
                      DMA("sp", hn[b].t[:], Hs[i * 128:(i + 1) * 128, :], [P.reg("HS", i)], [hn[b]])
                      TS("dve", z.t[:], Y0[b].t[:], GATES.t[:, i, 0:1], None, ALU.mult, None, [Y0[b], GATES], [z])
                      STT("dve", z.t[:], Y1[b].t[:], GATES.t[:, i, 1:2], z.t[:], ALU.mult, ALU.add, [Y1[b], GATES, z], [z])
                      TT("dve", z.t[:], z.t[:], G2.t[:], ALU.mult, [z, G2], [z])
                      TT("pool", h2[b].t[:], z.t[:], hn[b].t[:], ALU.add, [z, hn[b]], [h2[b]])
                      if debug and dbg_name in dbg:
                          DMA("sp", dbg[dbg_name][i * 128:(i + 1) * 128, :], h2[b].t[:], [h2[b]], [P.reg("DBG", i)])
                      if l == 0:
                          DMA("sp", Hs[i * 128:(i + 1) * 128, :], h2[b].t[:], [h2[b]], [P.reg("HS", i)])
                          norm_mod_transpose(es, h2[b], A1, B1, aT1, i * 128, (ss, rstd, junk, xn, ab_, pt))
                      else:
                          rms_rstd(h2[b].t[:], ss, rstd, junk, [h2[b]])
                          ACT(xn.t[:], h2[b].t[:], AF.Copy, [h2[b], rstd], [xn], scale=rstd.t[:])
                          TT("dve", z.t[:], xn.t[:], A1.t[:], ALU.mult, [xn, A1], [z])
                          DMA("sp", out[i * 128:(i + 1) * 128, :], z.t[:], [z], [P.reg("OUT", i)])
                  END_PHASE()

          post_mixer(0, w_out, x, "hmid0")
          experts(0)
          with ExitStack() as esM:
              MEAN = SB(esM, "MEAN", [128, T]); RSTD = SB(esM, "RSTD", [128, T])
              with ExitStack() as esG:
                  aT1 = SB(esG, "aT1", [128, 16, T], BF16)
                  combine(0, aT1, "hend0")
                  with ExitStack() as es:
                      wv = [SB(es, f"wv{i}", [128, 16, 128], BF16) for i in range(2)]
                      wgt = [SB(es, f"wgt{i}", [128, 16, 128], BF16) for i in range(2)]
                      WDW = SB(es, "WDW", [128, 16, 31]); BDW = SB(es, "BDW", [128, 16])
                      sgm = [SB(es, f"sgm{i}", [128, 512]) for i in range(2)]
                      uT = [SB(es, f"uT{i}", [128, T]) for i in range(2)]
                      acc = [SB(es, f"acc{i}", [128, T]) for i in range(2)]
                      v2 = SB(es, "v2", [128, T]); MQ = SB(es, "MQ", [128, T])
                      PV = [PS(es, f"PV{i}", [128, 512]) for i in range(2)]
                      PGt = [PS(es, f"PGt{i}", [128, 512]) for i in range(2)]
                      PS1 = [PS(es, f"PS1{i}", [128, 512]) for i in range(2)]
                      DMA("sp", WDW.t[:], wdw[:, :, :], [], [WDW]); DMA("sp", BDW.t[:], bdw[:, :], [], [BDW])
                      pwv = w_pw1.rearrange("(kc p) n -> p kc n", p=128)

                      def ldcw(cc_):
                          DMA("pool", wv[cc_ % 2].t[:], pwv[:, :, cc_ * 128:(cc_ + 1) * 128], [], [wv[cc_ % 2]])
                          DMA("pool", wgt[cc_ % 2].t[:], pwv[:, :, D + cc_ * 128:D + (cc_ + 1) * 128], [], [wgt[cc_ % 2]])
                      ldcw(0)
                      MEMSET("pool", MEAN.t[:], 0.0, [MEAN]); MEMSET("pool", MQ.t[:], 0.0, [MQ])
                      for cc_ in range(16):
                          b = cc_ % 2
                          if cc_ + 1 < 16:
                              ldcw(cc_ + 1)
                          for tq in range(4):
                              pv = PV[tq % 2]; pg = PGt[tq % 2]
                              for kc in range(16):
                                  MM(pv.t[:], wv[b].t[:, kc, :], aT1.t[:, kc, tq * 512:(tq + 1) * 512], kc == 0, kc == 15, [wv[b], aT1], [pv])
                              for kc in range(16):
                                  MM(pg.t[:], wgt[b].t[:, kc, :], aT1.t[:, kc, tq * 512:(tq + 1) * 512], kc == 0, kc == 15, [wgt[b], aT1], [pg])
                              ACT(sgm[tq % 2].t[:], pg.t[:], AF.Sigmoid, [pg], [sgm[tq % 2]])
                              TT("dve", uT[b].t[:, tq * 512:(tq + 1) * 512], pv.t[:], sgm[tq % 2].t[:], ALU.mult, [pv, sgm[tq % 2]], [uT[b]])
                          eng = "dve"
                          u_ = uT[b]; a_ = acc[b]
                          TS(eng, a_.t[:], u_.t[:], WDW.t[:, cc_, 15:16], BDW.t[:, cc_:cc_ + 1], ALU.mult, ALU.add, [u_, WDW, BDW], [a_])
                          for j in range(31):
                              s = j - 15
                              if s == 0:
                                  continue
                              wj = WDW.t[:, cc_, j:j + 1]
                              if cc_ < 8:
                                  c0 = max(0, -s); c1 = min(64, 64 - s)
                                  a3 = a_.t[:].rearrange("p (r c) -> p r c", c=64); u3 = u_.t[:].rearrange("p (r c) -> p r c", c=64)
                                  STT(eng, a3[:, :, c0:c1], u3[:, :, c0 + s:c1 + s], wj, a3[:, :, c0:c1], ALU.mult, ALU.add, [u_, WDW, a_], [a_])
                              else:
                                  r0 = max(0, -s); r1 = min(32, 32 - s)
                                  STT(eng, a_.t[:, r0 * 64:r1 * 64], u_.t[:, (r0 + s) * 64:(r1 + s) * 64], wj, a_.t[:, r0 * 64:r1 * 64], ALU.mult, ALU.add, [u_, WDW, a_], [a_])
                          DMA("sp", Vd[cc_ * 128:(cc_ + 1) * 128, :], a_.t[:], [a_], [P.reg("VD")])
                          ACT(v2.t[:], a_.t[:], AF.Square, [a_], [v2])
                          for tq in range(4):
                              MM(PS1[0].t[:], onesf, a_.t[:, tq * 512:(tq + 1) * 512], True, True, [CF, a_], [PS1[0]])
                              TT("dve", MEAN.t[:, tq * 512:(tq + 1) * 512], MEAN.t[:, tq * 512:(tq + 1) * 512], PS1[0].t[:], ALU.add, [MEAN, PS1[0]], [MEAN])
                              MM(PS1[1].t[:], onesf, v2.t[:, tq * 512:(tq + 1) * 512], True, True, [CF, v2], [PS1[1]])
                              TT("dve", MQ.t[:, tq * 512:(tq + 1) * 512], MQ.t[:, tq * 512:(tq + 1) * 512], PS1[1].t[:], ALU.add, [MQ, PS1[1]], [MQ])
                      TS("dve", MEAN.t[:], MEAN.t[:], 1.0 / D, None, ALU.mult, None, [MEAN], [MEAN])
                      TT("dve", v2.t[:], MEAN.t[:], MEAN.t[:], ALU.mult, [MEAN], [v2])
                      STT("dve", MQ.t[:], MQ.t[:], 1.0 / D, v2.t[:], ALU.mult, ALU.subtract, [MQ, v2], [MQ])
                      ACT(RSTD.t[:], MQ.t[:], AF.Sqrt, [MQ, EPSb], [RSTD], bias=EPSb.t[:])
                      RECIP(RSTD.t[:], RSTD.t[:], [RSTD], [RSTD])
                      END_PHASE()
              with ExitStack() as es:
                  LNG = SB(es, "LNG", [128, 16]); LNB = SB(es, "LNB", [128, 16])
                  vt = [SB(es, f"vt{i}", [128, T]) for i in range(2)]
                  sTc = [SB(es, f"sTc{i}", [128, T], BF16) for i in range(2)]
                  DMA("sp", LNG.t[:], lng[:, :], [], [LNG]); DMA("sp", LNB.t[:], lnb[:, :], [], [LNB])
                  for cc_ in range(16):
                      b = cc_ % 2
                      DMA("sp", vt[b].t[:], Vd[cc_ * 128:(cc_ + 1) * 128, :], [P.reg("VD")], [vt[b]])
                      TT("dve", vt[b].t[:], vt[b].t[:], MEAN.t[:], ALU.subtract, [vt[b], MEAN], [vt[b]])
                      TT("pool", vt[b].t[:], vt[b].t[:], RSTD.t[:], ALU.mult, [vt[b], RSTD], [vt[b]])
                      ACT(sTc[b].t[:], vt[b].t[:], AF.Silu, [vt[b], LNG, LNB], [sTc[b]], scale=LNG.t[:, cc_:cc_ + 1], bias=LNB.t[:, cc_:cc_ + 1])
                      DMA("sp", OGT[cc_ * 128:(cc_ + 1) * 128, :], sTc[b].t[:], [sTc[b]], [P.reg("OGT")])
                  END_PHASE()
          post_mixer(1, w_pw2, Hs, "hmid1")
          experts(1)
          combine(1, None, "hend1")
    except _Stop:
        pass
    return nc


def _consts():
    s = np.arange(128)[:, None]; t = np.arange(128)[None, :]
    cf = np.zeros((128, 9, 128), np.float32)
    cf[:, 0] = np.eye(128)
    cf[:, 1] = (s <= t).astype(np.float32) - (s <= 63).astype(np.float32)
    cf[:, 2] = (s >= t).astype(np.float32) - (s >= 64).astype(np.float32)
    cf[:, 3] = (s > t)
    cf[:, 4] = (s < t)
    cf[:, 5] = (s <= t)
    cf[:, 6] = (s >= t)
    cf[:, 7] = 1.0
    cf[:, 8, 0] = (np.arange(128) <= 63); cf[:, 8, 1] = (np.arange(128) >= 64)
    cb = np.zeros((128, 3, 128), np.float32)
    cb[:, 0] = np.eye(128); cb[:, 1] = 1.0; cb[:, 2] = (s < t)
    iot = np.tile(np.arange(65, dtype=np.float32)[None, :], (128, 1))
    iot[:, 64] = XS_ROWS + np.arange(128)
    return cf, cb.astype(ml_dtypes.bfloat16), iot


def _col(v):
    return np.ascontiguousarray(np.asarray(v, np.float32).reshape(16, 128).T)


def kernel(x, c, ctx, c_ctx, ada_w, ada_b, norm1_g, norm2_g, hgrn_w_in, hgrn_lb, hgrn_onorm_g,
           hgrn_w_out, conv_w_pw1, conv_w_dw, conv_b_dw, conv_ln_g, conv_ln_b, conv_w_pw2,
           moe_w_r1, moe_b_r1, moe_w_r2, moe_b_r2, moe_w_gate, moe_w_up, moe_w_down, final_g, _debug=False, _stop=None, _ncores=8, _trace=False):
    f = lambda a: np.ascontiguousarray(np.asarray(a, np.float32))
    key = ("nc", bool(_debug), _stop)
    if key not in _cache:
        _cache[key] = build_program(debug=_debug, stop=_stop)
    nc = _cache[key]
    cf, cb, iot = _consts()
    w_r2 = np.asarray(moe_w_r2, np.float32)
    wr_ = np.concatenate([np.asarray(moe_w_r1, np.float32), w_r2.transpose(0, 2, 1, 3).reshape(2, D, 64)], axis=2)
    br_ = np.concatenate([np.asarray(moe_b_r1, np.float32), np.asarray(moe_b_r2, np.float32).reshape(2, 64)], axis=1)
    wdw_ = np.ascontiguousarray(np.asarray(conv_w_dw, np.float32)[0].T.reshape(16, 128, 31).transpose(1, 0, 2))
    shared = {
        "ada_w": f(ada_w), "ada_b": f(ada_b), "n1g": f(norm1_g), "n2g": f(norm2_g), "fing": f(final_g).reshape(1, D),
        "w_in": np.ascontiguousarray(f(hgrn_w_in)[0].reshape(D, 5, 16, 128).transpose(2, 0, 1, 3).reshape(16, D, 640)), "hlb": f(hgrn_lb)[:, :, :], "ong": f(hgrn_onorm_g).reshape(1, 128), "w_out": f(hgrn_w_out)[0],
        "w_pw1": f(conv_w_pw1)[0], "wdw": wdw_, "bdw": _col(np.asarray(conv_b_dw)[0]), "lng": _col(np.asarray(conv_ln_g)[0]),
        "lnb": _col(np.asarray(conv_ln_b)[0]), "w_pw2": f(conv_w_pw2)[0],
        "wr": np.ascontiguousarray(wr_), "brr": np.ascontiguousarray(br_),
        "cst_f": cf, "cst_b": cb, "iot": iot,
    }
    if _stop is None or _stop >= 5:
        shared.update({"wg": f(moe_w_gate), "wu": f(moe_w_up), "wd": f(moe_w_down)})
    xx = f(x); cx = f(ctx); cc = np.asarray(c, np.float32); ccx = np.asarray(c_ctx, np.float32)
    in_maps = []
    for b in range(_ncores):
        m = dict(shared)
        m["x"] = xx[b]; m["ctx"] = cx[b]
        m["ccol"] = np.ascontiguousarray(np.concatenate([_col(cc[b]), _col(ccx)], axis=1))
        in_maps.append(m)
    res = run_bass_kernel_spmd(nc, in_maps, core_ids=list(range(_ncores)), **({'trace': True} if _trace else {}))
    if _trace:
        print('EXEC_NS', _stop, res.exec_time_ns)
    outp = np.stack([np.asarray(r["out"]) for r in res.results], axis=0).astype(np.float32)
    if _debug:
        return outp, res.results
    return outp
```

```python
import numpy as np
import concourse.bass as bass
import concourse.mybir as mybir

F32 = mybir.dt.float32
BF16 = mybir.dt.bfloat16
I32 = mybir.dt.int32
AF = mybir.ActivationFunctionType
ALU = mybir.AluOpType
AX = mybir.AxisListType

SAME_ENG_SYNC = False


class Reg:
    __slots__ = ("name", "w", "r", "excl")

    def __init__(self, name):
        self.name = name
        self.excl = False
        self.w = None
        self.r = {}


def _tok_key(t):
    return t[0:2]


class Prog:
    ENGS = ["pe", "act", "dve", "pool", "sp"]

    def __init__(self, nc, esems, dsems):
        self.nc = nc
        self.esems = esems
        self.dsems = dsems
        self.dcount = {q: [0] * len(v) for q, v in dsems.items()}
        self.drr = {q: 0 for q in dsems}
        self.ecount = {e: 0 for e in self.ENGS}
        self.regs = {}
        self.reset_phase()

    def reg(self, *key):
        r = self.regs.get(key)
        if r is None:
            r = self.regs[key] = Reg(key)
        return r

    def reset_phase(self):
        self.ops = {e: [] for e in self.ENGS}
        for r in self.regs.values():
            r.w = None
            r.r = {}

    def _deps(self, reads, writes):
        deps = {}

        def add(t):
            k = _tok_key(t)
            if k not in deps or deps[k][2] < t[2]:
                deps[k] = t
        for r in reads:
            if r.w is not None:
                add(r.w)
        for w in writes:
            if w.w is not None:
                add(w.w)
            for t in w.r.values():
                add(t)
        return deps

    def _commit(self, tok, reads, writes):
        k = _tok_key(tok)
        for r in reads:
            r.r[k] = tok
        for w in writes:
            w.w = tok
            w.r = {}

    def op(self, eng, fn, reads=(), writes=()):
        writes = list(writes) + [r for r in reads if r.excl]
        reads = [r for r in reads if not r.excl]
        deps = self._deps(reads, writes)
        idx = len(self.ops[eng])
        tok = ("E", eng, idx)
        t = deps.get(("E", eng))
        if t is not None and (eng == "pe" or idx - t[2] > 3):
            deps.pop(("E", eng), None)
        self.ops[eng].append(dict(fn=fn, deps=list(deps.values()), dma=None, inc=False))
        self._commit(tok, reads, writes)

    def dma(self, q, fn, reads=(), writes=()):
        deps = self._deps(reads, writes)
        i = self.drr[q]
        self.drr[q] = (i + 1) % len(self.dsems[q])
        prev = self.dcount[q][i]
        if prev > 0:
            t = ("D", (q, i), prev)
            k = _tok_key(t)
            if k not in deps or deps[k][2] < prev:
                deps[k] = t
        self.dcount[q][i] = prev + 16
        tok = ("D", (q, i), prev + 16)
        self.ops[q].append(dict(fn=fn, deps=list(deps.values()), dma=(q, i), inc=False))
        self._commit(tok, reads, writes)

    def emit_phase(self, name=None):
        nc = self.nc
        finals = {e: [] for e in self.ENGS}
        for q in self.dsems:
            for i, c in enumerate(self.dcount[q]):
                if c > 0:
                    finals[q].append((self.dsems[q][i], c))
        for e in self.ENGS:
            for o in self.ops[e]:
                for t in o["deps"]:
                    if t[0] == "E":
                        self.ops[t[1]][t[2]]["inc"] = True
        cum = {}
        for e in self.ENGS:
            c = self.ecount[e]
            arr = []
            for o in self.ops[e]:
                if o["inc"] and o["dma"] is None:
                    c += 1
                arr.append(c)
            cum[e] = arr
        ops = self.ops
        esems, dsems = self.esems, self.dsems

        def run(eng_name, eng):
            waited = {}
            for o in ops[eng_name]:
                for t in o["deps"]:
                    if t[0] == "E":
                        sem = esems[t[1]]
                        val = cum[t[1]][t[2]]
                        key = ("E", t[1])
                    else:
                        sem = dsems[t[1][0]][t[1][1]]
                        val = t[2]
                        key = t[1]
                    if waited.get(key, -1) >= val:
                        continue
                    waited[key] = val
                    eng.wait_ge(sem, val)
                ins = o["fn"](eng)
                if o["dma"] is not None:
                    q, i = o["dma"]
                    ins.then_inc(dsems[q][i], 16)
                elif o["inc"]:
                    ins.then_inc(esems[eng_name], 1)
            for sem, c in finals[eng_name]:
                eng.wait_ge(sem, c)

        with nc.Block() as block:
            @block.tensor
            def _(e):
                run("pe", e)

            @block.scalar
            def _(e):
                run("act", e)

            @block.vector
            def _(e):
                run("dve", e)

            @block.gpsimd
            def _(e):
                run("pool", e)

            @block.sync
            def _(e):
                run("sp", e)
        for e in self.ENGS:
            if cum[e]:
                self.ecount[e] = cum[e][-1]
        n = {e: len(self.ops[e]) for e in self.ENGS}
        self.reset_phase()
        return n


from contextlib import ExitStack
import ml_dtypes
from concourse.bass_utils import run_bass_kernel_spmd

T = 2048; D = 2048; CT = 256; NT = 16; NTC = 2; CAP = 256; NE = 64
EPS = 1e-6
XS_ROWS = NE * CAP

_cache = {}
CSTOP = 99
LATSTOP = 99
COPYSEL = 0


class Tn:
    def __init__(self, P, t, name):
        self.t = t
        self.r = P.reg(name)


class _Stop(Exception):
    pass


def build_program(debug=False, stop=None, nheads=16):
    nc = bass.Bass("TRN2", target_bir_lowering=False)
    dr = lambda n, s, d=F32: nc.dram_tensor(n, s, d, kind="ExternalInput").ap()
    x = dr("x", [T, D]); ctx = dr("ctx", [CT, D]); ccol = dr("ccol", [128, 32])
    ada_w = dr("ada_w", [2, D, 6 * D]); ada_b = dr("ada_b", [2, 6 * D])
    n1g = dr("n1g", [2, D]); n2g = dr("n2g", [2, D]); fing = dr("fing", [1, D])
    w_in = dr("w_in", [16, D, 640]); hlb = dr("hlb", [2, 3, D]); ong = dr("ong", [1, 128]); w_out = dr("w_out", [D, D])
    w_pw1 = dr("w_pw1", [D, 2 * D]); wdw = dr("wdw", [128, 16, 31]); bdw = dr("bdw", [128, 16])
    lng = dr("lng", [128, 16]); lnb = dr("lnb", [128, 16]); w_pw2 = dr("w_pw2", [D, D])
    wr = dr("wr", [2, D, 72]); brr = dr("brr", [2, 72])
    if stop is None or stop >= 5:
        wg = dr("wg", [2, NE, D, 512]); wu = dr("wu", [2, NE, D, 512]); wd = dr("wd", [2, NE, 512, D])
    cst_f = dr("cst_f", [128, 9, 128])
    cst_b = dr("cst_b", [128, 3, 128], BF16)
    iot = dr("iot", [128, 65])
    out = nc.dram_tensor("out", [T, D], F32, kind="ExternalOutput").ap()
    Hs = nc.dram_tensor("Hs", [T, D], F32, kind=("ExternalOutput" if debug else "Internal")).ap()
    DER = nc.dram_tensor("DER", [2, 128, 6, D], F32, kind=("ExternalOutput" if debug else "Internal")).ap()
    DERC = nc.dram_tensor("DERC", [128, 2, D], F32, kind=("ExternalOutput" if debug else "Internal")).ap()
    OGT = nc.dram_tensor("OGT", [D, T], BF16, kind=("ExternalOutput" if debug else "Internal")).ap()
    Xs = nc.dram_tensor("Xs", [XS_ROWS + 128, D], BF16, kind=("ExternalOutput" if debug else "Internal")).ap()
    Ys = nc.dram_tensor("Ys", [XS_ROWS, D], BF16, kind=("ExternalOutput" if debug else "Internal")).ap()
    BBd = nc.dram_tensor("BBd", [T, D], BF16).ap()
    Vd = nc.dram_tensor("Vd", [D, T], F32, kind=("ExternalOutput" if debug else "Internal")).ap()
    dbg = {}
    if debug:
        for n in ["hmid0", "hend0", "hmid1"]:
            dbg[n] = nc.dram_tensor("dbg_" + n, [T, D], F32, kind="ExternalOutput").ap()

    try:
      with ExitStack() as es0:
          esems = {e: es0.enter_context(nc.semaphore("e_" + e)) for e in Prog.ENGS}
          dsems = {q: [es0.enter_context(nc.semaphore(f"d_{q}{i}")) for i in range(n)] for q, n in [("sp", 8), ("pool", 6), ("act", 2)]}
          P = Prog(nc, esems, dsems)
          es0.enter_context(nc.allow_low_precision("bf16 matmul operands, fp32 accumulation"))

          uid = [0]

          def SB(es, name, shape, dt=F32):
              uid[0] += 1
              name = f"{name}_{uid[0]}"
              return Tn(P, es.enter_context(nc.sbuf_tensor(name, shape, dt)), name)

          def PS(es, name, shape, dt=F32):
              uid[0] += 1
              name = f"{name}_{uid[0]}"
              t_ = Tn(P, es.enter_context(nc.psum_tensor(name, shape, dt)), name)
              t_.r.excl = True
              return t_

          phase_no = [0]

          def END_PHASE():
              P.emit_phase()
              phase_no[0] += 1
              if stop is not None and phase_no[0] >= stop:
                  raise _Stop()

          def rr(l):
              return [a.r if isinstance(a, Tn) else a for a in l]

          def ACT(out, in_, func, R, W, **kw):
              P.op("act", lambda e: e.activation(out=out, in_=in_, func=func, **kw), rr(R), rr(W))

          def TT(eng, out, in0, in1, op, R, W):
              P.op(eng, lambda e: e.tensor_tensor(out=out, in0=in0, in1=in1, op=op), rr(R), rr(W))

          def TS(eng, out, in0, s1, s2, op0, op1, R, W, **kw):
              if s2 is None:
                  P.op(eng, lambda e: e.tensor_scalar(out=out, in0=in0, scalar1=s1, scalar2=None, op0=op0, **kw), rr(R), rr(W))
              else:
                  P.op(eng, lambda e: e.tensor_scalar(out=out, in0=in0, scalar1=s1, scalar2=s2, op0=op0, op1=op1, **kw), rr(R), rr(W))

          def STT(eng, out, in0, sc, in1, op0, op1, R, W):
              P.op(eng, lambda e: e.scalar_tensor_tensor(out=out, in0=in0, scalar=sc, in1=in1, op0=op0, op1=op1), rr(R), rr(W))

          def CP(eng, out, in_, R, W):
              if eng == "act":
                  P.op("act", lambda e: e.copy(out=out, in_=in_), rr(R), rr(W))
              else:
                  P.op(eng, lambda e: e.tensor_copy(out=out, in_=in_), rr(R), rr(W))

          def MM(out, lhsT, rhs, start, stop, R, W):
              P.op("pe", lambda e: e.matmul(out, lhsT=lhsT, rhs=rhs, start=start, stop=stop), rr(R), rr(W))

          def TR(out, in_, ident, R, W):
              P.op("pe", lambda e: e.transpose(out=out, in_=in_, identity=ident), rr(R), rr(W))

          def DMA(q, out, in_, R, W):
              P.dma(q, lambda e: e.dma_start(out=out, in_=in_), rr(R), rr(W))

          def RED(eng, out, in_, op, R, W):
              P.op(eng, lambda e: e.tensor_reduce(out=out, in_=in_, axis=AX.X, op=op), rr(R), rr(W))

          def RECIP(out, in_, R, W):
              P.op("dve", lambda e: e.reciprocal(out=out, in_=in_), rr(R), rr(W))

          def MEMSET(eng, ap, v, W):
              P.op(eng, lambda e: e.memset(ap, v), [], rr(W))

          CF = SB(es0, "CF", [128, 9, 128]); CB = SB(es0, "CB", [128, 3, 128], BF16)
          IOT = SB(es0, "IOT", [128, 65]); EPSb = SB(es0, "EPSb", [128, 1])
          SLOTG = SB(es0, "SLOTG", [128, NT, 2], I32); GATES = SB(es0, "GATES", [128, NT, 2])
          identf = CF.t[:, 0, :]; LcT = [CF.t[:, 1, :], CF.t[:, 2, :]]; E2T = [CF.t[:, 3, :], CF.t[:, 4, :]]
          onesf = CF.t[:, 7, :]; IND = CF.t[:, 8, 0:2]
          identb = CB.t[:, 0, :]; onesb = CB.t[:, 1, :]; UTs = CB.t[:, 2, :]

          def load_consts():
              DMA("sp", CF.t[:], cst_f[:, :, :], [], [CF])
              DMA("sp", CB.t[:], cst_b[:, :, :], [], [CB])
              DMA("sp", IOT.t[:], iot[:, :], [], [IOT])
              MEMSET("pool", EPSb.t[:], EPS, [EPSb])

          def rms_rstd(xt_ap, ss, rstd, junk, R, n=D):
              ACT(junk.t[:, 0:n], xt_ap, AF.Square, R, [junk, ss], accum_out=ss.t[:])
              ACT(rstd.t[:], ss.t[:], AF.Sqrt, [ss, EPSb], [rstd], scale=1.0 / n, bias=EPSb.t[:])
              RECIP(rstd.t[:], rstd.t[:], [rstd], [rstd])

          with ExitStack() as es:
              load_consts()
              cc = SB(es, "cc", [128, 32]); sc = SB(es, "sc", [128, 32])
              Srep = SB(es, "Srep", [128, 32, 128])
              wa = [SB(es, f"wa{i}", [128, 16, 512]) for i in range(2)]
              ab = [SB(es, f"ab{i}", [128, 512]) for i in range(2)]
              MODt = SB(es, "MODt", [128, 6 * D]); MODc = SB(es, "MODc", [128, 2 * D])
              gt = [SB(es, f"gt{i}", [128, D]) for i in range(2)]
              pa = [PS(es, f"pa{i}", [128, 512]) for i in range(2)]
              pc = [PS(es, f"pc{i}", [128, 512]) for i in range(2)]
              DMA("sp", cc.t[:], ccol[:, :], [], [cc])
              ACT(sc.t[:], cc.t[:], AF.Silu, [cc], [sc])
              for k in range(32):
                  ACT(Srep.t[:, k, :], onesf, AF.Copy, [CF, sc], [Srep], scale=sc.t[:, k:k + 1])
              for l in range(2):
                  awv = ada_w[l].rearrange("(kc p) n -> p kc n", p=128)
                  for j in range(24):
                      w_ = wa[j % 2]; a_ = ab[j % 2]; p_ = pa[j % 2]; q_ = pc[j % 2]
                      DMA("sp", w_.t[:], awv[:, :, j * 512:(j + 1) * 512], [], [w_])
                      DMA("act", a_.t[:], ada_b[l:l + 1, j * 512:(j + 1) * 512].partition_broadcast(128), [], [a_])
                      for kc in range(16):
                          MM(p_.t[:], Srep.t[:, kc, :], w_.t[:, kc, :], kc == 0, kc == 15, [Srep, w_], [p_])
                      TT("dve", MODt.t[:, j * 512:(j + 1) * 512], p_.t[:], a_.t[:], ALU.add, [p_, a_], [MODt])
                      if l == 0 and j < 8:
                          for kc in range(16):
                              MM(q_.t[:], Srep.t[:, 16 + kc, :], w_.t[:, kc, :], kc == 0, kc == 15, [Srep, w_], [q_])
                          TT("dve", MODc.t[:, j * 512:(j + 1) * 512], q_.t[:], a_.t[:], ALU.add, [q_, a_], [MODc])
                  DMA("act", gt[0].t[:], n1g[l:l + 1, :].partition_broadcast(128), [], [gt[0]])
                  DMA("act", gt[1].t[:], n2g[l:l + 1, :].partition_broadcast(128), [], [gt[1]])
                  STT("dve", MODt.t[:, D:2 * D], MODt.t[:, D:2 * D], 1.0, gt[0].t[:], ALU.add, ALU.mult, [MODt, gt[0]], [MODt])
                  STT("dve", MODt.t[:, 4 * D:5 * D], MODt.t[:, 4 * D:5 * D], 1.0, gt[1].t[:], ALU.add, ALU.mult, [MODt, gt[1]], [MODt])
                  DMA("sp", DER[l].rearrange("p s d -> p (s d)"), MODt.t[:], [MODt], [P.reg("DER")])
                  if l == 0:
                      STT("dve", MODc.t[:, D:2 * D], MODc.t[:, D:2 * D], 1.0, gt[0].t[:], ALU.add, ALU.mult, [MODc, gt[0]], [MODc])
                      DMA("sp", DERC.rearrange("p s d -> p (s d)"), MODc.t[:], [MODc], [P.reg("DERC")])
              END_PHASE()

          def norm_mod_transpose(es, xt, A1, B1, dstT, col0, bufs):
              ss, rstd, junk, xn, ab_, pt = bufs
              rms_rstd(xt.t[:], ss, rstd, junk, [xt])
              ACT(xn.t[:], xt.t[:], AF.Copy, [xt, rstd], [xn], scale=rstd.t[:])
              TT("dve", xn.t[:], xn.t[:], A1.t[:], ALU.mult, [xn, A1], [xn])
              TT("pool", ab_.t[:], xn.t[:], B1.t[:], ALU.add, [xn, B1], [ab_])
              for hf in range(2):
                  p_ = pt[hf]
                  for k in range(8):
                      kc = hf * 8 + k
                      TR(p_.t[:, k, :], ab_.t[:, kc * 128:(kc + 1) * 128], identb, [ab_, CB], [p_])
                  CP("dve" if hf == 0 else "act", dstT.t[:, hf * 8:(hf + 1) * 8, col0:col0 + 128], p_.t[:], [p_], [dstT])

          with ExitStack() as esBC:
              aT = SB(esBC, "aT", [128, 16, CT + T], BF16)
              with ExitStack() as es:
                  A1 = SB(es, "A1", [128, D]); B1 = SB(es, "B1", [128, D]); A1c = SB(es, "A1c", [128, D]); B1c = SB(es, "B1c", [128, D])
                  xt = [SB(es, f"xt{i}", [128, D]) for i in range(2)]
                  ss = SB(es, "ss", [128, 1]); rstd = SB(es, "rstd", [128, 1]); junk = SB(es, "junk", [128, D], BF16)
                  xn = SB(es, "xn", [128, D]); ab_ = SB(es, "abf", [128, D], BF16)
                  pt = [PS(es, f"pt{i}", [128, 8, 128], BF16) for i in range(2)]
                  DMA("sp", B1.t[:], DER[0, :, 0, :], [], [B1]); DMA("sp", A1.t[:], DER[0, :, 1, :], [], [A1])
                  DMA("sp", B1c.t[:], DERC[:, 0, :], [], [B1c]); DMA("sp", A1c.t[:], DERC[:, 1, :], [], [A1c])
                  for i in range(NTC + NT):
                      x_ = xt[i % 2]
                      src = ctx[i * 128:(i + 1) * 128, :] if i < NTC else x[(i - NTC) * 128:(i - NTC + 1) * 128, :]
                      DMA("sp", x_.t[:], src, [], [x_])
                      norm_mod_transpose(es, x_, A1c if i < NTC else A1, B1c if i < NTC else B1, aT, i * 128, (ss, rstd, junk, xn, ab_, pt))
                  END_PHASE()

              with ExitStack() as es:
                  NTT = NTC + NT
                  wsl = [SB(es, f"wsl{i}", [128, 16, 640], BF16) for i in range(2)]
                  lbr = SB(es, "lbr", [128, 2, 3, 128])
                  lb2 = [SB(es, "lb2", [128, 2, 128])] * 2; oml2 = [SB(es, "oml2", [128, 2, 128])] * 2
                  ongb = SB(es, "ongb", [128, 128])
                  QTb = [SB(es, f"QTb{i}", [128, NT, 128], BF16) for i in range(2)]
                  ER = [SB(es, f"ER{i}", [128, NTT, 4]) for i in range(2)]; ERD = [SB(es, f"ERD{i}", [128, NTT, 2]) for i in range(2)]
                  DSb = [SB(es, f"DSb{i}", [128, NTT, 128]) for i in range(2)]
                  OPb = [SB(es, f"OPb{i}", [128, NT, 128]) for i in range(2)]
                  SGS = [SB(es, f"SGS{i}", [128, NT, 128]) for i in range(2)]
                  OGh = [SB(es, "OGh", [128, T], BF16)] * 2
                  qs = [SB(es, f"qs{i}", [128, 128]) for i in range(2)]
                  sgt = [SB(es, "sgt", [128, 128])] * 2
                  vb = [SB(es, f"vb{i}", [128, 128], BF16) for i in range(3)]
                  sig = [SB(es, "sig", [128, 256])] * 2
                  gl = [SB(es, f"gl{i}", [128, 256]) for i in range(2)]
                  kk = [SB(es, f"kk{i}", [128, 256]) for i in range(2)]
                  Eb = [SB(es, "Eb", [128, 256])] * 2
                  Ei = [SB(es, "Ei", [128, 256])] * 2
                  E2 = [SB(es, "E2", [128, 256])] * 2
                  qt = [SB(es, f"qt{i}", [128, 256], BF16) for i in range(2)]
                  kt = [SB(es, f"kt{i}", [128, 256], BF16) for i in range(2)]
                  kh = [SB(es, f"kh{i}", [128, 256], BF16) for i in range(2)]
                  qTt = [SB(es, f"qTt{i}", [128, 2, 128], BF16) for i in range(2)]
                  kTt = [SB(es, f"kTt{i}", [128, 2, 128], BF16) for i in range(2)]
                  sT = [SB(es, f"sT{i}", [128, 2, 128], BF16) for i in range(2)]
                  kTz = [SB(es, f"kTz{i}", [128, 2, 128], BF16) for i in range(2)]
                  SmF = [SB(es, f"SmF{i}", [128, 128], BF16) for i in range(2)]
                  for i_ in range(2):
                      MEMSET("pool", kTz[i_].t[:], 0.0, [kTz[i_]])
                  SstF = SB(es, "SstF", [128, 128]); SstB = SB(es, "SstB", [128, 128]); SmB = SB(es, "SmB", [128, 128], BF16)
                  sqj = SB(es, "sqj", [128, 128]); ssq = SB(es, "ssq", [128, NT]); rs16 = SB(es, "rs16", [128, NT])
                  on = [SB(es, "on", [128, 128])] * 2; onb = [SB(es, "onb", [128, 128], BF16)] * 2
                  PQ = [PS(es, f"PQ{i}", [128, 512]) for i in range(2)]
                  PQ2 = PS(es, "PQ2", [128, 4, 128])
                  PC = PS(es, "PC", [128, 4, 128])
                  PT = PS(es, "PT", [128, 8, 128], BF16)
                  PSs = PS(es, "PSs", [128, 4, 128])
                  PD = PS(es, "PD", [128, 4, 128])
                  PCH = PS(es, "PCH", [128, 4, 128])
                  DMA("act", ongb.t[:], ong[0:1, :].partition_broadcast(128), [], [ongb])
                  def load_head_w(h):
                      w_ = wsl[h % 2]
                      DMA("pool", w_.t[:], w_in[h].rearrange("(kc p) n -> p kc n", p=128), [], [w_])

                  def head_prologue(h):
                      hb = h % 2
                      for d_ in range(2):
                          for s_ in range(3):
                              DMA("act", lbr.t[:, d_, s_, :], hlb[d_, s_:s_ + 1, h * 128:(h + 1) * 128].partition_broadcast(128), [], [lbr])
                      lbrf = lbr.t[:].rearrange("p a s k -> p (a s k)")
                      ACT(lbrf, lbrf, AF.Exp, [lbr], [lbr])
                      lsum = kk[0]; lsv = kk[0].t[:].rearrange("p (a k) -> p a k", k=128)
                      TT("dve", lsv, lbr.t[:, :, 0, :], lbr.t[:, :, 1, :], ALU.add, [lbr], [lsum])
                      TT("dve", lsv, lsv, lbr.t[:, :, 2, :], ALU.add, [lbr, lsum], [lsum])
                      RECIP(lsv, lsv, [lsum], [lsum])
                      TT("dve", lb2[hb].t[:], lbr.t[:, :, 0, :], lsv, ALU.mult, [lbr, lsum], [lb2[hb]])
                      TS("dve", oml2[hb].t[:], lb2[hb].t[:], -1.0, 1.0, ALU.mult, ALU.add, [lb2[hb]], [oml2[hb]])
                      MEMSET("pool", SstF.t[:], 0.0, [SstF])

                  def S1(h, i, part):
                      hb = h % 2; b = i % 2; lat = i >= NTC; li = i - NTC
                      w_ = wsl[hb]; pq = PQ[b]; v_ = vb[i % 3]
                      lbf = lb2[hb].t[:].rearrange("p a k -> p (a k)"); omf = oml2[hb].t[:].rearrange("p a k -> p (a k)")
                      if part in ("pe0", "pe1", "pe2"):
                          lo, hi = {"pe0": (0, 6), "pe1": (6, 11), "pe2": (11, 16)}[part]
                          for kc in range(lo, hi):
                              MM(pq.t[:], aT.t[:, kc, i * 128:(i + 1) * 128], w_.t[:, kc, 0:512], kc == 0, kc == 15, [aT, w_], [pq])
                          if lat and part == "pe2":
                              for kc in range(16):
                                  MM(PQ2.t[:, b, :], aT.t[:, kc, i * 128:(i + 1) * 128], w_.t[:, kc, 512:640], kc == 0, kc == 15, [aT, w_], [PQ2])
                          return
                      ACT(sig[b].t[:], pq.t[:, 256:512], AF.Sigmoid, [pq], [sig[b]])
                      if lat:
                          ACT(qs[b].t[:], pq.t[:, 0:128], AF.Sigmoid, [pq], [qs[b]])
                          ACT(sgt[b].t[:], PQ2.t[:, b, :], AF.Sigmoid, [PQ2], [sgt[b]])
                      CP("dve", v_.t[:], pq.t[:, 128:256], [pq], [v_])
                      if lat:
                          TT("dve", qs[b].t[:], qs[b].t[:], pq.t[:, 0:128], ALU.mult, [qs[b], pq], [qs[b]])
                          TT("dve", SGS[hb].t[:, li, :], sgt[b].t[:], PQ2.t[:, b, :], ALU.mult, [sgt[b], PQ2], [SGS[hb]])
                      TT("dve", sig[b].t[:], sig[b].t[:], omf, ALU.mult, [sig[b], oml2[hb]], [sig[b]])
                      TT("pool", sig[b].t[:], sig[b].t[:], lbf, ALU.add, [sig[b], lb2[hb]], [sig[b]])
                      ACT(gl[b].t[:], sig[b].t[:], AF.Ln, [sig[b]], [gl[b]])
                      TS("pool", kk[b].t[:], sig[b].t[:], -1.0, 1.0, ALU.mult, ALU.add, [sig[b]], [kk[b]])

                  def S2(h, i, part):
                      hb = h % 2; b = i % 2; lat = i >= NTC; v_ = vb[i % 3]
                      if part == "a":
                          for d_ in range(2):
                              if lat:
                                  MM(PC.t[:, d_, :], LcT[d_], gl[b].t[:, d_ * 128:(d_ + 1) * 128], True, True, [CF, gl[b]], [PC])
                              MM(PC.t[:, 2 + d_, :], E2T[d_], gl[b].t[:, d_ * 128:(d_ + 1) * 128], True, True, [CF, gl[b]], [PC])
                              MM(PSs.t[:, 3, 2 * d_:2 * d_ + 2], gl[b].t[:, d_ * 128:(d_ + 1) * 128], IND, True, True, [CF, gl[b]], [PSs])
                      elif part == "b1":
                          ACT(E2[b].t[:], PC.t[:, 2:4, :].rearrange("p a k -> p (a k)"), AF.Exp, [PC], [E2[b]])
                          ACT(ER[hb].t[:, i, :], PSs.t[:, 3, 0:4], AF.Exp, [PSs], [ER[hb]])
                          TT("pool", kh[b].t[:], kk[b].t[:], E2[b].t[:], ALU.mult, [kk[b], E2[b]], [kh[b]])
                          if lat:
                              ACT(Eb[b].t[:], PC.t[:, 0:2, :].rearrange("p a k -> p (a k)"), AF.Exp, [PC], [Eb[b]])
                              ACT(Ei[b].t[:], PC.t[:, 0:2, :].rearrange("p a k -> p (a k)"), AF.Exp, [PC], [Ei[b]], scale=-1.0)
                          TT("pool", ERD[hb].t[:, i, 0:1], ER[hb].t[:, i, 0:1], ER[hb].t[:, i, 1:2], ALU.mult, [ER[hb]], [ERD[hb]])
                          TT("pool", ERD[hb].t[:, i, 1:2], ER[hb].t[:, i, 3:4], ER[hb].t[:, i, 2:3], ALU.mult, [ER[hb]], [ERD[hb]])
                          if lat:
                              ACT(SmF[b].t[:], SstF.t[:], AF.Copy, [SstF, ER[hb]], [SmF[b]], scale=ER[hb].t[:, i, 0:1])
                              for d_ in range(2):
                                  TT("pool", qt[b].t[:, d_ * 128:(d_ + 1) * 128], qs[b].t[:], Eb[b].t[:, d_ * 128:(d_ + 1) * 128], ALU.mult, [qs[b], Eb[b]], [qt[b]])
                              TT("dve", kt[b].t[:], kk[b].t[:], Ei[b].t[:], ALU.mult, [kk[b], Ei[b]], [kt[b]])
                      elif part == "b2":
                          for d_ in range(2):
                              MM(PD.t[:, d_, :], kh[b].t[:, d_ * 128:(d_ + 1) * 128], v_.t[:], True, True, [kh[b], v_], [PD])
                      elif part == "b3":
                          STT("dve", SstF.t[:], SstF.t[:], ERD[hb].t[:, i, 0:1], PD.t[:, 0, :], ALU.mult, ALU.add, [SstF, ERD[hb], PD], [SstF])
                          CP("act", DSb[hb].t[:, i, :], PD.t[:, 1, :], [PD], [DSb[hb]])

                  def S3(h, i, part):
                      hb = h % 2; b = i % 2; lat = i >= NTC; li = i - NTC; v_ = vb[i % 3]
                      if not lat:
                          return
                      if part == "a":
                          for d_ in range(2):
                              TR(PT.t[:, d_, :], qt[b].t[:, d_ * 128:(d_ + 1) * 128], identb, [qt[b], CB], [PT])
                              TR(PT.t[:, 2 + d_, :], kt[b].t[:, d_ * 128:(d_ + 1) * 128], identb, [kt[b], CB], [PT])
                      elif part == "b1":
                          CP("dve", qTt[b].t[:], PT.t[:, 0:2, :], [PT], [qTt[b]])
                          CP("act", kTt[b].t[:], PT.t[:, 2:4, :], [PT], [kTt[b]])
                          CP("dve", kTz[b].t[:, 0, 0:64], PT.t[:, 2, 0:64], [PT], [kTz[b]])
                          CP("dve", kTz[b].t[:, 1, 64:128], PT.t[:, 3, 64:128], [PT], [kTz[b]])
                          CP("pool", QTb[hb].t[:, li, :], qTt[b].t[:, 1, :], [qTt[b]], [QTb[hb]])
                      elif part == "b2":
                          MM(PSs.t[:, 0, 64:128], kTt[b].t[:, 0, :], qTt[b].t[:, 0, 64:128], True, True, [kTt[b], qTt[b]], [PSs])
                          MM(PSs.t[:, 0, 0:64], kTz[b].t[:, 0, :], qTt[b].t[:, 0, 0:64], True, True, [kTz[b], qTt[b]], [PSs])
                          MM(PSs.t[:, 1, 0:64], kTt[b].t[:, 1, :], qTt[b].t[:, 1, 0:64], True, True, [kTt[b], qTt[b]], [PSs])
                          MM(PSs.t[:, 1, 64:128], kTz[b].t[:, 1, :], qTt[b].t[:, 1, 64:128], True, True, [kTz[b], qTt[b]], [PSs])
                      elif part == "b3":
                          TT("dve", sT[b].t[:], PSs.t[:, 0:2, :], CF.t[:, 5:7, :], ALU.mult, [PSs, CF], [sT[b]])
                      elif part == "b4":
                          MM(PSs.t[:, 2, :], sT[b].t[:, 0, :], v_.t[:], True, False, [sT[b], v_], [PSs])
                          MM(PSs.t[:, 2, :], sT[b].t[:, 1, :], v_.t[:], False, False, [sT[b], v_], [PSs])
                          MM(PSs.t[:, 2, :], qTt[b].t[:, 0, :], SmF[b].t[:], False, True, [qTt[b], SmF[b]], [PSs])
                      elif part == "b5":
                          CP("act", OPb[hb].t[:, li, :], PSs.t[:, 2, :], [PSs], [OPb[hb]])

                  bw_order = [1, 0] + list(range(NTT - 1, NTC - 1, -1))

                  def deferred(h, u, part):
                      hb = h % 2
                      i = bw_order[u]
                      li = i - NTC; k = li % 2
                      lat = i >= NTC
                      if part == "a":
                          if u == 0:
                              MEMSET("pool", SstB.t[:], 0.0, [SstB])
                          if lat:
                              ACT(SmB.t[:], SstB.t[:], AF.Copy, [SstB, ER[hb]], [SmB], scale=ER[hb].t[:, i, 3:4])
                              MM(PCH.t[:, 0, :], QTb[hb].t[:, li, :], SmB.t[:], True, True, [QTb[hb], SmB], [PCH])
                      elif part == "b1":
                          if lat:
                              TT("dve", OPb[hb].t[:, li, :], OPb[hb].t[:, li, :], PCH.t[:, 0, :], ALU.add, [OPb[hb], PCH], [OPb[hb]])
                          STT("dve", SstB.t[:], SstB.t[:], ERD[hb].t[:, i, 1:2], DSb[hb].t[:, i, :], ALU.mult, ALU.add, [SstB, ERD[hb], DSb[hb]], [SstB])
                          if lat:
                              TT("pool", sqj.t[:], OPb[hb].t[:, li, :], OPb[hb].t[:, li, :], ALU.mult, [OPb[hb]], [sqj])
                              RED("dve", ssq.t[:, li:li + 1], sqj.t[:], ALU.add, [sqj], [ssq])
                              ACT(rs16.t[:, li:li + 1], ssq.t[:, li:li + 1], AF.Ln, [ssq, EPSb], [rs16], scale=1.0 / 128, bias=EPSb.t[:])
                              ACT(rs16.t[:, li:li + 1], rs16.t[:, li:li + 1], AF.Exp, [rs16], [rs16], scale=-0.5)
                              STT("dve", on[k].t[:], OPb[hb].t[:, li, :], rs16.t[:, li:li + 1], SGS[hb].t[:, li, :], ALU.mult, ALU.mult, [OPb[hb], rs16, SGS[hb]], [on[k]])
                              TT("pool", onb[k].t[:], on[k].t[:], ongb.t[:], ALU.mult, [on[k], ongb], [onb[k]])
                      elif part == "b2":
                          if lat:
                              TR(PT.t[:, 4 + li % 4, :], onb[k].t[:], identb, [onb[k], CB], [PT])
                      elif part == "b3":
                          if lat:
                              CP("act", OGh[hb].t[:, li * 128:(li + 1) * 128], PT.t[:, 4 + li % 4, :], [PT], [OGh[hb]])
                          if u == NTT - 1:
                              DMA("sp", OGT[h * 128:(h + 1) * 128, :], OGh[hb].t[:], [OGh[hb]], [P.reg("OGT")])

                  load_head_w(0)
                  for h in range(nheads + 1):
                      if h < nheads:
                          if h + 1 < nheads:
                              load_head_w(h + 1)
                          head_prologue(h)
                      for step in range(NTT + 3):
                          cur = h < nheads
                          d_on = h >= 1 and step < NTT
                          ok_ = lambda t: cur and 0 <= t < NTT
                          seq = [("S2", step - 2, "a"), ("S3", step - 3, "a"), ("D", None, "a"), ("S1", step, "pe0"),
                                 ("S1", step - 1, "rest"), ("S2", step - 2, "b1"), ("S3", step - 3, "b1"), ("D", None, "b1"),
                                 ("S1", step, "pe1"), ("S2", step - 2, "b2"), ("S3", step - 3, "b2"),
                                 ("S2", step - 2, "b3"), ("S3", step - 3, "b3"), ("S1", step, "pe2"),
                                 ("S3", step - 3, "b4"), ("D", None, "b2"), ("S3", step - 3, "b5"), ("D", None, "b3")]
                          for kind, t_, part in seq:
                              if kind == "D":
                                  if d_on:
                                      deferred(h - 1, step, part)
                              elif ok_(t_):
                                  {"S1": S1, "S2": S2, "S3": S3}[kind](h, t_, part)
                  END_PHASE()

          def post_mixer(l, wmat, hin, dbg_name):
              with ExitStack() as es:
                  atl = [SB(es, f"atl{i}", [128, 16, 128], BF16) for i in range(2)]
                  OGTv = OGT.rearrange("(kc p) t -> p kc t", p=128)
                  wo = SB(es, "wo", [128, 16, D], BF16)
                  G1 = SB(es, "G1", [128, D]); A2 = SB(es, "A2", [128, D]); B2 = SB(es, "B2", [128, D])
                  WR = SB(es, "WR", [128, 16, 72]); BR = SB(es, "BR", [128, 72])
                  xt = [SB(es, f"pxt{i}", [128, D]) for i in range(2)]
                  hn = [SB(es, "phn", [128, D])] * 2
                  bfl = SB(es, "pbfl", [128, D]); bb = [SB(es, f"pbb{i}", [128, D], BF16) for i in range(2)]
                  LG = SB(es, "pLG", [128, NT, 72]); sm2 = SB(es, "psm2", [128, 26, NT]); t8 = SB(es, "pt8", [128, NT, 8])
                  PRs = SB(es, "pPRs", [128, NT, 64]); SIDX = SB(es, "pSIDX", [128, NT, 2], I32)
                  ss = SB(es, "pss", [128, 1]); rstd = SB(es, "prstd", [128, 1]); junkb = SB(es, "pjunkb", [128, D], BF16)
                  bT = SB(es, "pbT", [128, 16, 128])
                  As = SB(es, "pAs", [128, NT, 64], BF16)
                  oh1 = SB(es, "poh1", [128, NT, 8])
                  sel = SB(es, "psel", [128, NT, 8]); sel2 = SB(es, "psel2", [128, NT, 8]); oha = SB(es, "poha", [128, NT, 8]); ohb = SB(es, "pohb", [128, NT, 8])
                  Aa = SB(es, "pAa", [128, NT, 64]); Ab = SB(es, "pAb", [128, NT, 64]); t64 = SB(es, "pt64", [128, NT, 64])
                  PY = [PS(es, f"PY{i}", [128, 512]) for i in range(4)]
                  PTf = [PS(es, f"PTf{i}", [128, 4, 128]) for i in range(2)]
                  PL = PS(es, "PL", [128, 512]); PR = PS(es, "PRk", [128, 512])
                  DMA("pool", wo.t[:], wmat.rearrange("(kc p) n -> p kc n", p=128), [], [wo])
                  DMA("sp", G1.t[:], DER[l, :, 2, :], [], [G1]); DMA("sp", B2.t[:], DER[l, :, 3, :], [], [B2]); DMA("sp", A2.t[:], DER[l, :, 4, :], [], [A2])
                  DMA("sp", WR.t[:], wr[l].rearrange("(kc p) n -> p kc n", p=128), [], [WR])
                  DMA("act", BR.t[:], brr[l:l + 1, :].partition_broadcast(128), [], [BR])
                  c = lambda k: sm.t[:, k:k + 1]
                  for i in range(NT):
                      b = i % 2
                      x_ = xt[b]; h_ = hn[b]
                      DMA("sp", x_.t[:], hin[i * 128:(i + 1) * 128, :], [P.reg("HIN", i)], [x_])
                      at_ = atl[b]
                      DMA("sp", at_.t[:], OGTv[:, :, i * 128:(i + 1) * 128], [P.reg("OGT")], [at_])
                      for ch in range(4):
                          for kc in range(16):
                              MM(PY[ch].t[:], at_.t[:, kc, :], wo.t[:, kc, ch * 512:(ch + 1) * 512], kc == 0, kc == 15, [at_, wo], [PY[ch]])
                          TT("dve", h_.t[:, ch * 512:(ch + 1) * 512], PY[ch].t[:], G1.t[:, ch * 512:(ch + 1) * 512], ALU.mult, [PY[ch], G1], [h_])
                      TT("pool", h_.t[:], h_.t[:], x_.t[:], ALU.add, [h_, x_], [h_])
                      DMA("sp", Hs[i * 128:(i + 1) * 128, :], h_.t[:], [h_], [P.reg("HS", i)])
                      if debug:
                          DMA("sp", dbg[dbg_name][i * 128:(i + 1) * 128, :], h_.t[:], [h_], [P.reg("DBG", i)])
                      rms_rstd(h_.t[:], ss, rstd, junkb, [h_])
                      ACT(bfl.t[:], h_.t[:], AF.Copy, [h_, rstd], [bfl], scale=rstd.t[:])
                      TT("dve", bfl.t[:], bfl.t[:], A2.t[:], ALU.mult, [bfl, A2], [bfl])
                      TT("pool", bfl.t[:], bfl.t[:], B2.t[:], ALU.add, [bfl, B2], [bfl])
                      CP("act", bb[b].t[:], bfl.t[:], [bfl], [bb[b]])
                      DMA("sp", BBd[i * 128:(i + 1) * 128, :], bb[b].t[:], [bb[b]], [P.reg("BBD", i)])
                      for q4 in range(4):
                          p_ = PTf[q4 % 2]
                          for k in range(4):
                              kc = q4 * 4 + k
                              TR(p_.t[:, k, :], bfl.t[:, kc * 128:(kc + 1) * 128], identf, [bfl, CF], [p_])
                          CP("dve" if q4 % 2 == 0 else "act", bT.t[:, q4 * 4:(q4 + 1) * 4, :], p_.t[:], [p_], [bT])
                      for kc in range(16):
                          MM(PL.t[:, 0:72], bT.t[:, kc, :], WR.t[:, kc, :], kc == 0, kc == 15, [bT, WR], [PL])
                      TT("dve", LG.t[:, i, :], PL.t[:, 0:72], BR.t[:], ALU.add, [PL, BR], [LG])
                  def bc8(ap2):
                      return ap2.unsqueeze(2).broadcast_to([128, NT, 8])
                  L1 = LG.t[:, :, 0:8]
                  c2 = lambda k: sm2.t[:, k, :]
                  io8 = IOT.t[:, 0:8].unsqueeze(1).broadcast_to([128, NT, 8])
                  io64 = IOT.t[:, 0:64].unsqueeze(1).broadcast_to([128, NT, 64])
                  dumpc = IOT.t[:, 64:65].broadcast_to([128, NT])
                  RED("dve", c2(0), L1, ALU.max, [LG], [sm2])
                  TT("dve", oh1.t[:], L1, bc8(c2(0)), ALU.is_equal, [LG, sm2], [oh1])
                  TT("dve", t8.t[:], L1, bc8(c2(0)), ALU.subtract, [LG, sm2], [t8])
                  ACT(t8.t[:], t8.t[:], AF.Exp, [t8], [t8])
                  RED("dve", c2(2), t8.t[:], ALU.add, [t8], [sm2])
                  RECIP(c2(3), c2(2), [sm2], [sm2])
                  for g in range(8):
                      TT("dve", t8.t[:], LG.t[:, :, 8 + g * 8:16 + g * 8], bc8(oh1.t[:, :, g]), ALU.mult, [LG, oh1], [t8])
                      if g == 0:
                          CP("dve", sel.t[:], t8.t[:], [t8], [sel])
                      else:
                          TT("dve", sel.t[:], sel.t[:], t8.t[:], ALU.add, [sel, t8], [sel])
                  RED("dve", c2(4), sel.t[:], ALU.max, [sel], [sm2])
                  TT("dve", oha.t[:], sel.t[:], bc8(c2(4)), ALU.is_equal, [sel, sm2], [oha])
                  STT("dve", sel2.t[:], oha.t[:], -1e30, sel.t[:], ALU.mult, ALU.add, [oha, sel], [sel2])
                  RED("dve", c2(6), sel2.t[:], ALU.max, [sel2], [sm2])
                  TT("dve", ohb.t[:], sel2.t[:], bc8(c2(6)), ALU.is_equal, [sel2, sm2], [ohb])
                  TT("dve", c2(7), c2(6), c2(4), ALU.subtract, [sm2], [sm2])
                  ACT(c2(7), c2(7), AF.Exp, [sm2], [sm2])
                  TS("dve", c2(8), c2(7), 1.0, None, ALU.add, None, [sm2], [sm2])
                  RECIP(c2(8), c2(8), [sm2], [sm2])
                  TT("dve", c2(9), c2(3), c2(8), ALU.mult, [sm2], [sm2])
                  TT("dve", c2(10), c2(9), c2(7), ALU.mult, [sm2], [sm2])
                  for src_, dst_ in ((oh1, 11), (oha, 12), (ohb, 13)):
                      TT("dve", t8.t[:], src_.t[:], io8, ALU.mult, [src_, IOT], [t8])
                      RED("dve", c2(dst_), t8.t[:], ALU.add, [t8], [sm2])
                  STT("dve", c2(14), c2(11), 8.0, c2(12), ALU.mult, ALU.add, [sm2], [sm2])
                  STT("dve", c2(15), c2(11), 8.0, c2(13), ALU.mult, ALU.add, [sm2], [sm2])
                  TT("dve", Aa.t[:], io64, c2(14).unsqueeze(2).broadcast_to([128, NT, 64]), ALU.is_equal, [IOT, sm2], [Aa])
                  TT("dve", Ab.t[:], io64, c2(15).unsqueeze(2).broadcast_to([128, NT, 64]), ALU.is_equal, [IOT, sm2], [Ab])
                  TT("dve", As.t[:], Aa.t[:], Ab.t[:], ALU.add, [Aa, Ab], [As])
                  for i in range(NT):
                      pr_ = PY[i // 8]
                      for j in range(i + 1):
                          MM(pr_.t[:, (i % 8) * 64:(i % 8 + 1) * 64], UTs if j == i else onesb, As.t[:, j, :], j == 0, j == i, [As, CB], [pr_])
                  for hf_ in range(2):
                      CP("dve" if hf_ == 0 else "act", PRs.t[:, hf_ * 8:(hf_ + 1) * 8, :], PY[hf_].t[:].rearrange("p (a e) -> p a e", e=64), [PY[hf_]], [PRs])
                  TT("dve", t64.t[:], PRs.t[:], Aa.t[:], ALU.mult, [PRs, Aa], [t64]); RED("dve", c2(16), t64.t[:], ALU.add, [t64], [sm2])
                  TT("dve", t64.t[:], PRs.t[:], Ab.t[:], ALU.mult, [PRs, Ab], [t64]); RED("dve", c2(17), t64.t[:], ALU.add, [t64], [sm2])
                  for k in range(2):
                      rk = c2(16 + k); ok = c2(18 + k); sg_ = c2(20 + k); ssc = c2(22 + k); ek = c2(14 + k); gk = c2(9 + k)
                      TS("dve", ok, rk, float(CAP) - 0.5, None, ALU.is_lt, None, [sm2], [sm2])
                      TS("dve", c2(24), rk, float(CAP - 1), None, ALU.min, None, [sm2], [sm2])
                      STT("dve", sg_, ek, float(CAP), c2(24), ALU.mult, ALU.add, [sm2], [sm2])
                      TT("dve", c2(25), sg_, dumpc, ALU.subtract, [sm2, IOT], [sm2])
                      TT("dve", c2(25), c2(25), ok, ALU.mult, [sm2], [sm2])
                      TT("dve", ssc, c2(25), dumpc, ALU.add, [sm2, IOT], [sm2])
                      TT("dve", GATES.t[:, :, k], gk, ok, ALU.mult, [sm2], [GATES])
                      CP("dve", SLOTG.t[:, :, k], sg_, [sm2], [SLOTG])
                      CP("dve", SIDX.t[:, :, k], ssc, [sm2], [SIDX])
                  for i in range(NT):
                      b = i % 2
                      DMA("sp", bb[b].t[:], BBd[i * 128:(i + 1) * 128, :], [P.reg("BBD", i)], [bb[b]])
                      for k in range(2):
                          P.dma("pool", lambda e, i=i, k=k, b=b: e.indirect_dma_start(
                              out=Xs[:, :], out_offset=bass.IndirectOffsetOnAxis(ap=SIDX.t[:, i, k:k + 1], axis=0),
                              in_=bb[b].t[:], in_offset=None),
                              rr([bb[b], SIDX]), [P.reg("XS")])
                  END_PHASE()

          def experts(l):
              with ExitStack() as es:
                  WG = [SB(es, f"WG{i}", [128, 16, 512], BF16) for i in range(2)]
                  WU = [SB(es, f"WU{i}", [128, 16, 512], BF16) for i in range(2)]
                  WD = [SB(es, f"WD{i}", [128, 4, D], BF16) for i in range(2)]
                  xe = [SB(es, f"xe{i}", [128, 2, D], BF16) for i in range(2)]
                  xeT = [SB(es, f"xeT{i}", [128, 2, 16, 128], BF16) for i in range(2)]
                  sgl = [SB(es, f"sgl{i}", [128, 512]) for i in range(2)]
                  hh = [SB(es, f"hh{i}", [128, 512], BF16) for i in range(2)]
                  hT = [SB(es, f"hT{i}", [128, 4, 128], BF16) for i in range(2)]
                  ye = [SB(es, f"ye{i}", [128, D], BF16) for i in range(2)]
                  PX = [PS(es, f"PX{i}", [128, 8, 128], BF16) for i in range(2)]
                  PG = PS(es, "PG", [128, 512]); PU = PS(es, "PU", [128, 512])
                  PH = PS(es, "PH", [128, 8, 128], BF16)
                  PYe = [PS(es, f"PYe{i}", [128, 512]) for i in range(2)]

                  def ldw(e):
                      b = e % 2
                      DMA("pool", WG[b].t[:], wg[l, e].rearrange("(kc p) n -> p kc n", p=128), [], [WG[b]])
                      DMA("pool", WU[b].t[:], wu[l, e].rearrange("(kc p) n -> p kc n", p=128), [], [WU[b]])
                      DMA("pool", WD[b].t[:], wd[l, e].rearrange("(kc p) n -> p kc n", p=128), [], [WD[b]])
                  ldw(0)
                  cnt = 0
                  for e in range(NE):
                      b = e % 2
                      if e + 1 < NE:
                          ldw(e + 1)
                      DMA("sp", xe[b].t[:], Xs[e * CAP:(e + 1) * CAP, :].rearrange("(hf p) d -> p hf d", p=128), [P.reg("XS")], [xe[b]])
                      for hf in range(2):
                          for q in range(2):
                              p_ = PX[q]
                              for k in range(8):
                                  kc = q * 8 + k
                                  TR(p_.t[:, k, :], xe[b].t[:, hf, kc * 128:(kc + 1) * 128], identb, [xe[b], CB], [p_])
                              CP("dve" if q == 0 else "act", xeT[b].t[:, hf, q * 8:(q + 1) * 8, :], p_.t[:], [p_], [xeT[b]])
                      for hf in range(2):
                          u = cnt % 2; cnt += 1
                          for kc in range(16):
                              MM(PG.t[:], xeT[b].t[:, hf, kc, :], WG[b].t[:, kc, :], kc == 0, kc == 15, [xeT[b], WG[b]], [PG])
                          for kc in range(16):
                              MM(PU.t[:], xeT[b].t[:, hf, kc, :], WU[b].t[:, kc, :], kc == 0, kc == 15, [xeT[b], WU[b]], [PU])
                          ACT(sgl[u].t[:], PG.t[:], AF.Silu, [PG], [sgl[u]])
                          TT("dve", hh[u].t[:], sgl[u].t[:], PU.t[:], ALU.mult, [sgl[u], PU], [hh[u]])
                          for k in range(4):
                              TR(PH.t[:, k, :], hh[u].t[:, k * 128:(k + 1) * 128], identb, [hh[u], CB], [PH])
                          CP("dve", hT[u].t[:], PH.t[:, 0:4, :], [PH], [hT[u]])
                          for ch in range(4):
                              py = PYe[ch % 2]
                              for k in range(4):
                                  MM(py.t[:], hT[u].t[:, k, :], WD[b].t[:, k, ch * 512:(ch + 1) * 512], k == 0, k == 3, [hT[u], WD[b]], [py])
                              CP("act" if ch % 2 == 0 else "dve", ye[u].t[:, ch * 512:(ch + 1) * 512], py.t[:], [py], [ye[u]])
                          r0 = e * CAP + hf * 128
                          DMA("sp", Ys[r0:r0 + 128, :], ye[u].t[:], [ye[u]], [P.reg("YS")])
                  END_PHASE()

          def combine(l, aT1, dbg_name):
              with ExitStack() as es:
                  G2 = SB(es, "G2", [128, D]); A1 = SB(es, "cA1", [128, D]); B1 = SB(es, "cB1", [128, D])
                  Y0 = [SB(es, f"Y0{i}", [128, D], BF16) for i in range(2)]
                  Y1 = [SB(es, f"Y1{i}", [128, D], BF16) for i in range(2)]
                  hn = [SB(es, f"chn{i}", [128, D]) for i in range(2)]
                  z = SB(es, "cz", [128, D]); h2 = [SB(es, f"ch2{i}", [128, D]) for i in range(2)]
                  ss = SB(es, "css", [128, 1]); rstd = SB(es, "crstd", [128, 1]); junk = SB(es, "cjunk", [128, D], BF16)
                  xn = SB(es, "cxn", [128, D]); ab_ = SB(es, "cab", [128, D], BF16)
                  pt = [PS(es, f"cpt{i}", [128, 8, 128], BF16) for i in range(2)]
                  DMA("sp", G2.t[:], DER[l, :, 5, :], [], [G2])
                  if l == 0:
                      DMA("sp", B1.t[:], DER[1, :, 0, :], [], [B1]); DMA("sp", A1.t[:], DER[1, :, 1, :], [], [A1])
                  else:
                      DMA("act", A1.t[:], fing[0:1, :].partition_broadcast(128), [], [A1])
                  for i in range(NT):
                      b = i % 2
                      for k, Yk in enumerate([Y0[b], Y1[b]]):
                          P.dma("pool", lambda e, Yk=Yk, i=i, k=k: e.indirect_dma_start(
                              out=Yk.t[:], out_offset=None, in_=Ys[:, :],
                              in_offset=bass.IndirectOffsetOnAxis(ap=SLOTG.t[:, i, k:k + 1], axis=0)),
                              rr([SLOTG, P.reg("YS")]), rr([Yk]))
                      DMA("sp", hn[b].t[:], Hs[i * 128:(i + 1) * 128, :], [P.reg("HS", i)], [hn[b]])
                      TS("dve", z.t[:], Y0[b].t[:], GATES.t[:, i, 0:1], None, ALU.mult, None, [Y0[b], GATES], [z])
                      STT("dve", z.t[:], Y1[b].t[:], GATES.t[:, i, 1:2], z.t[:], ALU.mult, ALU.add, [Y1[b], GATES, z], [z])
                      TT("dve", z.t[:], z.t[:], G2.t[:], ALU.mult, [z, G2], [z])
                      TT("pool", h2[b].t[:], z.t[:], hn[b].t[:], ALU.add, [z, hn[b]], [h2[b]])
                      if debug and dbg_name in dbg:
                          DMA("sp", dbg[dbg_name][i * 128:(i + 1) * 128, :], h2[b].t[:], [h2[b]], [P.reg("DBG", i)])
                      if l == 0:
                          DMA("sp", Hs[i * 128:(i + 1) * 128, :], h2[b].t[:], [h2[b]], [P.reg("HS", i)])
                          norm_mod_transpose(es, h2[b], A1, B1, aT1, i * 128, (ss, rstd, junk, xn, ab_, pt))
                      else:
                          rms_rstd(h2[b].t[:], ss, rstd, junk, [h2[b]])
                          ACT(xn.t[:], h2[b].t[:], AF.Copy, [h2[b], rstd], [xn], scale=rstd.t[:])
                          TT("dve", z.t[:], xn.t[:], A1.t[:], ALU.mult, [xn, A1], [z])
                          DMA("sp", out[i * 128:(i + 1) * 128, :], z.t[:], [z], [P.reg("OUT", i)])
                  END_PHASE()

          post_mixer(0, w_out, x, "hmid0")
          experts(0)
          with ExitStack() as esM:
              MEAN = SB(esM, "MEAN", [128, T]); RSTD = SB(esM, "RSTD", [128, T])
              with ExitStack() as esG:
                  aT1 = SB(esG, "aT1", [128, 16, T], BF16)
                  combine(0, aT1, "hend0")
                  with ExitStack() as es:
                      wv = [SB(es, f"wv{i}", [128, 16, 128], BF16) for i in range(2)]
                      wgt = [SB(es, f"wgt{i}", [128, 16, 128], BF16) for i in range(2)]
                      WDW = SB(es, "WDW", [128, 16, 31]); BDW = SB(es, "BDW", [128, 16])
                      sgm = [SB(es, f"sgm{i}", [128, 512]) for i in range(2)]
                      uT = [SB(es, f"uT{i}", [128, T]) for i in range(2)]
                      acc = [SB(es, f"acc{i}", [128, T]) for i in range(2)]
                      v2 = SB(es, "v2", [128, T]); MQ = SB(es, "MQ", [128, T])
                      PV = [PS(es, f"PV{i}", [128, 512]) for i in range(2)]
                      PGt = [PS(es, f"PGt{i}", [128, 512]) for i in range(2)]
                      PS1 = [PS(es, f"PS1{i}", [128, 512]) for i in range(2)]
                      DMA("sp", WDW.t[:], wdw[:, :, :], [], [WDW]); DMA("sp", BDW.t[:], bdw[:, :], [], [BDW])
                      pwv = w_pw1.rearrange("(kc p) n -> p kc n", p=128)

                      def ldcw(cc_):
                          DMA("pool", wv[cc_ % 2].t[:], pwv[:, :, cc_ * 128:(cc_ + 1) * 128], [], [wv[cc_ % 2]])
                          DMA("pool", wgt[cc_ % 2].t[:], pwv[:, :, D + cc_ * 128:D + (cc_ + 1) * 128], [], [wgt[cc_ % 2]])
                      ldcw(0)
                      MEMSET("pool", MEAN.t[:], 0.0, [MEAN]); MEMSET("pool", MQ.t[:], 0.0, [MQ])
                      for cc_ in range(16):
                          b = cc_ % 2
                          if cc_ + 1 < 16:
                              ldcw(cc_ + 1)
                          for tq in range(4):
                              pv = PV[tq % 2]; pg = PGt[tq % 2]
                              for kc in range(16):
                                  MM(pv.t[:], wv[b].t[:, kc, :], aT1.t[:, kc, tq * 512:(tq + 1) * 512], kc == 0, kc == 15, [wv[b], aT1], [pv])
                              for kc in range(16):
                                  MM(pg.t[:], wgt[b].t[:, kc, :], aT1.t[:, kc, tq * 512:(tq + 1) * 512], kc == 0, kc == 15, [wgt[b], aT1], [pg])
                              ACT(sgm[tq % 2].t[:], pg.t[:], AF.Sigmoid, [pg], [sgm[tq % 2]])
                              TT("dve", uT[b].t[:, tq * 512:(tq + 1) * 512], pv.t[:], sgm[tq % 2].t[:], ALU.mult, [pv, sgm[tq % 2]], [uT[b]])
                          eng = "dve"
                          u_ = uT[b]; a_ = acc[b]
                          TS(eng, a_.t[:], u_.t[:], WDW.t[:, cc_, 15:16], BDW.t[:, cc_:cc_ + 1], ALU.mult, ALU.add, [u_, WDW, BDW], [a_])
                          for j in range(31):
                              s = j - 15
                              if s == 0:
                                  continue
                              wj = WDW.t[:, cc_, j:j + 1]
                              if cc_ < 8:
                                  c0 = max(0, -s); c1 = min(64, 64 - s)
                                  a3 = a_.t[:].rearrange("p (r c) -> p r c", c=64); u3 = u_.t[:].rearrange("p (r c) -> p r c", c=64)
                                  STT(eng, a3[:, :, c0:c1], u3[:, :, c0 + s:c1 + s], wj, a3[:, :, c0:c1], ALU.mult, ALU.add, [u_, WDW, a_], [a_])
                              else:
                                  r0 = max(0, -s); r1 = min(32, 32 - s)
                                  STT(eng, a_.t[:, r0 * 64:r1 * 64], u_.t[:, (r0 + s) * 64:(r1 + s) * 64], wj, a_.t[:, r0 * 64:r1 * 64], ALU.mult, ALU.add, [u_, WDW, a_], [a_])
                          DMA("sp", Vd[cc_ * 128:(cc_ + 1) * 128, :], a_.t[:], [a_], [P.reg("VD")])
                          ACT(v2.t[:], a_.t[:], AF.Square, [a_], [v2])
                          for tq in range(4):
                              MM(PS1[0].t[:], onesf, a_.t[:, tq * 512:(tq + 1) * 512], True, True, [CF, a_], [PS1[0]])
                              TT("dve", MEAN.t[:, tq * 512:(tq + 1) * 512], MEAN.t[:, tq * 512:(tq + 1) * 512], PS1[0].t[:], ALU.add, [MEAN, PS1[0]], [MEAN])
                              MM(PS1[1].t[:], onesf, v2.t[:, tq * 512:(tq + 1) * 512], True, True, [CF, v2], [PS1[1]])
                              TT("dve", MQ.t[:, tq * 512:(tq + 1) * 512], MQ.t[:, tq * 512:(tq + 1) * 512], PS1[1].t[:], ALU.add, [MQ, PS1[1]], [MQ])
                      TS("dve", MEAN.t[:], MEAN.t[:], 1.0 / D, None, ALU.mult, None, [MEAN], [MEAN])
                      TT("dve", v2.t[:], MEAN.t[:], MEAN.t[:], ALU.mult, [MEAN], [v2])
                      STT("dve", MQ.t[:], MQ.t[:], 1.0 / D, v2.t[:], ALU.mult, ALU.subtract, [MQ, v2], [MQ])
                      ACT(RSTD.t[:], MQ.t[:], AF.Sqrt, [MQ, EPSb], [RSTD], bias=EPSb.t[:])
                      RECIP(RSTD.t[:], RSTD.t[:], [RSTD], [RSTD])
                      END_PHASE()
              with ExitStack() as es:
                  LNG = SB(es, "LNG", [128, 16]); LNB = SB(es, "LNB", [128, 16])
                  vt = [SB(es, f"vt{i}", [128, T]) for i in range(2)]
                  sTc = [SB(es, f"sTc{i}", [128, T], BF16) for i in range(2)]
                  DMA("sp", LNG.t[:], lng[:, :], [], [LNG]); DMA("sp", LNB.t[:], lnb[:, :], [], [LNB])
                  for cc_ in range(16):
                      b = cc_ % 2
                      DMA("sp", vt[b].t[:], Vd[cc_ * 128:(cc_ + 1) * 128, :], [P.reg("VD")], [vt[b]])
                      TT("dve", vt[b].t[:], vt[b].t[:], MEAN.t[:], ALU.subtract, [vt[b], MEAN], [vt[b]])
                      TT("pool", vt[b].t[:], vt[b].t[:], RSTD.t[:], ALU.mult, [vt[b], RSTD], [vt[b]])
                      ACT(sTc[b].t[:], vt[b].t[:], AF.Silu, [vt[b], LNG, LNB], [sTc[b]], scale=LNG.t[:, cc_:cc_ + 1], bias=LNB.t[:, cc_:cc_ + 1])
                      DMA("sp", OGT[cc_ * 128:(cc_ + 1) * 128, :], sTc[b].t[:], [sTc[b]], [P.reg("OGT")])
                  END_PHASE()
          post_mixer(1, w_pw2, Hs, "hmid1")
          experts(1)
          combine(1, None, "hend1")
    except _Stop:
        pass
    return nc


def _consts():
    s = np.arange(128)[:, None]; t = np.arange(128)[None, :]
    cf = np.zeros((128, 9, 128), np.float32)
    cf[:, 0] = np.eye(128)
    cf[:, 1] = (s <= t).astype(np.float32) - (s <= 63).astype(np.float32)
    cf[:, 2] = (s >= t).astype(np.float32) - (s >= 64).astype(np.float32)
    cf[:, 3] = (s > t)
    cf[:, 4] = (s < t)
    cf[:, 5] = (s <= t)
    cf[:, 6] = (s >= t)
    cf[:, 7] = 1.0
    cf[:, 8, 0] = (np.arange(128) <= 63); cf[:, 8, 1] = (np.arange(128) >= 64)
    cb = np.zeros((128, 3, 128), np.float32)
    cb[:, 0] = np.eye(128); cb[:, 1] = 1.0; cb[:, 2] = (s < t)
    iot = np.tile(np.arange(65, dtype=np.float32)[None, :], (128, 1))
    iot[:, 64] = XS_ROWS + np.arange(128)
    return cf, cb.astype(ml_dtypes.bfloat16), iot


def _col(v):
    return np.ascontiguousarray(np.asarray(v, np.float32).reshape(16, 128).T)


def kernel(x, c, ctx, c_ctx, ada_w, ada_b, norm1_g, norm2_g, hgrn_w_in, hgrn_lb, hgrn_onorm_g,
           hgrn_w_out, conv_w_pw1, conv_w_dw, conv_b_dw, conv_ln_g, conv_ln_b, conv_w_pw2,
           moe_w_r1, moe_b_r1, moe_w_r2, moe_b_r2, moe_w_gate, moe_w_up, moe_w_down, final_g, _debug=False, _stop=None, _ncores=8, _trace=False):
    f = lambda a: np.ascontiguousarray(np.asarray(a, np.float32))
    key = ("nc", bool(_debug), _stop)
    if key not in _cache:
        _cache[key] = build_program(debug=_debug, stop=_stop)
    nc = _cache[key]
    cf, cb, iot = _consts()
    w_r2 = np.asarray(moe_w_r2, np.float32)
    wr_ = np.concatenate([np.asarray(moe_w_r1, np.float32), w_r2.transpose(0, 2, 1, 3).reshape(2, D, 64)], axis=2)
    br_ = np.concatenate([np.asarray(moe_b_r1, np.float32), np.asarray(moe_b_r2, np.float32).reshape(2, 64)], axis=1)
    wdw_ = np.ascontiguousarray(np.asarray(conv_w_dw, np.float32)[0].T.reshape(16, 128, 31).transpose(1, 0, 2))
    shared = {
        "ada_w": f(ada_w), "ada_b": f(ada_b), "n1g": f(norm1_g), "n2g": f(norm2_g), "fing": f(final_g).reshape(1, D),
        "w_in": np.ascontiguousarray(f(hgrn_w_in)[0].reshape(D, 5, 16, 128).transpose(2, 0, 1, 3).reshape(16, D, 640)), "hlb": f(hgrn_lb)[:, :, :], "ong": f(hgrn_onorm_g).reshape(1, 128), "w_out": f(hgrn_w_out)[0],
        "w_pw1": f(conv_w_pw1)[0], "wdw": wdw_, "bdw": _col(np.asarray(conv_b_dw)[0]), "lng": _col(np.asarray(conv_ln_g)[0]),
        "lnb": _col(np.asarray(conv_ln_b)[0]), "w_pw2": f(conv_w_pw2)[0],
        "wr": np.ascontiguousarray(wr_), "brr": np.ascontiguousarray(br_),
        "cst_f": cf, "cst_b": cb, "iot": iot,
    }
    if _stop is None or _stop >= 5:
        shared.update({"wg": f(moe_w_gate), "wu": f(moe_w_up), "wd": f(moe_w_down)})
    xx = f(x); cx = f(ctx); cc = np.asarray(c, np.float32); ccx = np.asarray(c_ctx, np.float32)
    in_maps = []
    for b in range(_ncores):
        m = dict(shared)
        m["x"] = xx[b]; m["ctx"] = cx[b]
        m["ccol"] = np.ascontiguousarray(np.concatenate([_col(cc[b]), _col(ccx)], axis=1))
        in_maps.append(m)
    res = run_bass_kernel_spmd(nc, in_maps, core_ids=list(range(_ncores)), **({'trace': True} if _trace else {}))
    if _trace:
        print('EXEC_NS', _stop, res.exec_time_ns)
    outp = np.stack([np.asarray(r["out"]) for r in res.results], axis=0).astype(np.float32)
    if _debug:
        return outp, res.results
    return outp
```
